# Optimizing a Trainium2 kernel written in Bass

```python
import math
import jax, jax.numpy as jnp
from jax import lax
import numpy as np

D_MODEL = 2048
BATCH = 4
SEQ = 8192
DEPTH = 1

HEAD_DIM = 128
HEADS_A = D_MODEL // 256
KV_HEADS_A = HEADS_A // 4
HEADS_B = D_MODEL // 256
KV_HEADS_B = HEADS_B // 4
Q_WIDTH_A = HEADS_A * HEAD_DIM
KV_WIDTH_A = KV_HEADS_A * HEAD_DIM
Q_WIDTH_B = HEADS_B * HEAD_DIM
KV_WIDTH_B = KV_HEADS_B * HEAD_DIM
IN_WIDTH = Q_WIDTH_A + 2 * KV_WIDTH_A + Q_WIDTH_B + 2 * KV_WIDTH_B + 2 * D_MODEL
BLOCK = 128
WINDOW = 128
GRID_W = 64
ROPE_THETA = 10000.0
NUM_BUCKETS = 32
MAX_DISTANCE = 128
N_EXPERTS = 16
CAPACITY_FACTOR = 2
D_FF_EXPERT = D_MODEL // 2
EPS = 1e-6
NEG = -1e30

kernel_name = "hybrid_gated_gqa_window_ecmoe"


def rmsnorm(x, g):
    xf = x.astype(jnp.float32)
    y = xf * lax.rsqrt(jnp.mean(xf * xf, axis=-1, keepdims=True) + EPS)
    return (y * g.astype(jnp.float32)).astype(x.dtype)


def rotate_half(xh, cos, sin):
    n = xh.shape[-1] // 2
    x1, x2 = xh[..., :n], xh[..., n:]
    return jnp.concatenate([x1 * cos - x2 * sin, x2 * cos + x1 * sin], axis=-1)


def axial_rope(x, seq_len):
    rows = seq_len // GRID_W
    r = jnp.repeat(jnp.arange(rows, dtype=jnp.float32), GRID_W)
    c = jnp.tile(jnp.arange(GRID_W, dtype=jnp.float32), rows)
    half = HEAD_DIM // 2
    inv = 1.0 / (ROPE_THETA ** (jnp.arange(0, half, 2, dtype=jnp.float32) / half))
    ang_r = r[:, None, None] * inv
    ang_c = c[:, None, None] * inv
    xr = rotate_half(x[..., :half], jnp.cos(ang_r), jnp.sin(ang_r))
    xc = rotate_half(x[..., half:], jnp.cos(ang_c), jnp.sin(ang_c))
    return jnp.concatenate([xr, xc], axis=-1)


def t5_bucket(rel):
    half = NUM_BUCKETS // 2
    ret = jnp.where(rel > 0, half, 0)
    n = jnp.abs(rel)
    max_exact = half // 2
    nf = jnp.maximum(n, 1).astype(jnp.float32)
    large = max_exact + (jnp.log(nf / max_exact) / math.log(MAX_DISTANCE / max_exact)
                         * (half - max_exact)).astype(jnp.int32)
    large = jnp.minimum(large, half - 1)
    return ret + jnp.where(n < max_exact, n, large)


def global_axial_gqa(q, k, v, qn, kn):
    bsz, s, _, hd = q.shape
    g = HEADS_A // KV_HEADS_A
    nb = s // BLOCK
    q = axial_rope(rmsnorm(q.astype(jnp.float32), qn), s)
    k = axial_rope(rmsnorm(k.astype(jnp.float32), kn), s)
    v = v.astype(jnp.float32)
    scale = 1.0 / math.sqrt(hd)
    qblocks = q.reshape(bsz, nb, BLOCK, KV_HEADS_A, g, hd).transpose(1, 0, 3, 4, 2, 5)
    kt = k.transpose(0, 2, 1, 3)
    vt = v.transpose(0, 2, 1, 3)

    def one_block(qb):
        sc = jnp.einsum('bkgqd,bksd->bkgqs', qb, kt) * scale
        p = jax.nn.softmax(sc, axis=-1)
        return jnp.einsum('bkgqs,bksd->bkgqd', p, vt)

    o = lax.map(one_block, qblocks)
    return o.transpose(1, 0, 4, 2, 3, 5).reshape(bsz, s, HEADS_A * hd)


def window_sink_gqa(q, k, v, sink, rel_bias):
    bsz, s, _, hd = q.shape
    g = HEADS_B // KV_HEADS_B
    nb = s // BLOCK
    scale = 1.0 / math.sqrt(hd)
    q = q.astype(jnp.float32).reshape(bsz, nb, BLOCK, KV_HEADS_B, g, hd)
    pad = ((0, 0), (WINDOW, WINDOW), (0, 0), (0, 0))
    kp = jnp.pad(k.astype(jnp.float32), pad).reshape(bsz, nb + 2, BLOCK, KV_HEADS_B, hd)
    vp = jnp.pad(v.astype(jnp.float32), pad).reshape(bsz, nb + 2, BLOCK, KV_HEADS_B, hd)
    kband = jnp.concatenate([kp[:, :-2], kp[:, 1:-1], kp[:, 2:]], axis=2)
    vband = jnp.concatenate([vp[:, :-2], vp[:, 1:-1], vp[:, 2:]], axis=2)
    sc = jnp.einsum('bnqkgd,bnjkd->bnkgqj', q, kband) * scale
    rel = (jnp.arange(3 * BLOCK) - BLOCK)[None, :] - jnp.arange(BLOCK)[:, None]
    bias = rel_bias.astype(jnp.float32)[t5_bucket(rel)]
    bias = bias.transpose(2, 0, 1).reshape(KV_HEADS_B, g, BLOCK, 3 * BLOCK)
    kpos = jnp.arange(nb)[:, None] * BLOCK - BLOCK + jnp.arange(3 * BLOCK)[None, :]
    valid = (jnp.abs(rel) <= WINDOW)[None] & ((kpos >= 0) & (kpos < s))[:, None, :]
    valid = valid[None, :, None, None]
    sc = jnp.where(valid, sc + bias, NEG)
    sk = sink.astype(jnp.float32).reshape(KV_HEADS_B, g, 1, 1)
    m = jnp.maximum(jnp.max(sc, axis=-1, keepdims=True), sk)
    e = jnp.exp(sc - m)
    p = e / (jnp.sum(e, axis=-1, keepdims=True) + jnp.exp(sk - m))
    o = jnp.einsum('bnkgqj,bnjkd->bnqkgd', p, vband)
    return o.reshape(bsz, s, HEADS_B * hd)


def expert_choice_moe(h, w_router, w_gate_e, w_up_e, w_down_e):
    bsz, s, d = h.shape
    cap = CAPACITY_FACTOR * s // N_EXPERTS
    logits = jnp.einsum('bsd,de->bse', h, w_router).astype(jnp.float32)
    aff = jax.nn.softmax(logits, axis=-1).transpose(0, 2, 1)
    gates, idx = lax.top_k(aff, cap)
    xin = jax.vmap(lambda hb, ib: hb[ib])(h, idx)
    a = jnp.einsum('becd,edf->becf', xin, w_gate_e)
    u = jnp.einsum('becd,edf->becf', xin, w_up_e)
    y = jnp.einsum('becf,efd->becd', jax.nn.silu(a) * u, w_down_e)
    y = y * gates[..., None].astype(y.dtype)

    def scatter(ib, yb):
        return jnp.zeros((s, d), yb.dtype).at[ib.reshape(-1)].add(yb.reshape(-1, d))

    return jax.vmap(scatter)(idx, y)


def setup_inputs(seed: int = 0) -> dict:
    key = jax.random.key(seed)
    ks = jax.random.split(key, 20)
    f32 = jnp.float32
    nrm = lambda k, shape, sc: jax.random.normal(k, shape, f32) * sc
    return {
        "x": nrm(ks[0], (BATCH, SEQ, D_MODEL), 1.0),
        "g_mix": 1.0 + nrm(ks[1], (DEPTH, D_MODEL), 0.05),
        "w_in": nrm(ks[2], (DEPTH, D_MODEL, IN_WIDTH), D_MODEL ** -0.5),
        "b_gate": nrm(ks[3], (DEPTH, 2 * D_MODEL), 0.1),
        "qn_a": 1.0 + nrm(ks[4], (DEPTH, HEAD_DIM), 0.05),
        "kn_a": 1.0 + nrm(ks[5], (DEPTH, HEAD_DIM), 0.05),
        "w_proj_a": nrm(ks[6], (DEPTH, Q_WIDTH_A, D_MODEL), Q_WIDTH_A ** -0.5),
        "sink_b": nrm(ks[7], (DEPTH, HEADS_B), 0.5),
        "rel_bias": nrm(ks[8], (NUM_BUCKETS, HEADS_B), 0.5),
        "w_proj_b": nrm(ks[9], (DEPTH, Q_WIDTH_B, D_MODEL), Q_WIDTH_B ** -0.5),
        "w_o": nrm(ks[10], (DEPTH, D_MODEL, D_MODEL), D_MODEL ** -0.5),
        "g_ffn": 1.0 + nrm(ks[11], (DEPTH, D_MODEL), 0.05),
        "w_router": nrm(ks[12], (DEPTH, D_MODEL, N_EXPERTS), D_MODEL ** -0.5),
        "w_gate_e": nrm(ks[13], (DEPTH, N_EXPERTS, D_MODEL, D_FF_EXPERT), D_MODEL ** -0.5),
        "w_up_e": nrm(ks[14], (DEPTH, N_EXPERTS, D_MODEL, D_FF_EXPERT), D_MODEL ** -0.5),
        "w_down_e": nrm(ks[15], (DEPTH, N_EXPERTS, D_FF_EXPERT, D_MODEL), D_FF_EXPERT ** -0.5),
        "g_final": 1.0 + nrm(ks[16], (D_MODEL,), 0.05),
    }


def reference(x, g_mix, w_in, b_gate, qn_a, kn_a, w_proj_a, sink_b, rel_bias, w_proj_b,
              w_o, g_ffn, w_router, w_gate_e, w_up_e, w_down_e, g_final):
    bsz, s, d = x.shape
    splits = np.cumsum([Q_WIDTH_A, KV_WIDTH_A, KV_WIDTH_A, Q_WIDTH_B, KV_WIDTH_B, KV_WIDTH_B, D_MODEL]).tolist()
    for l in range(DEPTH):
        h = rmsnorm(x, g_mix[l])
        proj = jnp.einsum('bsd,dn->bsn', h, w_in[l])
        qa, ka, va, qb, kb, vb, ga, gb = jnp.split(proj, splits, axis=-1)
        oa = global_axial_gqa(qa.reshape(bsz, s, HEADS_A, HEAD_DIM),
                              ka.reshape(bsz, s, KV_HEADS_A, HEAD_DIM),
                              va.reshape(bsz, s, KV_HEADS_A, HEAD_DIM), qn_a[l], kn_a[l]).astype(x.dtype)
        ob = window_sink_gqa(qb.reshape(bsz, s, HEADS_B, HEAD_DIM),
                             kb.reshape(bsz, s, KV_HEADS_B, HEAD_DIM),
                             vb.reshape(bsz, s, KV_HEADS_B, HEAD_DIM), sink_b[l], rel_bias).astype(x.dtype)
        gate_a = jax.nn.sigmoid(ga + b_gate[l, :D_MODEL])
        gate_b = jax.nn.sigmoid(gb + b_gate[l, D_MODEL:])
        merged = gate_a * jnp.einsum('bsc,cd->bsd', oa, w_proj_a[l]) \
            + gate_b * jnp.einsum('bsc,cd->bsd', ob, w_proj_b[l])
        x = x + jnp.einsum('bsd,de->bse', merged, w_o[l])
        x = x + expert_choice_moe(rmsnorm(x, g_ffn[l]), w_router[l], w_gate_e[l], w_up_e[l], w_down_e[l])
    return rmsnorm(x, g_final)
```

```python
import math
from contextlib import ExitStack
import numpy as np
import ml_dtypes
import concourse.bass as bass
import concourse.mybir as mybir
from concourse.bass_utils import run_bass_kernel_spmd

F32 = mybir.dt.float32
BF16 = mybir.dt.bfloat16
I32 = mybir.dt.int32
AF = mybir.ActivationFunctionType
ALU = mybir.AluOpType
AX = mybir.AxisListType

S = 8192
D = 2048
NT = 64
NOWN = 32
KC = 16
NE = 16
CAP = 1024
FF = 1024
EPS = 1e-6
SCALE = 1.0 / math.sqrt(128.0)
NEG = -1e30
NBISECT = 26
SPECIAL = [0, 31, 32, 63]


class Buf:
    __slots__ = ("name", "w", "r", "excl")

    def __init__(self, name, excl=False):
        self.name = name
        self.w = None
        self.r = {}
        self.excl = excl


class Trk:
    EPOCH = 12000
    RING = 16

    def __init__(self, nc, stack):
        self.nc = nc
        self.stack = stack
        self.eng = {"pe": nc.tensor, "act": nc.scalar, "dve": nc.vector, "pool": nc.gpsimd, "sp": nc.sync}
        self.sems = {}
        self.cnt = {e: 0 for e in self.eng}
        self.dcnt = {e: 0 for e in self.eng}
        self.obs = {e: {} for e in self.eng}
        self.owner = {}
        self.latest = {}
        self.nwait = 0
        self.pq = []
        self.pq_sum = 0
        self.nop = 0
        self.limit = None
        self.log = []

    def _sem(self, key, owner):
        if key not in self.sems:
            self.sems[key] = self.stack.enter_context(self.nc.semaphore("s_%s_%s" % key))
            self.owner[key] = owner
        return self.sems[key]

    def _wait(self, e, key, val):
        if self.obs[e].get(key, 0) >= val:
            return
        self.eng[e].wait_ge(self.sems[key], val)
        self.obs[e][key] = val
        self.nwait += 1

    def op(self, e, fn, reads=(), writes=(), dma=False, nd=2048):
        self.nop += 1
        if self.limit is not None and self.nop > self.limit:
            return None
        if self.limit is not None:
            import inspect
            self.log.append((self.nop, e, inspect.stack()[1].lineno))
        deps = {}
        if dma and e == "pool":
            while self.pq and self.pq_sum + nd > 9000:
                otok, ond = self.pq.pop(0)
                self._wait("pool", otok[0], otok[1])
                self.pq_sum -= ond

        def add(tok):
            if tok is None:
                return
            k, v = tok
            if deps.get(k, 0) < v:
                deps[k] = v

        for b in reads:
            add(b.w)
            if b.excl:
                for k, v in b.r.items():
                    if self.owner[k][0] != e:
                        add((k, v))
        for b in writes:
            add(b.w)
            for k, v in b.r.items():
                add((k, v))
        for k, v in deps.items():
            if e == "pe" and self.owner[k] == ("pe", False):
                continue
            self._wait(e, k, v)
        if dma:
            n = self.dcnt[e]
            self.dcnt[e] += 1
            key = ("d" + e, n % self.RING)
            val = 16 * (n // self.RING + 1)
            sem = self._sem(key, (e, True))
            fn(self.eng[e]).then_inc(sem, 16)
        else:
            n = self.cnt[e]
            self.cnt[e] += 1
            key = (e, n // self.EPOCH)
            val = n % self.EPOCH + 1
            sem = self._sem(key, (e, False))
            fn(self.eng[e]).then_inc(sem, 1)
        tok = (key, val)
        if dma and e == "pool":
            self.pq.append((tok, nd))
            self.pq_sum += nd
        self.latest[key] = val
        for b in reads:
            if b.r.get(key, 0) < val:
                b.r[key] = val
        for b in writes:
            b.w = tok
            b.r = {}
        return tok

    def sync_all(self, engines=None):
        for e in (engines or list(self.eng)):
            for k, v in self.latest.items():
                self._wait(e, k, v)


def rope(T, src, t1, t2, dst, C, S_, nh, reads, writes):
    T.op("dve", lambda e: e.tensor_tensor(out=t1[:], in0=src[:], in1=C[:].unsqueeze(1).to_broadcast([128, nh, 128]), op=ALU.mult),
         reads=reads, writes=writes)
    s5 = src[:].rearrange("p h (a b c) -> p h a b c", a=2, b=2, c=32)
    t5 = t2[:].rearrange("p h (a b c) -> p h a b c", a=2, b=2, c=32)
    S4 = S_[:].rearrange("p (a b c) -> p a b c", a=2, b=2, c=32)
    for b in range(2):
        T.op("dve", lambda e, b=b: e.tensor_tensor(out=t5[:, :, :, b, :], in0=s5[:, :, :, 1 - b, :],
                                                   in1=S4[:, :, b, :].unsqueeze(1).to_broadcast([128, nh, 2, 32]), op=ALU.mult),
             reads=reads, writes=writes)
    T.op("dve", lambda e: e.tensor_tensor(out=dst[:], in0=t1[:], in1=t2[:], op=ALU.add), reads=reads, writes=writes)


def kernel(x, g_mix, w_in, b_gate, qn_a, kn_a, w_proj_a, sink_b, rel_bias, w_proj_b,
           w_o, g_ffn, w_router, w_gate_e, w_up_e, w_down_e, g_final):
    nc = build_program()
    in_maps = make_inputs(x, g_mix, w_in, b_gate, qn_a, kn_a, w_proj_a, sink_b, rel_bias, w_proj_b,
                          w_o, g_ffn, w_router, w_gate_e, w_up_e, w_down_e, g_final)
    res = run_bass_kernel_spmd(nc, in_maps, core_ids=list(range(8)))
    out = np.empty((4, S, D), np.float32)
    for c in range(8):
        b, hf = c // 2, c % 2
        out[b, hf * 4096:(hf + 1) * 4096] = res.results[c]["out"]
    return out


def _t5_bucket(rel):
    half = 16
    ret = np.where(rel > 0, half, 0)
    n = np.abs(rel)
    max_exact = 8
    nf = np.maximum(n, 1).astype(np.float32)
    large = max_exact + (np.log(nf / max_exact) / math.log(128 / max_exact) * (half - max_exact)).astype(np.int32)
    large = np.minimum(large, half - 1)
    return ret + np.where(n < max_exact, n, large)


def make_inputs(x, g_mix, w_in, b_gate, qn_a, kn_a, w_proj_a, sink_b, rel_bias, w_proj_b,
                w_o, g_ffn, w_router, w_gate_e, w_up_e, w_down_e, g_final):
    f = lambda a: np.ascontiguousarray(np.asarray(a, dtype=np.float32))
    x = f(x)
    pos = np.arange(S)
    r = (pos // 64).astype(np.float32)
    c = (pos % 64).astype(np.float32)
    inv = (1.0 / (10000.0 ** (np.arange(0, 64, 2, dtype=np.float32) / 64.0))).astype(np.float32)
    ar = r[:, None] * inv[None, :]
    ac = c[:, None] * inv[None, :]
    ropeC = np.concatenate([np.cos(ar), np.cos(ar), np.cos(ac), np.cos(ac)], axis=1).astype(np.float32)
    ropeS = np.concatenate([-np.sin(ar), np.sin(ar), -np.sin(ac), np.sin(ac)], axis=1).astype(np.float32)
    rel = (np.arange(384) - 128)[None, :] - np.arange(128)[:, None]
    bucket = _t5_bucket(rel)
    band = np.where(np.abs(rel) <= 128, 0.0, NEG).astype(np.float32)
    biasB = np.ascontiguousarray(f(rel_bias)[bucket].transpose(0, 2, 1))
    ident = np.eye(128, dtype=np.float32).astype(ml_dtypes.bfloat16)
    triU = np.triu(np.ones((128, 128), np.float32), 1).astype(ml_dtypes.bfloat16)
    bg = np.ascontiguousarray(f(b_gate).reshape(32, 128).T)
    shared = {
        "w_in": f(w_in)[0], "w_pa": f(w_proj_a)[0], "w_pb": f(w_proj_b)[0], "w_o": f(w_o)[0],
        "w_r": f(w_router)[0], "w_g": f(w_gate_e)[0], "w_u": f(w_up_e)[0], "w_d": f(w_down_e)[0],
        "g_mix": f(g_mix).reshape(1, D), "g_ffn": f(g_ffn).reshape(1, D), "g_fin": f(g_final).reshape(1, D),
        "qn": f(qn_a).reshape(1, 128), "kn": f(kn_a).reshape(1, 128), "sink": f(sink_b).reshape(1, 8),
        "bgate": bg, "biasB": biasB, "band": band, "ident": ident, "triU": triU,
    }
    maps = []
    for core in range(8):
        b, hf = core // 2, core % 2
        perm = (np.arange(S) + hf * 4096) % S
        em = np.zeros((4, 384), np.float32)
        for i, t in enumerate(SPECIAL):
            orig = (t + hf * 32) % 64
            if orig == 0:
                em[i, 0:128] = NEG
            if orig == 63:
                em[i, 256:384] = NEG
        m = dict(shared)
        m["x"] = np.ascontiguousarray(x[b][perm])
        m["ropeC"] = np.ascontiguousarray(ropeC[perm])
        m["ropeS"] = np.ascontiguousarray(ropeS[perm])
        m["emask"] = em
        maps.append(m)
    return maps


def build_program(stop=None, dbg=(), nt1=NT, flags=(), limit=None):
    nc = bass.Bass("TRN2", target_bir_lowering=False)
    dt_in = lambda n, s, d=F32: nc.dram_tensor(n, s, d, kind="ExternalInput").ap()
    dt_sc = lambda n, s, d: nc.dram_tensor(n, s, d, kind=("ExternalOutput" if n in dbg else "Internal")).ap()
    x = dt_in("x", [S, D])
    w_in = dt_in("w_in", [D, 7168]); w_pa = dt_in("w_pa", [1024, D]); w_pb = dt_in("w_pb", [1024, D])
    w_o = dt_in("w_o", [D, D]); w_r = dt_in("w_r", [D, NE])
    w_g = dt_in("w_g", [NE, D, FF]); w_u = dt_in("w_u", [NE, D, FF]); w_d = dt_in("w_d", [NE, FF, D])
    g_mix = dt_in("g_mix", [1, D]); g_ffn = dt_in("g_ffn", [1, D]); g_fin = dt_in("g_fin", [1, D])
    qn = dt_in("qn", [1, 128]); kn = dt_in("kn", [1, 128]); sink = dt_in("sink", [1, 8])
    bgate_d = dt_in("bgate", [128, 32]); biasB_d = dt_in("biasB", [128, 8, 384]); band_d = dt_in("band", [128, 384])
    ident_d = dt_in("ident", [128, 128], BF16); triU_d = dt_in("triU", [128, 128], BF16)
    ropeC = dt_in("ropeC", [S, 128]); ropeS = dt_in("ropeS", [S, 128]); emask_d = dt_in("emask", [4, 384])
    out = nc.dram_tensor("out", [4096, D], F32, kind="ExternalOutput").ap()

    w_in_bf = dt_sc("w_in_bf", [D, 7168], BF16)
    wgt_s = dt_sc("wgt_s", [32, 128, KC, 128], BF16)
    wp_s = dt_sc("wp_s", [2, 16, 128, 8, 128], BF16)
    wr_bf = dt_sc("wr_bf", [D, NE], BF16)
    wg_bf = dt_sc("wg_bf", [NE, D, FF], BF16); wu_bf = dt_sc("wu_bf", [NE, D, FF], BF16)
    wd_bf = dt_sc("wd_bf", [NE, FF, D], BF16)
    hT_s = dt_sc("hT_s", [16, 128, KC, 512], BF16)
    wq_s = dt_sc("wq_s", [4, 128, KC, 512], BF16)
    wo_s = dt_sc("wo_s", [4, 128, KC, 512], BF16)
    kbT_s = dt_sc("kbT_s", [2, 128, S], BF16)
    vb_s = dt_sc("vb_s", [S, 256], BF16)
    oT_s = dt_sc("oT_s", [2, 16, 128, 8, 512], BF16)
    x1_s = dt_sc("x1_s", [4096, D], F32)
    h2_s = dt_sc("h2_s", [4096, D], BF16)
    Xg = [dt_sc("Xg%d" % i, [CAP, D], BF16) for i in range(NE)]
    Yg = [dt_sc("Yg%d" % i, [CAP, D], BF16) for i in range(NE)]

    with ExitStack() as stack:
        T = Trk(nc, stack)
        T.limit = limit
        build_program.T = T
        sb = lambda n, s, d: stack.enter_context(nc.sbuf_tensor("sb_" + n, s, d))
        B = Buf

        banks = [stack.enter_context(nc.psum_tensor("ps%d" % i, [128, 512], F32)) for i in range(8)]
        bankb = [B("bank%d" % i, True) for i in range(8)]

        class Rot:
            def __init__(self, ids):
                self.ids = ids; self.i = 0

            def get(self):
                k = self.ids[self.i % len(self.ids)]; self.i += 1
                return banks[k], bankb[k]

        b_win = [B("win%d" % i) for i in range(4)]
        for i in range(4):
            T.op("pool", lambda e, i=i: e.dma_start(
                out=w_in_bf[i * 512:(i + 1) * 512, :].rearrange("r (a c) -> r a c", c=1024),
                in_=w_in[i * 512:(i + 1) * 512, :].rearrange("r (a c) -> r a c", c=1024)), writes=[b_win[i]], dma=True, nd=3584)
        b_wr = B("wr")
        b_wqs = [B("wqs%d" % i) for i in range(4)]; b_wosc = [B("wos_s%d" % i) for i in range(4)]
        b_wgt = [B("wgt%d" % i) for i in range(32)]
        b_wp = [B("wp%d" % i) for i in range(32)]
        for g_, c0 in enumerate((0, 512, 1536, 2048)):
            T.op("pool", lambda e, g_=g_, c0=c0: e.dma_start(
                out=wq_s[g_], in_=w_in[:, c0:c0 + 512].rearrange("(k p) c -> p k c", p=128)), writes=[b_wqs[g_]], dma=True)
        deferred = []
        for g_ in range(4):
            deferred.append(lambda g_=g_: T.op("pool", lambda e: e.dma_start(
                out=wo_s[g_], in_=w_o[:, g_ * 512:(g_ + 1) * 512].rearrange("(k p) c -> p k c", p=128)), writes=[b_wosc[g_]], dma=True))
        deferred.append(lambda: T.op("pool", lambda e: e.dma_start(out=wr_bf, in_=w_r), writes=[b_wr], dma=True))
        for n_ in range(32):
            deferred.append(lambda n_=n_: T.op("pool", lambda e: e.dma_start(
                out=wgt_s[n_], in_=w_in[:, 3072 + n_ * 128:3072 + (n_ + 1) * 128].rearrange("(k p) c -> p k c", p=128)),
                writes=[b_wgt[n_]], dma=True))
        for ab, wsrc in enumerate((w_pa, w_pb)):
            for n_ in range(16):
                deferred.append(lambda ab=ab, n_=n_, wsrc=wsrc: T.op("pool", lambda e: e.dma_start(
                    out=wp_s[ab, n_], in_=wsrc[:, n_ * 128:(n_ + 1) * 128].rearrange("(h p) c -> p h c", p=128)),
                    writes=[b_wp[ab * 16 + n_]], dma=True))
        b_wg = [B("wg%d" % i) for i in range(NE)]; b_wu = [B("wu%d" % i) for i in range(NE)]
        b_wd = [B("wd%d" % i) for i in range(NE)]

        def cast_expert(e_):
            T.op("pool", lambda e: e.dma_start(out=wg_bf[e_], in_=w_g[e_]), writes=[b_wg[e_]], dma=True)
            T.op("pool", lambda e: e.dma_start(out=wu_bf[e_], in_=w_u[e_]), writes=[b_wu[e_]], dma=True)
            T.op("pool", lambda e: e.dma_start(out=wd_bf[e_].rearrange("r (a c) -> r a c", c=1024),
                                               in_=w_d[e_].rearrange("r (a c) -> r a c", c=1024)),
                 writes=[b_wd[e_]], dma=True)

        ident = sb("ident", [128, 128], BF16); ones = sb("ones", [128, 128], BF16); triU = sb("triU", [128, 128], BF16)
        qn_bc = sb("qn_bc", [128, 128], F32); kn_bc = sb("kn_bc", [128, 128], F32)
        bgate = sb("bgate", [128, 32], F32); sink_bc = sb("sink_bc", [128, 8], F32); nsink_bc = sb("nsink_bc", [128, 8], F32)
        aff = sb("aff", [128, NT, NE], F32)
        b_const = B("const"); b_aff = [B("aff%d" % j) for j in range(NT)]
        T.op("sp", lambda e: e.dma_start(out=ident[:], in_=ident_d), writes=[b_const], dma=True)
        T.op("sp", lambda e: e.dma_start(out=triU[:], in_=triU_d), writes=[b_const], dma=True)
        T.op("sp", lambda e: e.dma_start(out=qn_bc[:], in_=qn.partition_broadcast(128)), writes=[b_const], dma=True)
        T.op("sp", lambda e: e.dma_start(out=kn_bc[:], in_=kn.partition_broadcast(128)), writes=[b_const], dma=True)
        T.op("sp", lambda e: e.dma_start(out=bgate[:], in_=bgate_d), writes=[b_const], dma=True)
        T.op("sp", lambda e: e.dma_start(out=sink_bc[:], in_=sink.partition_broadcast(128)), writes=[b_const], dma=True)
        T.op("dve", lambda e: e.memset(ones[:], 1.0), writes=[b_const])
        T.op("dve", lambda e: e.tensor_scalar(out=nsink_bc[:], in0=sink_bc[:], scalar1=-1.0, scalar2=None, op0=ALU.mult),
             reads=[b_const], writes=[b_const])

        if stop == 0:
            T.sync_all()
            return nc

        def rstd_from_ss(ss, tmp, rstd, n, inv_n, bufs):
            T.op("dve", lambda e: e.tensor_scalar(out=tmp[:, 0:n], in0=ss[:, 0:n], scalar1=inv_n, scalar2=EPS,
                                                  op0=ALU.mult, op1=ALU.add), reads=bufs, writes=bufs)
            T.op("act", lambda e: e.activation(out=tmp[:, 0:n], in_=tmp[:, 0:n], func=AF.Ln), reads=bufs, writes=bufs)
            T.op("act", lambda e: e.activation(out=rstd[:, 0:n], in_=tmp[:, 0:n], func=AF.Exp, scale=-0.5), reads=bufs, writes=bufs)

        with ExitStack() as kvstack:
            sbk = lambda n, s, d: kvstack.enter_context(nc.sbuf_tensor("sb_" + n, s, d))
            KAT = sbk("KAT", [128, 2, S], BF16)
            VA = sbk("VA", [128, NT, 256], BF16)
            b_kat = [B("kat%d" % j) for j in range(NT)]
            b_va = [B("va%d" % j) for j in range(NT)]
            b_hT = [B("hTs%d" % j) for j in range(NT)]
            b_kb = [B("kbs%d" % j) for j in range(NT)]
            b_vb = [B("vbs%d" % j) for j in range(NT)]

            with ExitStack() as p1:
                s1 = lambda n, s, d: p1.enter_context(nc.sbuf_tensor("sb_" + n, s, d))
                NB1 = 3
                g_bc = s1("g_bc", [128, D], F32); b_g = B("g_bc")
                wkv = s1("wkv", [128, KC, 1024], BF16); b_wkv = B("wkv")
                xt = [s1("xt%d" % i, [128, D], F32) for i in range(NB1)]; b_xt = [B("xt%d" % i) for i in range(NB1)]
                junk = s1("junk", [128, D], BF16); b_junk = B("junk")
                hb = [s1("hb%d" % i, [128, D], BF16) for i in range(NB1)]; b_hb = [B("hb%d" % i) for i in range(NB1)]
                hTt = [s1("hTt%d" % i, [128, KC, 128], BF16) for i in range(NB1)]; b_hTt = [B("hTt%d" % i) for i in range(NB1)]
                st = [s1("st%d" % i, [128, 16], F32) for i in range(NB1)]; b_st = [B("st%d" % i) for i in range(NB1)]
                ksb_ = [s1("ksb%d" % i, [128, 256], F32) for i in range(NB1)]; ksq_ = [s1("ksq%d" % i, [128, 256], F32) for i in range(NB1)]
                knm_ = [s1("knm%d" % i, [128, 2, 128], F32) for i in range(NB1)]
                kt1_ = [s1("kt1%d" % i, [128, 2, 128], F32) for i in range(NB1)]; kt2_ = [s1("kt2%d" % i, [128, 2, 128], F32) for i in range(NB1)]
                krot_ = [s1("krot%d" % i, [128, 2, 128], BF16) for i in range(NB1)]; b_k_ = [B("kscratch%d" % i) for i in range(NB1)]
                rC = [s1("rC%d" % i, [128, 128], F32) for i in range(NB1)]; rS = [s1("rS%d" % i, [128, 128], F32) for i in range(NB1)]
                b_rope = [B("rope%d" % i) for i in range(NB1)]
                kbb_ = [s1("kbb%d" % i, [128, 256], BF16) for i in range(NB1)]; b_kbb_ = [B("kbb%d" % i) for i in range(NB1)]
                kbTt = [s1("kbTt%d" % i, [128, 2, 128], BF16) for i in range(NB1)]; b_kbTt = [B("kbTt%d" % i) for i in range(NB1)]
                vbb = [s1("vbb%d" % i, [128, 256], BF16) for i in range(NB1)]; b_vbb = [B("vbb%d" % i) for i in range(NB1)]
                rot_t = Rot([0, 1]); rot_m = Rot([2, 3, 4, 5]); rot_k = Rot([6, 7])

                T.op("sp", lambda e: e.dma_start(out=g_bc[:], in_=g_mix.partition_broadcast(128)), writes=[b_g], dma=True)
                for hh, c0 in enumerate((1024, 2560)):
                    T.op("sp", lambda e, hh=hh, c0=c0: e.dma_start(
                        out=wkv[:, :, hh * 512:(hh + 1) * 512],
                        in_=w_in_bf[:, c0:c0 + 512].rearrange("(k p) c -> p k c", p=128)),
                        reads=b_win, writes=[b_wkv], dma=True)

                def p1_head(j):
                        i2 = j % NB1
                        X, bX = xt[i2], b_xt[i2]
                        ksb, ksq, knm, kt1, kt2, krot, b_k = ksb_[i2], ksq_[i2], knm_[i2], kt1_[i2], kt2_[i2], krot_[i2], b_k_[i2]
                        kbb, b_kbb = kbb_[i2], b_kbb_[i2]
                        T.op("sp", lambda e, X=X, j=j: e.dma_start(out=X[:], in_=x[j * 128:(j + 1) * 128, :]), writes=[bX], dma=True)
                        T.op("sp", lambda e, j=j, i2=i2: e.dma_start(out=rC[i2][:], in_=ropeC[j * 128:(j + 1) * 128, :]),
                             writes=[b_rope[i2]], dma=True)
                        T.op("sp", lambda e, j=j, i2=i2: e.dma_start(out=rS[i2][:], in_=ropeS[j * 128:(j + 1) * 128, :]),
                             writes=[b_rope[i2]], dma=True)
                        ST, bST = st[i2], b_st[i2]
                        T.op("act", lambda e, X=X, ST=ST: e.activation(out=junk[:], in_=X[:], func=AF.Square, accum_out=ST[:, 0:1]),
                             reads=[bX], writes=[b_junk, bST])
                        rstd_from_ss(ST[:, 0:1], ST[:, 1:2], ST[:, 2:3], 1, 1.0 / D, [bST])
                        H, bH = hb[i2], b_hb[i2]
                        T.op("dve", lambda e, H=H, X=X, ST=ST: e.scalar_tensor_tensor(
                            out=H[:], in0=X[:], scalar=ST[:, 2:3], in1=g_bc[:], op0=ALU.mult, op1=ALU.mult),
                            reads=[bX, bST, b_g], writes=[bH])
                        HT, bHT = hTt[i2], b_hTt[i2]
                        for q4 in range(4):
                            pb, pbb = rot_t.get()
                            pv = pb[:].bitcast(BF16)
                            for kk in range(4):
                                k = q4 * 4 + kk
                                T.op("pe", lambda e, pv=pv, kk=kk, H=H, k=k: e.transpose(
                                    out=pv[:, kk * 128:(kk + 1) * 128], in_=H[:, k * 128:(k + 1) * 128], identity=ident[:]),
                                    reads=[bH, b_const], writes=[pbb])
                            ce = "act" if q4 % 2 == 0 else "dve"
                            if ce == "act":
                                T.op("act", lambda e, HT=HT, q4=q4, pv=pv: e.copy(
                                    out=HT[:, q4 * 4:(q4 + 1) * 4, :], in_=pv[:, 0:512].rearrange("p (k t) -> p k t", t=128)),
                                    reads=[pbb], writes=[bHT])
                            else:
                                T.op("dve", lambda e, HT=HT, q4=q4, pv=pv: e.tensor_copy(
                                    out=HT[:, q4 * 4:(q4 + 1) * 4, :], in_=pv[:, 0:512].rearrange("p (k t) -> p k t", t=128)),
                                    reads=[pbb], writes=[bHT])
                        T.op("pool", lambda e, HT=HT, j=j: e.dma_start(
                            out=hT_s[j // 4, :, :, (j % 4) * 128:(j % 4 + 1) * 128], in_=HT[:]),
                            reads=[bHT], writes=[b_hT[j]], dma=True)
                        pA, pAb = rot_m.get(); pB, pBb = rot_m.get()
                        for k in range(KC):
                            T.op("pe", lambda e, pA=pA, HT=HT, k=k: e.matmul(pA[:], lhsT=HT[:, k, :], rhs=wkv[:, k, 0:512],
                                                                              start=(k == 0), stop=(k == KC - 1)),
                                 reads=[bHT, b_wkv], writes=[pAb])
                        for k in range(KC):
                            T.op("pe", lambda e, pB=pB, HT=HT, k=k: e.matmul(pB[:], lhsT=HT[:, k, :], rhs=wkv[:, k, 512:1024],
                                                                              start=(k == 0), stop=(k == KC - 1)),
                                 reads=[bHT, b_wkv], writes=[pBb])
                        return (j, i2, X, bX, ksb, ksq, knm, kt1, kt2, krot, b_k, kbb, b_kbb, ST, bST, H, bH, HT, bHT, pA, pAb, pB, pBb)

                def p1_tail(L):
                        (j, i2, X, bX, ksb, ksq, knm, kt1, kt2, krot, b_k, kbb, b_kbb, ST, bST, H, bH, HT, bHT, pA, pAb, pB, pBb) = L
                        T.op("act", lambda e, pA=pA, j=j: e.copy(out=VA[:, j, :], in_=pA[:, 256:512]), reads=[pAb], writes=[b_va[j]])
                        T.op("act", lambda e, pB=pB, kbb=kbb: e.copy(out=kbb[:], in_=pB[:, 0:256]), reads=[pBb], writes=[b_kbb])
                        VB_, bVB_ = vbb[i2], b_vbb[i2]
                        T.op("act", lambda e, pB=pB, VB_=VB_: e.copy(out=VB_[:], in_=pB[:, 256:512]), reads=[pBb], writes=[bVB_])
                        T.op("pool", lambda e, VB_=VB_, j=j: e.dma_start(out=vb_s[j * 128:(j + 1) * 128, :], in_=VB_[:]),
                             reads=[bVB_], writes=[b_vb[j]], dma=True, nd=160)
                        T.op("dve", lambda e, pA=pA, ksb=ksb: e.tensor_copy(out=ksb[:], in_=pA[:, 0:256]), reads=[pAb], writes=[b_k])
                        T.op("dve", lambda e, ksq=ksq, ksb=ksb: e.tensor_tensor(out=ksq[:], in0=ksb[:], in1=ksb[:], op=ALU.mult), reads=[b_k], writes=[b_k])
                        T.op("dve", lambda e, ST=ST, ksq=ksq: e.tensor_reduce(out=ST[:, 4:6], in_=ksq[:].rearrange("p (h d) -> p h d", d=128),
                                                                      axis=AX.X, op=ALU.add), reads=[b_k], writes=[bST])
                        rstd_from_ss(ST[:, 4:6], ST[:, 6:8], ST[:, 8:10], 2, 1.0 / 128, [bST])
                        for hh in range(2):
                            T.op("dve", lambda e, hh=hh, ST=ST, knm=knm, ksb=ksb: e.scalar_tensor_tensor(
                                out=knm[:, hh, :], in0=ksb[:, hh * 128:(hh + 1) * 128], scalar=ST[:, 8 + hh:9 + hh], in1=kn_bc[:],
                                op0=ALU.mult, op1=ALU.mult), reads=[b_k, bST, b_const], writes=[b_k])
                        rope(T, knm, kt1, kt2, krot, rC[i2], rS[i2], 2, [b_k, b_rope[i2]], [b_k])
                        pk, pkb = rot_k.get()
                        pkv = pk[:].bitcast(BF16)
                        for hh in range(2):
                            T.op("pe", lambda e, pkv=pkv, hh=hh, krot=krot: e.transpose(out=pkv[:, hh * 128:(hh + 1) * 128], in_=krot[:, hh, :],
                                                                             identity=ident[:]), reads=[b_k, b_const], writes=[pkb])
                        for hh in range(2):
                            T.op("pe", lambda e, pkv=pkv, hh=hh, kbb=kbb: e.transpose(out=pkv[:, 256 + hh * 128:256 + (hh + 1) * 128],
                                                                             in_=kbb[:, hh * 128:(hh + 1) * 128], identity=ident[:]),
                                 reads=[b_kbb, b_const], writes=[pkb])
                        T.op("dve", lambda e, pkv=pkv, j=j: e.tensor_copy(
                            out=KAT[:, :, j * 128:(j + 1) * 128], in_=pkv[:, 0:256].rearrange("p (h t) -> p h t", t=128)),
                            reads=[pkb], writes=[b_kat[j]])
                        KBT, bKBT = kbTt[i2], b_kbTt[i2]
                        T.op("act", lambda e, pkv=pkv, KBT=KBT: e.copy(out=KBT[:], in_=pkv[:, 256:512].rearrange("p (h t) -> p h t", t=128)),
                             reads=[pkb], writes=[bKBT])
                        T.op("pool", lambda e, KBT=KBT, j=j: e.dma_start(
                            out=kbT_s[:, :, j * 128:(j + 1) * 128].rearrange("h p t -> p h t"), in_=KBT[:]),
                            reads=[bKBT], writes=[b_kb[j]], dma=True, nd=300)

                Lp = p1_head(0)
                for j in range(nt1):
                    Ln_ = p1_head(j + 1) if j + 1 < nt1 else None
                    p1_tail(Lp)
                    Lp = Ln_
                T.sync_all()
                if stop == 1:
                    return nc

            b_oT = [B("oT%d" % i) for i in range(16)]
            with ExitStack() as p2:
                s2 = lambda n, s, d: p2.enter_context(nc.sbuf_tensor("sb_" + n, s, d))
                hTb = s2("hTb", [128, KC, 512], BF16); b_hTb = B("hTb")
                wq = [s2("wq%d" % i, [128, KC, 512], BF16) for i in range(2)]; b_wq = [B("wq0"), B("wq1")]
                qAT = s2("qAT", [128, 8, 512], BF16); b_qAT = B("qAT")
                qBT = s2("qBT", [128, 8, 512], BF16); b_qBT = B("qBT")
                oAT = s2("oAT", [128, 8, 512], BF16); b_oAT = B("oAT")
                oBT = s2("oBT", [128, 8, 512], BF16); b_oBT = B("oBT")
                biasB = s2("biasB", [128, 8, 384], F32); b_bias = B("biasB")
                emk = s2("emk", [128, 384], F32); b_emk = B("emk")
                qsb_ = [s2("qsb%d" % i, [128, 512], F32) for i in range(2)]; qnm_ = [s2("qnm%d" % i, [128, 4, 128], F32) for i in range(2)]
                qt1_ = [s2("qt1%d" % i, [128, 4, 128], F32) for i in range(2)]; qt2_ = [s2("qt2%d" % i, [128, 4, 128], F32) for i in range(2)]
                qrot_ = [s2("qrot%d" % i, [128, 4, 128], BF16) for i in range(2)]
                b_q_ = [B("qscratch0"), B("qscratch1")]
                st2_ = [s2("st2%d" % i, [128, 16], F32) for i in range(2)]; b_st2_ = [B("st20"), B("st21")]
                qunit = 0
                rC2 = [s2("rC2%d" % i, [128, 128], F32) for i in range(2)]; rS2 = [s2("rS2%d" % i, [128, 128], F32) for i in range(2)]
                b_rope2 = [B("rope20"), B("rope21")]
                PT = [s2("PT%d" % i, [128, 512], BF16) for i in range(4)]; b_PT = [B("PT%d" % i) for i in range(4)]
                rD = [s2("rD0", [128, 512], F32)] * 2; b_rD = [B("rD0")] * 2
                kbw = s2("kbw", [128, 2, 768], BF16); vbw = s2("vbw", [128, 6, 256], BF16); b_kbw = B("kbw"); b_vbw = B("vbw")
                ssb = s2("ssb", [128, 4, 384], F32); pbf = s2("pbf", [128, 4, 384], BF16)
                pTs = s2("pTs", [128, 12, 128], BF16); b_bs = B("b_s"); b_be = B("b_e"); b_bp = B("b_p"); b_bpT = B("b_pT")
                stB = s2("stB", [128, 32], F32); b_stB = B("stB")

                T.op("sp", lambda e: e.dma_start(out=biasB[:], in_=biasB_d), writes=[b_bias], dma=True)
                T.op("sp", lambda e: e.dma_start(out=emk[:], in_=band_d), writes=[b_emk], dma=True)
                T.op("dve", lambda e: e.tensor_tensor(out=biasB[:], in0=biasB[:], in1=emk[:].unsqueeze(1).to_broadcast([128, 8, 384]),
                                                      op=ALU.add), reads=[b_bias, b_emk], writes=[b_bias])
                rot_q = Rot([6, 7]); rot_s = Rot([0, 1, 2])

                for bi in range(16):
                    t0 = bi * 512
                    if bi == 0:
                        T.op("sp", lambda e: e.dma_start(out=hTb[:], in_=hT_s[0]), reads=b_hT[0:4], writes=[b_hTb], dma=True)
                        T.op("sp", lambda e: e.dma_start(out=wq[0][:], in_=wq_s[0]), reads=[b_wqs[0]], writes=[b_wq[0]], dma=True)
                        T.op("sp", lambda e: e.dma_start(out=wq[1][:], in_=wq_s[2]), reads=[b_wqs[2]], writes=[b_wq[1]], dma=True)
                    jl = (bi * 4 - 1) % NT
                    jr = (bi * 4 + 4) % NT
                    segs = [(0, jl, 1), (1, bi * 4, 4), (5, jr, 1)]
                    for (w0, j0, n) in segs:
                        T.op("sp", lambda e, w0=w0, j0=j0, n=n: e.dma_start(
                            out=kbw[:, :, w0 * 128:(w0 + n) * 128],
                            in_=kbT_s[:, :, j0 * 128:(j0 + n) * 128].rearrange("h p t -> p h t")),
                            reads=b_kb[j0:j0 + n], writes=[b_kbw], dma=True)
                        T.op("sp", lambda e, w0=w0, j0=j0, n=n: e.dma_start(
                            out=vbw[:, w0:w0 + n, :],
                            in_=vb_s[j0 * 128:(j0 + n) * 128, :].rearrange("(t p) c -> p t c", p=128)),
                            reads=b_vb[j0:j0 + n], writes=[b_vbw], dma=True)
                    pendq = None

                    def finish_unit(u):
                        (qrot_u, b_q_u, cg_u, tt_u) = u
                        pt, ptb = rot_s.get()
                        ptv = pt[:].bitcast(BF16)
                        for hh in range(4):
                            T.op("pe", lambda e, ptv=ptv, hh=hh: e.transpose(out=ptv[:, hh * 128:(hh + 1) * 128], in_=qrot_u[:, hh, :],
                                                                             identity=ident[:]), reads=[b_q_u, b_const], writes=[ptb])
                        T.op("dve", lambda e, ptv=ptv: e.tensor_copy(
                            out=qAT[:, cg_u * 4:(cg_u + 1) * 4, tt_u * 128:(tt_u + 1) * 128],
                            in_=ptv[:, 0:512].rearrange("p (h t) -> p h t", t=128)), reads=[ptb], writes=[b_qAT])

                    rot_qb = Rot([3, 4])
                    for cg in range(2):
                        WA, bWA = wq[0], b_wq[0]
                        WB, bWB = wq[1], b_wq[1]
                        if cg == 1:
                            T.op("sp", lambda e: e.dma_start(out=WA[:], in_=wq_s[1]), reads=[b_wqs[1]], writes=[bWA], dma=True)
                            T.op("sp", lambda e: e.dma_start(out=WB[:], in_=wq_s[3]), reads=[b_wqs[3]], writes=[bWB], dma=True)
                        W, bW = WA, bWA
                        for tt in range(4):
                            j = bi * 4 + tt
                            i2 = (cg * 4 + tt) % 2
                            T.op("sp", lambda e, j=j, i2=i2: e.dma_start(out=rC2[i2][:], in_=ropeC[j * 128:(j + 1) * 128, :]),
                                 writes=[b_rope2[i2]], dma=True)
                            T.op("sp", lambda e, j=j, i2=i2: e.dma_start(out=rS2[i2][:], in_=ropeS[j * 128:(j + 1) * 128, :]),
                                 writes=[b_rope2[i2]], dma=True)
                            us = qunit % 2; qunit += 1
                            qsb, qnm, qt1, qt2, qrot, b_q, st2, b_st2 = qsb_[us], qnm_[us], qt1_[us], qt2_[us], qrot_[us], b_q_[us], st2_[us], b_st2_[us]
                            pq, pqb = rot_q.get()
                            for k in range(KC):
                                T.op("pe", lambda e, pq=pq, k=k, tt=tt, W=W: e.matmul(
                                    pq[:], lhsT=hTb[:, k, tt * 128:(tt + 1) * 128], rhs=W[:, k, :], start=(k == 0), stop=(k == KC - 1)),
                                    reads=[b_hTb, bW], writes=[pqb])
                            if pendq is not None:
                                finish_unit(pendq)
                            for hh in range(4):
                                T.op("act", lambda e, pq=pq, hh=hh: e.activation(out=qsb[:, hh * 128:(hh + 1) * 128], in_=pq[:, hh * 128:(hh + 1) * 128],
                                                                                 func=AF.Square, accum_out=st2[:, hh:hh + 1]),
                                     reads=[pqb], writes=[b_q, b_st2])
                            T.op("dve", lambda e: e.tensor_scalar(out=st2[:, 4:8], in0=st2[:, 0:4], scalar1=1.0 / 128, scalar2=EPS,
                                                                  op0=ALU.mult, op1=ALU.add), reads=[b_st2], writes=[b_st2])
                            T.op("act", lambda e: e.activation(out=st2[:, 4:8], in_=st2[:, 4:8], func=AF.Ln), reads=[b_st2], writes=[b_st2])
                            T.op("act", lambda e: e.activation(out=st2[:, 8:12], in_=st2[:, 4:8], func=AF.Exp, scale=-0.5), reads=[b_st2], writes=[b_st2])
                            for hh in range(4):
                                T.op("dve", lambda e, hh=hh, pq=pq: e.scalar_tensor_tensor(
                                    out=qnm[:, hh, :], in0=pq[:, hh * 128:(hh + 1) * 128], scalar=st2[:, 8 + hh:9 + hh], in1=qn_bc[:],
                                    op0=ALU.mult, op1=ALU.mult), reads=[pqb, b_st2, b_const], writes=[b_q])
                            rope(T, qnm, qt1, qt2, qrot, rC2[i2], rS2[i2], 4, [b_q, b_rope2[i2]], [b_q])
                            pendq = (qrot, b_q, cg, tt)
                            hB = cg * 4 + tt
                            pqB, pqBb = rot_qb.get()
                            for k in range(KC):
                                T.op("pe", lambda e, pqB=pqB, k=k, tt=tt: e.matmul(
                                    pqB[:], lhsT=WB[:, k, tt * 128:(tt + 1) * 128], rhs=hTb[:, k, :], start=(k == 0), stop=(k == KC - 1)),
                                    reads=[b_hTb, bWB], writes=[pqBb])
                            T.op("act", lambda e, pqB=pqB, hB=hB: e.activation(out=qBT[:, hB, :], in_=pqB[:], func=AF.Copy, scale=SCALE),
                                 reads=[pqBb], writes=[b_qBT])
                    finish_unit(pendq)
                    if bi < 15:
                        T.op("sp", lambda e, bi=bi: e.dma_start(out=hTb[:], in_=hT_s[bi + 1]),
                             reads=b_hT[bi * 4 + 4:bi * 4 + 8], writes=[b_hTb], dma=True)
                        T.op("sp", lambda e: e.dma_start(out=wq[0][:], in_=wq_s[0]), reads=[b_wqs[0]], writes=[b_wq[0]], dma=True)
                        T.op("sp", lambda e: e.dma_start(out=wq[1][:], in_=wq_s[2]), reads=[b_wqs[2]], writes=[b_wq[1]], dma=True)
                    for _ in range(6):
                        if deferred:
                            deferred.pop(0)()
                    cast_expert(bi)
                    pB7, pB7b = banks[7], bankb[7]

                    def b_stages(tt, kvhb):
                        j = bi * 4 + tt
                        special = j in SPECIAL
                        st = []

                        def s_head(hh):
                            h = kvhb * 4 + hh
                            if hh == 0 and special:
                                si = SPECIAL.index(j)
                                T.op("sp", lambda e: e.dma_start(out=emk[:], in_=emask_d[si:si + 1, :].partition_broadcast(128)),
                                     writes=[b_emk], dma=True)
                            T.op("pe", lambda e: e.matmul(pB7[:, 0:384], lhsT=qBT[:, h, tt * 128:(tt + 1) * 128],
                                                          rhs=kbw[:, kvhb, tt * 128:tt * 128 + 384], start=True, stop=True),
                                 reads=[b_qBT, b_kbw], writes=[pB7b])
                            T.op("dve", lambda e: e.tensor_tensor(out=ssb[:, hh, :], in0=pB7[:, 0:384], in1=biasB[:, h, :], op=ALU.add),
                                 reads=[pB7b, b_bias], writes=[b_bs])
                        for hh in range(4):
                            st.append(lambda hh=hh: s_head(hh))

                        def chain():
                            if special:
                                T.op("dve", lambda e: e.tensor_tensor(out=ssb[:], in0=ssb[:], in1=emk[:].unsqueeze(1).to_broadcast([128, 4, 384]),
                                                                      op=ALU.add), reads=[b_bs, b_emk], writes=[b_bs])
                            T.op("dve", lambda e: e.tensor_reduce(out=stB[:, 0:4], in_=ssb[:], axis=AX.X, op=ALU.max, negate=True),
                                 reads=[b_bs], writes=[b_stB])
                            T.op("dve", lambda e: e.tensor_tensor(out=stB[:, 4:8], in0=stB[:, 0:4], in1=nsink_bc[:, kvhb * 4:kvhb * 4 + 4],
                                                                  op=ALU.min), reads=[b_stB, b_const], writes=[b_stB])
                            for hh in range(4):
                                T.op("act", lambda e, hh=hh: e.activation(out=ssb[:, hh, :], in_=ssb[:, hh, :], func=AF.Exp,
                                                                          bias=stB[:, 4 + hh:5 + hh], scale=1.0, accum_out=stB[:, 8 + hh:9 + hh]),
                                     reads=[b_bs, b_stB], writes=[b_bs, b_stB])
                            T.op("dve", lambda e: e.tensor_tensor(out=stB[:, 12:16], in0=stB[:, 4:8], in1=sink_bc[:, kvhb * 4:kvhb * 4 + 4],
                                                                  op=ALU.add), reads=[b_stB, b_const], writes=[b_stB])
                            T.op("act", lambda e: e.activation(out=stB[:, 12:16], in_=stB[:, 12:16], func=AF.Exp), reads=[b_stB], writes=[b_stB])
                            T.op("dve", lambda e: e.tensor_tensor(out=stB[:, 16:20], in0=stB[:, 8:12], in1=stB[:, 12:16], op=ALU.add),
                                 reads=[b_stB], writes=[b_stB])
                            T.op("dve", lambda e: e.reciprocal(out=stB[:, 20:24], in_=stB[:, 16:20]), reads=[b_stB], writes=[b_stB])
                            T.op("dve", lambda e: e.tensor_tensor(out=pbf[:], in0=ssb[:], in1=stB[:, 20:24].unsqueeze(2).to_broadcast([128, 4, 384]),
                                                                  op=ALU.mult), reads=[b_bs, b_stB], writes=[b_bp])
                        st.append(chain)

                        def transposes(half):
                            ptv = pB7[:].bitcast(BF16)
                            for hq in range(2):
                                hh = half * 2 + hq
                                for jj in range(3):
                                    T.op("pe", lambda e, hq=hq, hh=hh, jj=jj: e.transpose(
                                        out=ptv[:, (hq * 3 + jj) * 128:(hq * 3 + jj + 1) * 128], in_=pbf[:, hh, jj * 128:(jj + 1) * 128],
                                        identity=ident[:]), reads=[b_bp, b_const], writes=[pB7b])
                            T.op("dve", lambda e: e.tensor_copy(out=pTs[:, half * 6:half * 6 + 6, :],
                                                                in_=ptv[:, 0:768].rearrange("p (j t) -> p j t", t=128)),
                                 reads=[pB7b], writes=[b_bpT])
                        st.append(lambda: transposes(0))
                        st.append(lambda: transposes(1))

                        def pv():
                            for hh in range(4):
                                for jj in range(3):
                                    T.op("pe", lambda e, hh=hh, jj=jj: e.matmul(
                                        pB7[:, hh * 128:(hh + 1) * 128], lhsT=vbw[:, tt + jj, kvhb * 128:(kvhb + 1) * 128],
                                        rhs=pTs[:, hh * 3 + jj, :], start=(jj == 0), stop=(jj == 2), skip_group_check=True),
                                        reads=[b_vbw, b_bpT], writes=[pB7b])
                            T.op("dve", lambda e: e.tensor_copy(
                                out=oBT[:, kvhb * 4:(kvhb + 1) * 4, tt * 128:(tt + 1) * 128], in_=pB7[:].rearrange("p (h t) -> p h t", t=128)),
                                reads=[pB7b], writes=[b_oBT])
                        st.append(pv)
                        return st

                    BSLOT = {2: 0, 6: 1, 10: 2, 14: 3, 15: 4, 40: 5, 46: 6, 54: 7}
                    for h in range(8):
                        kvh = h // 4
                        pO, pOb = banks[3 + (h % 2)], bankb[3 + (h % 2)]
                        pD, pDb = banks[5 + (h % 2)], bankb[5 + (h % 2)]
                        pend = []
                        LA = 2
                        bst = b_stages(h % 4, h // 4)
                        for kb in range(NT + LA):
                            if kb in BSLOT:
                                bst[BSLOT[kb]]()
                            if kb < NT:
                                ps, psb = rot_s.get()
                                T.op("pe", lambda e, ps=ps, kvh=kvh, kb=kb, h=h: e.matmul(
                                    ps[:], lhsT=KAT[:, kvh, kb * 128:(kb + 1) * 128], rhs=qAT[:, h, :], start=True, stop=True),
                                    reads=[b_kat[kb], b_qAT], writes=[psb])
                                P_, bP_ = PT[kb % 4], b_PT[kb % 4]
                                T.op("act", lambda e, P_=P_, ps=ps: e.activation(out=P_[:], in_=ps[:], func=AF.Exp, scale=SCALE),
                                     reads=[psb], writes=[bP_])
                                pend.append((kb, P_, bP_))
                            if kb >= LA:
                                (kb0, P0, bP0) = pend.pop(0)
                                T.op("pe", lambda e, pO=pO, kb0=kb0, kvh=kvh, P0=P0: e.matmul(
                                    pO[:], lhsT=VA[:, kb0, kvh * 128:(kvh + 1) * 128], rhs=P0[:], start=(kb0 == 0), stop=(kb0 == NT - 1)),
                                    reads=[b_va[kb0], bP0], writes=[pOb])
                                T.op("pe", lambda e, pD=pD, kb0=kb0, P0=P0: e.matmul(
                                    pD[:], lhsT=ones[:], rhs=P0[:], start=(kb0 == 0), stop=(kb0 == NT - 1)),
                                    reads=[b_const, bP0], writes=[pDb])
                        R_, bR_ = rD[h % 2], b_rD[h % 2]
                        T.op("dve", lambda e, R_=R_, pD=pD: e.reciprocal(out=R_[:], in_=pD[:]), reads=[pDb], writes=[bR_])
                        T.op("dve", lambda e, R_=R_, pO=pO, h=h: e.tensor_tensor(out=oAT[:, h, :], in0=pO[:], in1=R_[:], op=ALU.mult),
                             reads=[pOb, bR_], writes=[b_oAT])
                    T.op("pool", lambda e, t0=t0: e.dma_start(out=oT_s[0, bi], in_=oAT[:]),
                         reads=[b_oAT], writes=[b_oT[bi]], dma=True)
                    T.op("pool", lambda e, t0=t0: e.dma_start(out=oT_s[1, bi], in_=oBT[:]),
                         reads=[b_oBT], writes=[b_oT[bi]], dma=True)
                while deferred:
                    deferred.pop(0)()
                T.sync_all()
                if stop == 2:
                    return nc

        b_x1 = [B("x1_%d" % j) for j in range(NOWN)]
        b_h2 = [B("h2_%d" % j) for j in range(NOWN)]
        with ExitStack() as p3:
            s3 = lambda n, s, d: p3.enter_context(nc.sbuf_tensor("sb_" + n, s, d))
            g_bc = s3("g_bc2", [128, D], F32); b_g = B("g_bc2")
            wrs = s3("wrs", [128, KC, NE], BF16); b_wrs = B("wrs")
            hTb = s3("hTb2", [128, KC, 512], BF16); b_hTb = B("hTb2")
            oAT = s3("oAT2", [128, 8, 512], BF16); oBT = s3("oBT2", [128, 8, 512], BF16); b_o = B("o2")
            mT = s3("mT", [128, KC, 512], BF16); b_mT = B("mT")
            wgs = [s3("wgs%d" % i, [128, 2, KC, 128], BF16) for i in range(3)]
            wps = [s3("wps%d" % i, [128, 2, 8, 128], BF16) for i in range(3)]
            b_ws = [B("ws%d" % i) for i in range(3)]
            wos = [s3("wos%d" % i, [128, KC, 512], BF16) for i in range(2)]; b_wos = [B("wos0"), B("wos1")]
            gsb = [s3("gsb%d" % i, [128, 512], F32) for i in range(2)]; b_gsb = [B("gsb0"), B("gsb1")]
            m1 = s3("m1", [128, 512], F32); b_m1 = B("m1")
            x1blk = [s3("x1blk%d" % i, [128, D], F32) for i in range(4)]; b_x1blk = [B("x1blk%d" % i) for i in range(4)]
            junk = s3("junk2", [128, D], BF16); b_junk = B("junk2")
            h2b = [s3("h2b%d" % i, [128, D], BF16) for i in range(2)]; b_h2b = [B("h2b0"), B("h2b1")]
            h2T = s3("h2T", [128, KC, 128], BF16); b_h2T = B("h2T")
            st = s3("st3", [128, 16], F32); b_st = B("st3")
            lg = s3("lg", [128, NE], F32); b_lg = B("lg")
            rot_a = Rot([0, 1]); rot_b = Rot([2, 3]); rot_o = Rot([4, 5]); rot_t = Rot([6, 7])

            T.op("sp", lambda e: e.dma_start(out=g_bc[:], in_=g_ffn.partition_broadcast(128)), writes=[b_g], dma=True)
            T.op("sp", lambda e: e.dma_start(out=wrs[:], in_=wr_bf.rearrange("(k p) c -> p k c", p=128)), reads=[b_wr], writes=[b_wrs], dma=True)
            nslot = 0
            for bi in range(16):
                t0 = bi * 512
                T.op("sp", lambda e, t0=t0: e.dma_start(out=hTb[:], in_=hT_s[bi]),
                     reads=b_hT[bi * 4:bi * 4 + 4], writes=[b_hTb], dma=True)
                T.op("sp", lambda e, t0=t0: e.dma_start(out=oAT[:], in_=oT_s[0, bi]),
                     reads=[b_oT[bi]], writes=[b_o], dma=True)
                T.op("sp", lambda e, t0=t0: e.dma_start(out=oBT[:], in_=oT_s[1, bi]),
                     reads=[b_oT[bi]], writes=[b_o], dma=True)
                for n in range(KC):
                    sl = nslot % 3; nslot += 1
                    WG, WP, bW = wgs[sl], wps[sl], b_ws[sl]
                    for ab in range(2):
                        T.op("sp", lambda e, WG=WG, ab=ab, n=n: e.dma_start(out=WG[:, ab], in_=wgt_s[ab * 16 + n]),
                             reads=[b_wgt[ab * 16 + n]], writes=[bW], dma=True)
                        T.op("sp", lambda e, WP=WP, ab=ab, n=n: e.dma_start(out=WP[:, ab], in_=wp_s[ab, n]),
                             reads=[b_wp[ab * 16 + n]], writes=[bW], dma=True)
                    for ab in range(2):
                        pg, pgb = rot_a.get()
                        for k in range(KC):
                            T.op("pe", lambda e, pg=pg, WG=WG, ab=ab, k=k: e.matmul(pg[:], lhsT=WG[:, ab, k, :], rhs=hTb[:, k, :],
                                                                                    start=(k == 0), stop=(k == KC - 1)),
                                 reads=[bW, b_hTb], writes=[pgb])
                        G_, bG_ = gsb[ab], b_gsb[ab]
                        T.op("act", lambda e, G_=G_, pg=pg, ab=ab, n=n: e.activation(out=G_[:], in_=pg[:], func=AF.Sigmoid,
                                                                                     bias=bgate[:, ab * 16 + n:ab * 16 + n + 1], scale=1.0),
                             reads=[pgb, b_const], writes=[bG_])
                        pp, ppb = rot_b.get()
                        osrc = oAT if ab == 0 else oBT
                        for hh in range(8):
                            T.op("pe", lambda e, pp=pp, WP=WP, ab=ab, hh=hh, osrc=osrc: e.matmul(
                                pp[:], lhsT=WP[:, ab, hh, :], rhs=osrc[:, hh, :], start=(hh == 0), stop=(hh == 7)),
                                reads=[bW, b_o], writes=[ppb])
                        if ab == 0:
                            T.op("dve", lambda e, pp=pp, G_=G_: e.tensor_tensor(out=m1[:], in0=pp[:], in1=G_[:], op=ALU.mult),
                                 reads=[ppb, bG_], writes=[b_m1])
                        else:
                            T.op("dve", lambda e, pp=pp, G_=G_: e.tensor_tensor(out=G_[:], in0=pp[:], in1=G_[:], op=ALU.mult),
                                 reads=[ppb, bG_], writes=[bG_])
                            T.op("dve", lambda e, G_=G_, n=n: e.tensor_tensor(out=mT[:, n, :], in0=m1[:], in1=G_[:], op=ALU.add),
                                 reads=[b_m1, bG_], writes=[b_mT])
                for tt in range(4):
                    j = bi * 4 + tt
                    T.op("sp", lambda e, tt=tt, j=j: e.dma_start(out=x1blk[tt][:], in_=x[j * 128:(j + 1) * 128, :]),
                         writes=[b_x1blk[tt]], dma=True)
                for cg in range(4):
                    WO, bWO = wos[cg % 2], b_wos[cg % 2]
                    T.op("sp", lambda e, WO=WO, cg=cg: e.dma_start(
                        out=WO[:], in_=wo_s[cg]), reads=[b_wosc[cg]], writes=[bWO], dma=True)
                    for tt in range(4):
                        po, pob = rot_o.get()
                        for k in range(KC):
                            T.op("pe", lambda e, po=po, k=k, tt=tt, WO=WO: e.matmul(
                                po[:], lhsT=mT[:, k, tt * 128:(tt + 1) * 128], rhs=WO[:, k, :], start=(k == 0), stop=(k == KC - 1)),
                                reads=[b_mT, bWO], writes=[pob])
                        T.op("dve", lambda e, po=po, tt=tt, cg=cg: e.tensor_tensor(
                            out=x1blk[tt][:, cg * 512:(cg + 1) * 512], in0=po[:], in1=x1blk[tt][:, cg * 512:(cg + 1) * 512], op=ALU.add),
                            reads=[pob, b_x1blk[tt]], writes=[b_x1blk[tt]])
                for tt in range(4):
                    j = bi * 4 + tt
                    XB, bXB = x1blk[tt], b_x1blk[tt]
                    if j < NOWN:
                        T.op("pool", lambda e, XB=XB, j=j: e.dma_start(out=x1_s[j * 128:(j + 1) * 128, :], in_=XB[:]),
                             reads=[bXB], writes=[b_x1[j]], dma=True)
                    T.op("act", lambda e, XB=XB: e.activation(out=junk[:], in_=XB[:], func=AF.Square, accum_out=st[:, 0:1]),
                         reads=[bXB], writes=[b_junk, b_st])
                    rstd_from_ss(st[:, 0:1], st[:, 1:2], st[:, 2:3], 1, 1.0 / D, [b_st])
                    H2, bH2 = h2b[tt % 2], b_h2b[tt % 2]
                    T.op("dve", lambda e, H2=H2, XB=XB: e.scalar_tensor_tensor(out=H2[:], in0=XB[:], scalar=st[:, 2:3], in1=g_bc[:],
                                                                                op0=ALU.mult, op1=ALU.mult),
                         reads=[bXB, b_st, b_g], writes=[bH2])
                    if j < NOWN:
                        T.op("pool", lambda e, H2=H2, j=j: e.dma_start(out=h2_s[j * 128:(j + 1) * 128, :], in_=H2[:]),
                             reads=[bH2], writes=[b_h2[j]], dma=True)
                    for q4 in range(4):
                        pb, pbb = rot_t.get()
                        pv = pb[:].bitcast(BF16)
                        for kk in range(4):
                            k = q4 * 4 + kk
                            T.op("pe", lambda e, pv=pv, kk=kk, H2=H2, k=k: e.transpose(
                                out=pv[:, kk * 128:(kk + 1) * 128], in_=H2[:, k * 128:(k + 1) * 128], identity=ident[:]),
                                reads=[bH2, b_const], writes=[pbb])
                        T.op("act", lambda e, q4=q4, pv=pv: e.copy(out=h2T[:, q4 * 4:(q4 + 1) * 4, :],
                                                                   in_=pv[:, 0:512].rearrange("p (k t) -> p k t", t=128)),
                             reads=[pbb], writes=[b_h2T])
                    pl, plb = rot_t.get()
                    for k in range(KC):
                        T.op("pe", lambda e, pl=pl, k=k: e.matmul(pl[:, 0:NE], lhsT=h2T[:, k, :], rhs=wrs[:, k, :],
                                                                  start=(k == 0), stop=(k == KC - 1)),
                             reads=[b_h2T, b_wrs], writes=[plb])
                    T.op("dve", lambda e, pl=pl: e.tensor_reduce(out=st[:, 4:5], in_=pl[:, 0:NE], axis=AX.X, op=ALU.max, negate=True),
                         reads=[plb], writes=[b_st])
                    T.op("act", lambda e, pl=pl: e.activation(out=lg[:], in_=pl[:, 0:NE], func=AF.Exp, bias=st[:, 4:5], scale=1.0,
                                                              accum_out=st[:, 5:6]), reads=[plb, b_st], writes=[b_lg, b_st])
                    T.op("dve", lambda e: e.reciprocal(out=st[:, 6:7], in_=st[:, 5:6]), reads=[b_st], writes=[b_st])
                    T.op("dve", lambda e, j=j: e.tensor_scalar(out=aff[:, j, :], in0=lg[:], scalar1=st[:, 6:7], scalar2=None, op0=ALU.mult),
                         reads=[b_lg, b_st], writes=[b_aff[j]])
            T.sync_all()
            if "aff_dbg" in dbg:
                aff_dbg = nc.dram_tensor("aff_dbg", [128, NT * NE], F32, kind="ExternalOutput").ap()
                T.op("sp", lambda e: e.dma_start(out=aff_dbg, in_=aff[:].rearrange("p j e -> p (j e)")), reads=b_aff, writes=[B("affd")], dma=True)
                T.sync_all()
            if stop == 3:
                return nc

        b_Xg = [B("Xg%d" % i) for i in range(NE)]
        b_Yg = [B("Yg%d" % i) for i in range(NE)]
        with ExitStack() as p4:
            s4 = lambda n, s, d: p4.enter_context(nc.sbuf_tensor("sb_" + n, s, d))
            gt = s4("gt", [128, NOWN, NE], F32); b_gt = B("gt")
            idx_i = s4("idx_i", [128, NOWN * NE], I32); b_idx = B("idx")
            bc_reg = nc.gpsimd.to_reg(CAP - 1)
            with ExitStack() as p4a:
                sa = lambda n, s, d: p4a.enter_context(nc.sbuf_tensor("sb_" + n, s, d))
                cmp_ = sa("cmp", [128, NT, NE], F32); b_cmp = B("cmp")
                lo = sa("lo", [128, NE], F32); mid = sa("mid", [128, NE], F32); ge = sa("ge", [128, NE], F32)
                cntp = sa("cntp", [128, NE], BF16); b_bis = B("bis")
                msk = sa("msk", [128, NOWN, NE], F32); mskb = sa("mskb", [128, NOWN * NE], BF16); b_msk = B("msk")
                cc = sa("cc", [128, NOWN, NE], F32); offs = sa("offs", [128, NOWN, NE], F32); b_offs = B("offs")
                posf = sa("posf", [128, NOWN * NE], F32); tmpf = sa("tmpf", [128, NOWN * NE], F32); b_pos = B("pos")
                pc, pcb = banks[0], bankb[0]
                T.op("dve", lambda e: e.memset(lo[:], 0.0), writes=[b_bis])
                for it in range(NBISECT):
                    c_ = 2.0 ** (-(it + 1))
                    T.op("dve", lambda e, c_=c_: e.tensor_scalar(out=mid[:], in0=lo[:], scalar1=c_, scalar2=None, op0=ALU.add),
                         reads=[b_bis], writes=[b_bis])
                    T.op("dve", lambda e: e.tensor_tensor(out=cmp_[:], in0=aff[:], in1=mid[:].unsqueeze(1).to_broadcast([128, NT, NE]),
                                                          op=ALU.is_ge), reads=b_aff + [b_bis], writes=[b_cmp])
                    with nc.allow_low_precision(reason="integer counts <= 64 are exact in bf16"):
                        T.op("dve", lambda e: e.tensor_reduce(out=cntp[:], in_=cmp_[:].rearrange("p j e -> p e j"), axis=AX.X, op=ALU.add),
                             reads=[b_cmp], writes=[b_bis])
                    T.op("pe", lambda e: e.matmul(pc[:, 0:NE], lhsT=ones[:], rhs=cntp[:], start=True, stop=True),
                         reads=[b_bis, b_const], writes=[pcb])
                    T.op("dve", lambda e: e.tensor_scalar(out=ge[:], in0=pc[:, 0:NE], scalar1=float(CAP) - 0.5, scalar2=None, op0=ALU.is_ge),
                         reads=[pcb], writes=[b_bis])
                    T.op("dve", lambda e, c_=c_: e.scalar_tensor_tensor(out=lo[:], in0=ge[:], scalar=c_, in1=lo[:], op0=ALU.mult, op1=ALU.add),
                         reads=[b_bis], writes=[b_bis])
                T.op("dve", lambda e: e.tensor_tensor(out=msk[:], in0=aff[:, 0:NOWN, :], in1=lo[:].unsqueeze(1).to_broadcast([128, NOWN, NE]),
                                                      op=ALU.is_ge), reads=b_aff + [b_bis], writes=[b_msk])
                T.op("dve", lambda e: e.tensor_tensor(out=gt[:], in0=aff[:, 0:NOWN, :], in1=msk[:], op=ALU.mult), reads=b_aff + [b_msk], writes=[b_gt])
                T.op("dve", lambda e: e.tensor_copy(out=mskb[:], in_=msk[:].rearrange("p j e -> p (j e)")), reads=[b_msk], writes=[b_msk])
                pp_, ppb_ = banks[1], bankb[1]
                pc2, pc2b = banks[2], bankb[2]
                T.op("pe", lambda e: e.matmul(pp_[:], lhsT=triU[:], rhs=mskb[:], start=True, stop=True), reads=[b_msk, b_const], writes=[ppb_])
                T.op("pe", lambda e: e.matmul(pc2[:], lhsT=ones[:], rhs=mskb[:], start=True, stop=True), reads=[b_msk, b_const], writes=[pc2b])
                T.op("act", lambda e: e.copy(out=cc[:], in_=pc2[:].rearrange("p (j e) -> p j e", e=NE)), reads=[pc2b], writes=[b_offs])
                T.op("dve", lambda e: e.memset(offs[:, 0, :], 0.0), reads=[b_offs], writes=[b_offs])
                for j in range(1, NOWN):
                    T.op("dve", lambda e, j=j: e.tensor_tensor(out=offs[:, j, :], in0=offs[:, j - 1, :], in1=cc[:, j - 1, :], op=ALU.add),
                         reads=[b_offs], writes=[b_offs])
                T.op("dve", lambda e: e.tensor_tensor(out=posf[:], in0=pp_[:], in1=offs[:].rearrange("p j e -> p (j e)"), op=ALU.add),
                     reads=[ppb_, b_offs], writes=[b_pos])
                T.op("dve", lambda e: e.tensor_scalar(out=tmpf[:], in0=msk[:].rearrange("p j e -> p (j e)"), scalar1=-4096.0, scalar2=4096.0,
                                                      op0=ALU.mult, op1=ALU.add), reads=[b_msk], writes=[b_pos])
                T.op("dve", lambda e: e.tensor_tensor(out=posf[:], in0=posf[:], in1=tmpf[:], op=ALU.add), reads=[b_pos], writes=[b_pos])
                T.op("dve", lambda e: e.tensor_copy(out=idx_i[:], in_=posf[:]), reads=[b_pos], writes=[b_idx])
                T.sync_all()
            with ExitStack() as p4b:
                sbx = lambda n, s, d: p4b.enter_context(nc.sbuf_tensor("sb_" + n, s, d))
                Wg = sbx("Wg", [128, KC, FF], BF16); Wu = sbx("Wu", [128, KC, FF], BF16); Wd = sbx("Wd", [128, 8, D], BF16)
                b_Wg = B("Wg"); b_Wu = B("Wu"); b_Wd = B("Wd")
                XgT = sbx("XgT", [128, KC, CAP], BF16); b_XgT = [B("XgT%d" % i) for i in range(8)]
                HT = sbx("HT", [128, 8, CAP], BF16); b_HT = [B("HT0"), B("HT1")]
                xg = [sbx("xg%d" % i, [128, D], BF16) for i in range(2)]; b_xg = [B("xg0"), B("xg1")]
                ysb = [sbx("ysb%d" % i, [128, D], BF16) for i in range(2)]; b_ysb = [B("ysb0"), B("ysb1")]
                sg = [sbx("sg%d" % i, [128, 512], F32) for i in range(2)]; b_sg = [B("sg0"), B("sg1")]
                rot_t = Rot([0, 1]); rot_a = Rot([2, 3]); rot_u = Rot([4, 5]); rot_y = Rot([6, 7])
                hsc = [sbx("hsc%d" % i, [128, D], BF16) for i in range(4)]; b_hsc = [B("hsc%d" % i) for i in range(4)]
                sc_tok = [[] for _ in range(NE)]
                nsc = [0]

                def scatter_expert(ex):
                    for j in range(NOWN):
                        Hs, bHs = hsc[nsc[0] % 4], b_hsc[nsc[0] % 4]; nsc[0] += 1
                        T.op("sp", lambda e, Hs=Hs, j=j: e.dma_start(out=Hs[:], in_=h2_s[j * 128:(j + 1) * 128, :]),
                             reads=[b_h2[j]], writes=[bHs], dma=True)
                        sc_tok[ex].append(T.op("pool", lambda e, Hs=Hs, j=j: e.indirect_dma_start(
                            out=Xg[ex], out_offset=bass.IndirectOffsetOnAxis(ap=idx_i[:, j * NE + ex:j * NE + ex + 1], axis=0),
                            in_=Hs[:, :], in_offset=None, bounds_check=bc_reg, oob_is_err=False),
                            reads=[bHs, b_idx], writes=[], dma=True, nd=160))

                def load_w(ex, which):
                    if "g" in which:
                        T.op("sp", lambda e: e.dma_start(out=Wg[:], in_=wg_bf[ex].rearrange("(k p) f -> p k f", p=128)),
                             reads=[b_wg[ex]], writes=[b_Wg], dma=True)
                        T.op("sp", lambda e: e.dma_start(out=Wu[:], in_=wu_bf[ex].rearrange("(k p) f -> p k f", p=128)),
                             reads=[b_wu[ex]], writes=[b_Wu], dma=True)
                    if "d" in which:
                        T.op("sp", lambda e: e.dma_start(out=Wd[:], in_=wd_bf[ex].rearrange("(k p) f -> p k f", p=128)),
                             reads=[b_wd[ex]], writes=[b_Wd], dma=True)

                load_w(0, "gd")
                scatter_expert(0)
                for e_ in range(NE):
                    for tok in sc_tok[e_]:
                        T._wait("sp", tok[0], tok[1])
                    for st_ in range(8):
                        XGt, bXGt = xg[st_ % 2], b_xg[st_ % 2]
                        T.op("sp", lambda e, XGt=XGt, e_=e_, st_=st_: e.dma_start(out=XGt[:], in_=Xg[e_][st_ * 128:(st_ + 1) * 128, :]),
                             reads=[b_Xg[e_]], writes=[bXGt], dma=True)
                        for q4 in range(4):
                            pb, pbb = rot_t.get()
                            pv = pb[:].bitcast(BF16)
                            for kk in range(4):
                                k = q4 * 4 + kk
                                T.op("pe", lambda e, pv=pv, kk=kk, XGt=XGt, k=k: e.transpose(
                                    out=pv[:, kk * 128:(kk + 1) * 128], in_=XGt[:, k * 128:(k + 1) * 128], identity=ident[:]),
                                    reads=[bXGt, b_const], writes=[pbb])
                            if q4 % 2 == 0:
                                T.op("act", lambda e, pv=pv, q4=q4, st_=st_: e.copy(
                                    out=XgT[:, q4 * 4:(q4 + 1) * 4, st_ * 128:(st_ + 1) * 128],
                                    in_=pv[:, 0:512].rearrange("p (k t) -> p k t", t=128)), reads=[pbb], writes=[b_XgT[st_]])
                            else:
                                T.op("dve", lambda e, pv=pv, q4=q4, st_=st_: e.tensor_copy(
                                    out=XgT[:, q4 * 4:(q4 + 1) * 4, st_ * 128:(st_ + 1) * 128],
                                    in_=pv[:, 0:512].rearrange("p (k t) -> p k t", t=128)), reads=[pbb], writes=[b_XgT[st_]])
                    if e_ + 1 < NE:
                        scatter_expert(e_ + 1)
                    for sh in range(2):
                        for fc in range(8):
                            pa, pab = rot_a.get(); pu, pub = rot_u.get()
                            for k in range(KC):
                                T.op("pe", lambda e, pa=pa, k=k, fc=fc, sh=sh: e.matmul(
                                    pa[:], lhsT=Wg[:, k, fc * 128:(fc + 1) * 128], rhs=XgT[:, k, sh * 512:(sh + 1) * 512],
                                    start=(k == 0), stop=(k == KC - 1)), reads=[b_Wg] + b_XgT[sh * 4:sh * 4 + 4], writes=[pab])
                            for k in range(KC):
                                T.op("pe", lambda e, pu=pu, k=k, fc=fc, sh=sh: e.matmul(
                                    pu[:], lhsT=Wu[:, k, fc * 128:(fc + 1) * 128], rhs=XgT[:, k, sh * 512:(sh + 1) * 512],
                                    start=(k == 0), stop=(k == KC - 1)), reads=[b_Wu] + b_XgT[sh * 4:sh * 4 + 4], writes=[pub])
                            SG, bSG = sg[fc % 2], b_sg[fc % 2]
                            T.op("act", lambda e, SG=SG, pa=pa: e.activation(out=SG[:], in_=pa[:], func=AF.Silu), reads=[pab], writes=[bSG])
                            T.op("dve", lambda e, SG=SG, pu=pu, fc=fc, sh=sh: e.tensor_tensor(
                                out=HT[:, fc, sh * 512:(sh + 1) * 512], in0=pu[:], in1=SG[:], op=ALU.mult),
                                reads=[pub, bSG], writes=[b_HT[sh]])
                    if e_ + 1 < NE:
                        load_w(e_ + 1, "g")
                    for st_ in range(8):
                        Y, bY = ysb[st_ % 2], b_ysb[st_ % 2]
                        for dg in range(4):
                            py, pyb = rot_y.get()
                            for fc in range(8):
                                T.op("pe", lambda e, py=py, fc=fc, st_=st_, dg=dg: e.matmul(
                                    py[:], lhsT=HT[:, fc, st_ * 128:(st_ + 1) * 128], rhs=Wd[:, fc, dg * 512:(dg + 1) * 512],
                                    start=(fc == 0), stop=(fc == 7)), reads=[b_HT[st_ // 4], b_Wd], writes=[pyb])
                            if dg % 2 == 0:
                                T.op("act", lambda e, Y=Y, py=py, dg=dg: e.copy(out=Y[:, dg * 512:(dg + 1) * 512], in_=py[:]),
                                     reads=[pyb], writes=[bY])
                            else:
                                T.op("dve", lambda e, Y=Y, py=py, dg=dg: e.tensor_copy(out=Y[:, dg * 512:(dg + 1) * 512], in_=py[:]),
                                     reads=[pyb], writes=[bY])
                        T.op("sp", lambda e, Y=Y, e_=e_, st_=st_: e.dma_start(out=Yg[e_][st_ * 128:(st_ + 1) * 128, :], in_=Y[:]),
                             reads=[bY], writes=[b_Yg[e_]], dma=True)
                    if e_ + 1 < NE:
                        load_w(e_ + 1, "d")
                T.sync_all()
                if stop == 5:
                    return nc
            with ExitStack() as p4c:
                sc = lambda n, s, d: p4c.enter_context(nc.sbuf_tensor("sb_" + n, s, d))
                g_bc = sc("g_bc3", [128, D], F32); b_g = B("g_bc3")
                acc = [sc("acc%d" % i, [128, D], F32) for i in range(2)]; b_acc = [B("acc0"), B("acc1")]
                NG = 12
                G = [sc("G%d" % i, [128, D], BF16) for i in range(NG)]; b_G = [B("G%d" % i) for i in range(NG)]
                dgm = [sc("dgm%d" % i, [128, 128], BF16) for i in range(NG)]; b_dgm = [B("dgm%d" % i) for i in range(NG)]
                ob = [sc("ob%d" % i, [128, D], F32) for i in range(2)]; b_ob = [B("ob0"), B("ob1")]
                junk = sc("junk3", [128, D], BF16); b_junk = B("junk3")
                st = sc("st4", [128, 8], F32); b_st = B("st4")
                T.op("sp", lambda e: e.dma_start(out=g_bc[:], in_=g_fin.partition_broadcast(128)), writes=[b_g], dma=True)
                for i in range(NG):
                    T.op("dve", lambda e, i=i: e.memset(G[i][:], 0.0), writes=[b_G[i]])
                ng = 0
                b_out = B("out")
                for j in range(NOWN):
                    A, bA = acc[j % 2], b_acc[j % 2]
                    T.op("sp", lambda e, A=A, j=j: e.dma_start(out=A[:], in_=x1_s[j * 128:(j + 1) * 128, :]), reads=[b_x1[j]], writes=[bA], dma=True)
                    pbk = [(banks[(j % 2) * 4 + c], bankb[(j % 2) * 4 + c]) for c in range(4)]
                    for e_ in range(NE):
                        Gb, bGb = G[ng % NG], b_G[ng % NG]
                        Dg, bDg = dgm[ng % NG], b_dgm[ng % NG]; ng += 1
                        T.op("pool", lambda e, Gb=Gb, j=j, e_=e_: e.indirect_dma_start(
                            out=Gb[:, :], out_offset=None, in_=Yg[e_],
                            in_offset=bass.IndirectOffsetOnAxis(ap=idx_i[:, j * NE + e_:j * NE + e_ + 1], axis=0),
                            bounds_check=bc_reg, oob_is_err=False), reads=[b_Yg[e_], b_idx], writes=[bGb], dma=True, nd=160)
                        T.op("dve", lambda e, Dg=Dg, j=j, e_=e_: e.tensor_scalar(out=Dg[:], in0=ident[:], scalar1=gt[:, j, e_:e_ + 1], scalar2=None,
                                                                                op0=ALU.mult), reads=[b_const, b_gt], writes=[bDg])
                        for c in range(4):
                            T.op("pe", lambda e, c=c, Dg=Dg, Gb=Gb, e_=e_: e.matmul(
                                pbk[c][0][:], lhsT=Dg[:], rhs=Gb[:, c * 512:(c + 1) * 512], start=(e_ == 0), stop=(e_ == NE - 1)),
                                reads=[bDg, bGb], writes=[pbk[c][1]])
                    for c in range(4):
                        T.op("dve", lambda e, c=c, A=A: e.tensor_tensor(out=A[:, c * 512:(c + 1) * 512], in0=pbk[c][0][:],
                                                                         in1=A[:, c * 512:(c + 1) * 512], op=ALU.add),
                             reads=[pbk[c][1], bA], writes=[bA])
                    T.op("act", lambda e, A=A: e.activation(out=junk[:], in_=A[:], func=AF.Square, accum_out=st[:, 0:1]),
                         reads=[bA], writes=[b_junk, b_st])
                    rstd_from_ss(st[:, 0:1], st[:, 1:2], st[:, 2:3], 1, 1.0 / D, [b_st])
                    O_, bO_ = ob[j % 2], b_ob[j % 2]
                    T.op("dve", lambda e, O_=O_, A=A: e.scalar_tensor_tensor(out=O_[:], in0=A[:], scalar=st[:, 2:3], in1=g_bc[:],
                                                                            op0=ALU.mult, op1=ALU.mult), reads=[bA, b_st, b_g], writes=[bO_])
                    T.op("sp", lambda e, O_=O_, j=j: e.dma_start(out=out[j * 128:(j + 1) * 128, :], in_=O_[:]), reads=[bO_], writes=[b_out], dma=True)
                T.sync_all()
        print("ops:", T.cnt, "dmas:", T.dcnt, "waits:", T.nwait, "sems:", len(T.sems))
    return nc
```

```python
import math
from contextlib import ExitStack
import numpy as np
import ml_dtypes
import concourse.bass as bass
import concourse.mybir as mybir
from concourse.bass_utils import run_bass_kernel_spmd

F32 = mybir.dt.float32
BF16 = mybir.dt.bfloat16
I32 = mybir.dt.int32
AF = mybir.ActivationFunctionType
ALU = mybir.AluOpType
AX = mybir.AxisListType

S = 8192
D = 2048
NT = 64
NOWN = 32
KC = 16
NE = 16
CAP = 1024
FF = 1024
EPS = 1e-6
SCALE = 1.0 / math.sqrt(128.0)
NEG = -1e30
NBISECT = 26
SPECIAL = [0, 31, 32, 63]


class Buf:
    __slots__ = ("name", "w", "r", "excl")

    def __init__(self, name, excl=False):
        self.name = name
        self.w = None
        self.r = {}
        self.excl = excl


class Trk:
    EPOCH = 12000
    RING = 16

    def __init__(self, nc, stack):
        self.nc = nc
        self.stack = stack
        self.eng = {"pe": nc.tensor, "act": nc.scalar, "dve": nc.vector, "pool": nc.gpsimd, "sp": nc.sync}
        self.sems = {}
        self.cnt = {e: 0 for e in self.eng}
        self.dcnt = {e: 0 for e in self.eng}
        self.obs = {e: {} for e in self.eng}
        self.owner = {}
        self.latest = {}
        self.nwait = 0
        self.pq = []
        self.pq_sum = 0
        self.nop = 0
        self.limit = None
        self.log = []

    def _sem(self, key, owner):
        if key not in self.sems:
            self.sems[key] = self.stack.enter_context(self.nc.semaphore("s_%s_%s" % key))
            self.owner[key] = owner
        return self.sems[key]

    def _wait(self, e, key, val):
        if self.obs[e].get(key, 0) >= val:
            return
        self.eng[e].wait_ge(self.sems[key], val)
        self.obs[e][key] = val
        self.nwait += 1

    def op(self, e, fn, reads=(), writes=(), dma=False, nd=2048):
        self.nop += 1
        if self.limit is not None and self.nop > self.limit:
            return None
        if self.limit is not None:
            import inspect
            self.log.append((self.nop, e, inspect.stack()[1].lineno))
        deps = {}
        if dma and e == "pool":
            while self.pq and self.pq_sum + nd > 9000:
                otok, ond = self.pq.pop(0)
                self._wait("pool", otok[0], otok[1])
                self.pq_sum -= ond

        def add(tok):
            if tok is None:
                return
            k, v = tok
            if deps.get(k, 0) < v:
                deps[k] = v

        for b in reads:
            add(b.w)
            if b.excl:
                for k, v in b.r.items():
                    if self.owner[k][0] != e:
                        add((k, v))
        for b in writes:
            add(b.w)
            for k, v in b.r.items():
                add((k, v))
        for k, v in deps.items():
            if e == "pe" and self.owner[k] == ("pe", False):
                continue
            self._wait(e, k, v)
        if dma:
            n = self.dcnt[e]
            self.dcnt[e] += 1
            key = ("d" + e, n % self.RING)
            val = 16 * (n // self.RING + 1)
            sem = self._sem(key, (e, True))
            fn(self.eng[e]).then_inc(sem, 16)
        else:
            n = self.cnt[e]
            self.cnt[e] += 1
            key = (e, n // self.EPOCH)
            val = n % self.EPOCH + 1
            sem = self._sem(key, (e, False))
            fn(self.eng[e]).then_inc(sem, 1)
        tok = (key, val)
        if dma and e == "pool":
            self.pq.append((tok, nd))
            self.pq_sum += nd
        self.latest[key] = val
        for b in reads:
            if b.r.get(key, 0) < val:
                b.r[key] = val
        for b in writes:
            b.w = tok
            b.r = {}
        return tok

    def sync_all(self, engines=None):
        for e in (engines or list(self.eng)):
            for k, v in self.latest.items():
                self._wait(e, k, v)


def rope(T, src, t1, t2, dst, C, S_, nh, reads, writes):
    T.op("dve", lambda e: e.tensor_tensor(out=t1[:], in0=src[:], in1=C[:].unsqueeze(1).to_broadcast([128, nh, 128]), op=ALU.mult),
         reads=reads, writes=writes)
    s5 = src[:].rearrange("p h (a b c) -> p h a b c", a=2, b=2, c=32)
    t5 = t2[:].rearrange("p h (a b c) -> p h a b c", a=2, b=2, c=32)
    S4 = S_[:].rearrange("p (a b c) -> p a b c", a=2, b=2, c=32)
    for b in range(2):
        T.op("dve", lambda e, b=b: e.tensor_tensor(out=t5[:, :, :, b, :], in0=s5[:, :, :, 1 - b, :],
                                                   in1=S4[:, :, b, :].unsqueeze(1).to_broadcast([128, nh, 2, 32]), op=ALU.mult),
             reads=reads, writes=writes)
    T.op("dve", lambda e: e.tensor_tensor(out=dst[:], in0=t1[:], in1=t2[:], op=ALU.add), reads=reads, writes=writes)


def kernel(x, g_mix, w_in, b_gate, qn_a, kn_a, w_proj_a, sink_b, rel_bias, w_proj_b,
           w_o, g_ffn, w_router, w_gate_e, w_up_e, w_down_e, g_final):
    nc = build_program()
    in_maps = make_inputs(x, g_mix, w_in, b_gate, qn_a, kn_a, w_proj_a, sink_b, rel_bias, w_proj_b,
                          w_o, g_ffn, w_router, w_gate_e, w_up_e, w_down_e, g_final)
    res = run_bass_kernel_spmd(nc, in_maps, core_ids=list(range(8)))
    out = np.empty((4, S, D), np.float32)
    for c in range(8):
        b, hf = c // 2, c % 2
        out[b, hf * 4096:(hf + 1) * 4096] = res.results[c]["out"]
    return out


def _t5_bucket(rel):
    half = 16
    ret = np.where(rel > 0, half, 0)
    n = np.abs(rel)
    max_exact = 8
    nf = np.maximum(n, 1).astype(np.float32)
    large = max_exact + (np.log(nf / max_exact) / math.log(128 / max_exact) * (half - max_exact)).astype(np.int32)
    large = np.minimum(large, half - 1)
    return ret + np.where(n < max_exact, n, large)


def make_inputs(x, g_mix, w_in, b_gate, qn_a, kn_a, w_proj_a, sink_b, rel_bias, w_proj_b,
                w_o, g_ffn, w_router, w_gate_e, w_up_e, w_down_e, g_final):
    f = lambda a: np.ascontiguousarray(np.asarray(a, dtype=np.float32))
    x = f(x)
    pos = np.arange(S)
    r = (pos // 64).astype(np.float32)
    c = (pos % 64).astype(np.float32)
    inv = (1.0 / (10000.0 ** (np.arange(0, 64, 2, dtype=np.float32) / 64.0))).astype(np.float32)
    ar = r[:, None] * inv[None, :]
    ac = c[:, None] * inv[None, :]
    ropeC = np.concatenate([np.cos(ar), np.cos(ar), np.cos(ac), np.cos(ac)], axis=1).astype(np.float32)
    ropeS = np.concatenate([-np.sin(ar), np.sin(ar), -np.sin(ac), np.sin(ac)], axis=1).astype(np.float32)
    rel = (np.arange(384) - 128)[None, :] - np.arange(128)[:, None]
    bucket = _t5_bucket(rel)
    band = np.where(np.abs(rel) <= 128, 0.0, NEG).astype(np.float32)
    biasB = np.ascontiguousarray(f(rel_bias)[bucket].transpose(0, 2, 1))
    ident = np.eye(128, dtype=np.float32).astype(ml_dtypes.bfloat16)
    triU = np.triu(np.ones((128, 128), np.float32), 1).astype(ml_dtypes.bfloat16)
    bg = np.ascontiguousarray(f(b_gate).reshape(32, 128).T)
    shared = {
        "w_in": f(w_in)[0], "w_pa": f(w_proj_a)[0], "w_pb": f(w_proj_b)[0], "w_o": f(w_o)[0],
        "w_r": f(w_router)[0], "w_g": f(w_gate_e)[0], "w_u": f(w_up_e)[0], "w_d": f(w_down_e)[0],
        "g_mix": f(g_mix).reshape(1, D), "g_ffn": f(g_ffn).reshape(1, D), "g_fin": f(g_final).reshape(1, D),
        "qn": f(qn_a).reshape(1, 128), "kn": f(kn_a).reshape(1, 128), "sink": f(sink_b).reshape(1, 8),
        "bgate": bg, "biasB": biasB, "band": band, "ident": ident, "triU": triU,
    }
    maps = []
    for core in range(8):
        b, hf = core // 2, core % 2
        perm = (np.arange(S) + hf * 4096) % S
        em = np.zeros((4, 384), np.float32)
        for i, t in enumerate(SPECIAL):
            orig = (t + hf * 32) % 64
            if orig == 0:
                em[i, 0:128] = NEG
            if orig == 63:
                em[i, 256:384] = NEG
        m = dict(shared)
        m["x"] = np.ascontiguousarray(x[b][perm])
        m["ropeC"] = np.ascontiguousarray(ropeC[perm])
        m["ropeS"] = np.ascontiguousarray(ropeS[perm])
        m["emask"] = em
        maps.append(m)
    return maps


def build_program(stop=None, dbg=(), nt1=NT, flags=(), limit=None):
    nc = bass.Bass("TRN2", target_bir_lowering=False)
    dt_in = lambda n, s, d=F32: nc.dram_tensor(n, s, d, kind="ExternalInput").ap()
    dt_sc = lambda n, s, d: nc.dram_tensor(n, s, d, kind=("ExternalOutput" if n in dbg else "Internal")).ap()
    x = dt_in("x", [S, D])
    w_in = dt_in("w_in", [D, 7168]); w_pa = dt_in("w_pa", [1024, D]); w_pb = dt_in("w_pb", [1024, D])
    w_o = dt_in("w_o", [D, D]); w_r = dt_in("w_r", [D, NE])
    w_g = dt_in("w_g", [NE, D, FF]); w_u = dt_in("w_u", [NE, D, FF]); w_d = dt_in("w_d", [NE, FF, D])
    g_mix = dt_in("g_mix", [1, D]); g_ffn = dt_in("g_ffn", [1, D]); g_fin = dt_in("g_fin", [1, D])
    qn = dt_in("qn", [1, 128]); kn = dt_in("kn", [1, 128]); sink = dt_in("sink", [1, 8])
    bgate_d = dt_in("bgate", [128, 32]); biasB_d = dt_in("biasB", [128, 8, 384]); band_d = dt_in("band", [128, 384])
    ident_d = dt_in("ident", [128, 128], BF16); triU_d = dt_in("triU", [128, 128], BF16)
    ropeC = dt_in("ropeC", [S, 128]); ropeS = dt_in("ropeS", [S, 128]); emask_d = dt_in("emask", [4, 384])
    out = nc.dram_tensor("out", [4096, D], F32, kind="ExternalOutput").ap()

    w_in_bf = dt_sc("w_in_bf", [D, 7168], BF16)
    wgt_s = dt_sc("wgt_s", [32, 128, KC, 128], BF16)
    wp_s = dt_sc("wp_s", [2, 16, 128, 8, 128], BF16)
    wr_bf = dt_sc("wr_bf", [D, NE], BF16)
    wg_bf = dt_sc("wg_bf", [NE, D, FF], BF16); wu_bf = dt_sc("wu_bf", [NE, D, FF], BF16)
    wd_bf = dt_sc("wd_bf", [NE, FF, D], BF16)
    hT_s = dt_sc("hT_s", [16, 128, KC, 512], BF16)
    wq_s = dt_sc("wq_s", [4, 128, KC, 512], BF16)
    wo_s = dt_sc("wo_s", [4, 128, KC, 512], BF16)
    kbT_s = dt_sc("kbT_s", [2, 128, S], BF16)
    vb_s = dt_sc("vb_s", [S, 256], BF16)
    oT_s = dt_sc("oT_s", [2, 16, 128, 8, 512], BF16)
    x1_s = dt_sc("x1_s", [4096, D], F32)
    h2_s = dt_sc("h2_s", [4096, D], BF16)
    Xg = [dt_sc("Xg%d" % i, [CAP, D], BF16) for i in range(NE)]
    Yg = [dt_sc("Yg%d" % i, [CAP, D], BF16) for i in range(NE)]

    with ExitStack() as stack:
        T = Trk(nc, stack)
        T.limit = limit
        build_program.T = T
        sb = lambda n, s, d: stack.enter_context(nc.sbuf_tensor("sb_" + n, s, d))
        B = Buf

        banks = [stack.enter_context(nc.psum_tensor("ps%d" % i, [128, 512], F32)) for i in range(8)]
        bankb = [B("bank%d" % i, True) for i in range(8)]

        class Rot:
            def __init__(self, ids):
                self.ids = ids; self.i = 0

            def get(self):
                k = self.ids[self.i % len(self.ids)]; self.i += 1
                return banks[k], bankb[k]

        b_win = [B("win%d" % i) for i in range(4)]
        for i in range(4):
            T.op("pool", lambda e, i=i: e.dma_start(
                out=w_in_bf[i * 512:(i + 1) * 512, :].rearrange("r (a c) -> r a c", c=1024),
                in_=w_in[i * 512:(i + 1) * 512, :].rearrange("r (a c) -> r a c", c=1024)), writes=[b_win[i]], dma=True, nd=3584)
        b_wr = B("wr")
        b_wqs = [B("wqs%d" % i) for i in range(4)]; b_wosc = [B("wos_s%d" % i) for i in range(4)]
        b_wgt = [B("wgt%d" % i) for i in range(32)]
        b_wp = [B("wp%d" % i) for i in range(32)]
        for g_, c0 in enumerate((0, 512, 1536, 2048)):
            T.op("pool", lambda e, g_=g_, c0=c0: e.dma_start(
                out=wq_s[g_], in_=w_in[:, c0:c0 + 512].rearrange("(k p) c -> p k c", p=128)), writes=[b_wqs[g_]], dma=True)
        deferred = []
        for g_ in range(4):
            deferred.append(lambda g_=g_: T.op("pool", lambda e: e.dma_start(
                out=wo_s[g_], in_=w_o[:, g_ * 512:(g_ + 1) * 512].rearrange("(k p) c -> p k c", p=128)), writes=[b_wosc[g_]], dma=True))
        deferred.append(lambda: T.op("pool", lambda e: e.dma_start(out=wr_bf, in_=w_r), writes=[b_wr], dma=True))
        for n_ in range(32):
            deferred.append(lambda n_=n_: T.op("pool", lambda e: e.dma_start(
                out=wgt_s[n_], in_=w_in[:, 3072 + n_ * 128:3072 + (n_ + 1) * 128].rearrange("(k p) c -> p k c", p=128)),
                writes=[b_wgt[n_]], dma=True))
        for ab, wsrc in enumerate((w_pa, w_pb)):
            for n_ in range(16):
                deferred.append(lambda ab=ab, n_=n_, wsrc=wsrc: T.op("pool", lambda e: e.dma_start(
                    out=wp_s[ab, n_], in_=wsrc[:, n_ * 128:(n_ + 1) * 128].rearrange("(h p) c -> p h c", p=128)),
                    writes=[b_wp[ab * 16 + n_]], dma=True))
        b_wg = [B("wg%d" % i) for i in range(NE)]; b_wu = [B("wu%d" % i) for i in range(NE)]
        b_wd = [B("wd%d" % i) for i in range(NE)]

        def cast_expert(e_):
            T.op("pool", lambda e: e.dma_start(out=wg_bf[e_], in_=w_g[e_]), writes=[b_wg[e_]], dma=True)
            T.op("pool", lambda e: e.dma_start(out=wu_bf[e_], in_=w_u[e_]), writes=[b_wu[e_]], dma=True)
            T.op("pool", lambda e: e.dma_start(out=wd_bf[e_].rearrange("r (a c) -> r a c", c=1024),
                                               in_=w_d[e_].rearrange("r (a c) -> r a c", c=1024)),
                 writes=[b_wd[e_]], dma=True)

        ident = sb("ident", [128, 128], BF16); ones = sb("ones", [128, 128], BF16); triU = sb("triU", [128, 128], BF16)
        qn_bc = sb("qn_bc", [128, 128], F32); kn_bc = sb("kn_bc", [128, 128], F32)
        bgate = sb("bgate", [128, 32], F32); sink_bc = sb("sink_bc", [128, 8], F32); nsink_bc = sb("nsink_bc", [128, 8], F32)
        aff = sb("aff", [128, NT, NE], F32)
        b_const = B("const"); b_aff = [B("aff%d" % j) for j in range(NT)]
        T.op("sp", lambda e: e.dma_start(out=ident[:], in_=ident_d), writes=[b_const], dma=True)
        T.op("sp", lambda e: e.dma_start(out=triU[:], in_=triU_d), writes=[b_const], dma=True)
        T.op("sp", lambda e: e.dma_start(out=qn_bc[:], in_=qn.partition_broadcast(128)), writes=[b_const], dma=True)
        T.op("sp", lambda e: e.dma_start(out=kn_bc[:], in_=kn.partition_broadcast(128)), writes=[b_const], dma=True)
        T.op("sp", lambda e: e.dma_start(out=bgate[:], in_=bgate_d), writes=[b_const], dma=True)
        T.op("sp", lambda e: e.dma_start(out=sink_bc[:], in_=sink.partition_broadcast(128)), writes=[b_const], dma=True)
        T.op("dve", lambda e: e.memset(ones[:], 1.0), writes=[b_const])
        T.op("dve", lambda e: e.tensor_scalar(out=nsink_bc[:], in0=sink_bc[:], scalar1=-1.0, scalar2=None, op0=ALU.mult),
             reads=[b_const], writes=[b_const])

        if stop == 0:
            T.sync_all()
            return nc

        def rstd_from_ss(ss, tmp, rstd, n, inv_n, bufs):
            T.op("dve", lambda e: e.tensor_scalar(out=tmp[:, 0:n], in0=ss[:, 0:n], scalar1=inv_n, scalar2=EPS,
                                                  op0=ALU.mult, op1=ALU.add), reads=bufs, writes=bufs)
            T.op("act", lambda e: e.activation(out=tmp[:, 0:n], in_=tmp[:, 0:n], func=AF.Ln), reads=bufs, writes=bufs)
            T.op("act", lambda e: e.activation(out=rstd[:, 0:n], in_=tmp[:, 0:n], func=AF.Exp, scale=-0.5), reads=bufs, writes=bufs)

        with ExitStack() as kvstack:
            sbk = lambda n, s, d: kvstack.enter_context(nc.sbuf_tensor("sb_" + n, s, d))
            KAT = sbk("KAT", [128, 2, S], BF16)
            VA = sbk("VA", [128, NT, 256], BF16)
            b_kat = [B("kat%d" % j) for j in range(NT)]
            b_va = [B("va%d" % j) for j in range(NT)]
            b_hT = [B("hTs%d" % j) for j in range(NT)]
            b_kb = [B("kbs%d" % j) for j in range(NT)]
            b_vb = [B("vbs%d" % j) for j in range(NT)]

            with ExitStack() as p1:
                s1 = lambda n, s, d: p1.enter_context(nc.sbuf_tensor("sb_" + n, s, d))
                NB1 = 3
                g_bc = s1("g_bc", [128, D], F32); b_g = B("g_bc")
                wkv = s1("wkv", [128, KC, 1024], BF16); b_wkv = B("wkv")
                xt = [s1("xt%d" % i, [128, D], F32) for i in range(NB1)]; b_xt = [B("xt%d" % i) for i in range(NB1)]
                junk = s1("junk", [128, D], BF16); b_junk = B("junk")
                hb = [s1("hb%d" % i, [128, D], BF16) for i in range(NB1)]; b_hb = [B("hb%d" % i) for i in range(NB1)]
                hTt = [s1("hTt%d" % i, [128, KC, 128], BF16) for i in range(NB1)]; b_hTt = [B("hTt%d" % i) for i in range(NB1)]
                st = [s1("st%d" % i, [128, 16], F32) for i in range(NB1)]; b_st = [B("st%d" % i) for i in range(NB1)]
                ksb_ = [s1("ksb%d" % i, [128, 256], F32) for i in range(NB1)]; ksq_ = [s1("ksq%d" % i, [128, 256], F32) for i in range(NB1)]
                knm_ = [s1("knm%d" % i, [128, 2, 128], F32) for i in range(NB1)]
                kt1_ = [s1("kt1%d" % i, [128, 2, 128], F32) for i in range(NB1)]; kt2_ = [s1("kt2%d" % i, [128, 2, 128], F32) for i in range(NB1)]
                krot_ = [s1("krot%d" % i, [128, 2, 128], BF16) for i in range(NB1)]; b_k_ = [B("kscratch%d" % i) for i in range(NB1)]
                rC = [s1("rC%d" % i, [128, 128], F32) for i in range(NB1)]; rS = [s1("rS%d" % i, [128, 128], F32) for i in range(NB1)]
                b_rope = [B("rope%d" % i) for i in range(NB1)]
                kbb_ = [s1("kbb%d" % i, [128, 256], BF16) for i in range(NB1)]; b_kbb_ = [B("kbb%d" % i) for i in range(NB1)]
                kbTt = [s1("kbTt%d" % i, [128, 2, 128], BF16) for i in range(NB1)]; b_kbTt = [B("kbTt%d" % i) for i in range(NB1)]
                vbb = [s1("vbb%d" % i, [128, 256], BF16) for i in range(NB1)]; b_vbb = [B("vbb%d" % i) for i in range(NB1)]
                rot_t = Rot([0, 1]); rot_m = Rot([2, 3, 4, 5]); rot_k = Rot([6, 7])

                T.op("sp", lambda e: e.dma_start(out=g_bc[:], in_=g_mix.partition_broadcast(128)), writes=[b_g], dma=True)
                for hh, c0 in enumerate((1024, 2560)):
                    T.op("sp", lambda e, hh=hh, c0=c0: e.dma_start(
                        out=wkv[:, :, hh * 512:(hh + 1) * 512],
                        in_=w_in_bf[:, c0:c0 + 512].rearrange("(k p) c -> p k c", p=128)),
                        reads=b_win, writes=[b_wkv], dma=True)

                def p1_head(j):
                        i2 = j % NB1
                        X, bX = xt[i2], b_xt[i2]
                        ksb, ksq, knm, kt1, kt2, krot, b_k = ksb_[i2], ksq_[i2], knm_[i2], kt1_[i2], kt2_[i2], krot_[i2], b_k_[i2]
                        kbb, b_kbb = kbb_[i2], b_kbb_[i2]
                        T.op("sp", lambda e, X=X, j=j: e.dma_start(out=X[:], in_=x[j * 128:(j + 1) * 128, :]), writes=[bX], dma=True)
                        T.op("sp", lambda e, j=j, i2=i2: e.dma_start(out=rC[i2][:], in_=ropeC[j * 128:(j + 1) * 128, :]),
                             writes=[b_rope[i2]], dma=True)
                        T.op("sp", lambda e, j=j, i2=i2: e.dma_start(out=rS[i2][:], in_=ropeS[j * 128:(j + 1) * 128, :]),
                             writes=[b_rope[i2]], dma=True)
                        ST, bST = st[i2], b_st[i2]
                        T.op("act", lambda e, X=X, ST=ST: e.activation(out=junk[:], in_=X[:], func=AF.Square, accum_out=ST[:, 0:1]),
                             reads=[bX], writes=[b_junk, bST])
                        rstd_from_ss(ST[:, 0:1], ST[:, 1:2], ST[:, 2:3], 1, 1.0 / D, [bST])
                        H, bH = hb[i2], b_hb[i2]
                        T.op("dve", lambda e, H=H, X=X, ST=ST: e.scalar_tensor_tensor(
                            out=H[:], in0=X[:], scalar=ST[:, 2:3], in1=g_bc[:], op0=ALU.mult, op1=ALU.mult),
                            reads=[bX, bST, b_g], writes=[bH])
                        HT, bHT = hTt[i2], b_hTt[i2]
                        for q4 in range(4):
                            pb, pbb = rot_t.get()
                            pv = pb[:].bitcast(BF16)
                            for kk in range(4):
                                k = q4 * 4 + kk
                                T.op("pe", lambda e, pv=pv, kk=kk, H=H, k=k: e.transpose(
                                    out=pv[:, kk * 128:(kk + 1) * 128], in_=H[:, k * 128:(k + 1) * 128], identity=ident[:]),
                                    reads=[bH, b_const], writes=[pbb])
                            ce = "act" if q4 % 2 == 0 else "dve"
                            if ce == "act":
                                T.op("act", lambda e, HT=HT, q4=q4, pv=pv: e.copy(
                                    out=HT[:, q4 * 4:(q4 + 1) * 4, :], in_=pv[:, 0:512].rearrange("p (k t) -> p k t", t=128)),
                                    reads=[pbb], writes=[bHT])
                            else:
                                T.op("dve", lambda e, HT=HT, q4=q4, pv=pv: e.tensor_copy(
                                    out=HT[:, q4 * 4:(q4 + 1) * 4, :], in_=pv[:, 0:512].rearrange("p (k t) -> p k t", t=128)),
                                    reads=[pbb], writes=[bHT])
                        T.op("pool", lambda e, HT=HT, j=j: e.dma_start(
                            out=hT_s[j // 4, :, :, (j % 4) * 128:(j % 4 + 1) * 128], in_=HT[:]),
                            reads=[bHT], writes=[b_hT[j]], dma=True)
                        pA, pAb = rot_m.get(); pB, pBb = rot_m.get()
                        for k in range(KC):
                            T.op("pe", lambda e, pA=pA, HT=HT, k=k: e.matmul(pA[:], lhsT=HT[:, k, :], rhs=wkv[:, k, 0:512],
                                                                              start=(k == 0), stop=(k == KC - 1)),
                                 reads=[bHT, b_wkv], writes=[pAb])
                        for k in range(KC):
                            T.op("pe", lambda e, pB=pB, HT=HT, k=k: e.matmul(pB[:], lhsT=HT[:, k, :], rhs=wkv[:, k, 512:1024],
                                                                              start=(k == 0), stop=(k == KC - 1)),
                                 reads=[bHT, b_wkv], writes=[pBb])
                        return (j, i2, X, bX, ksb, ksq, knm, kt1, kt2, krot, b_k, kbb, b_kbb, ST, bST, H, bH, HT, bHT, pA, pAb, pB, pBb)

                def p1_tail(L):
                        (j, i2, X, bX, ksb, ksq, knm, kt1, kt2, krot, b_k, kbb, b_kbb, ST, bST, H, bH, HT, bHT, pA, pAb, pB, pBb) = L
                        T.op("act", lambda e, pA=pA, j=j: e.copy(out=VA[:, j, :], in_=pA[:, 256:512]), reads=[pAb], writes=[b_va[j]])
                        T.op("act", lambda e, pB=pB, kbb=kbb: e.copy(out=kbb[:], in_=pB[:, 0:256]), reads=[pBb], writes=[b_kbb])
                        VB_, bVB_ = vbb[i2], b_vbb[i2]
                        T.op("act", lambda e, pB=pB, VB_=VB_: e.copy(out=VB_[:], in_=pB[:, 256:512]), reads=[pBb], writes=[bVB_])
                        T.op("pool", lambda e, VB_=VB_, j=j: e.dma_start(out=vb_s[j * 128:(j + 1) * 128, :], in_=VB_[:]),
                             reads=[bVB_], writes=[b_vb[j]], dma=True, nd=160)
                        T.op("dve", lambda e, pA=pA, ksb=ksb: e.tensor_copy(out=ksb[:], in_=pA[:, 0:256]), reads=[pAb], writes=[b_k])
                        T.op("dve", lambda e, ksq=ksq, ksb=ksb: e.tensor_tensor(out=ksq[:], in0=ksb[:], in1=ksb[:], op=ALU.mult), reads=[b_k], writes=[b_k])
                        T.op("dve", lambda e, ST=ST, ksq=ksq: e.tensor_reduce(out=ST[:, 4:6], in_=ksq[:].rearrange("p (h d) -> p h d", d=128),
                                                                      axis=AX.X, op=ALU.add), reads=[b_k], writes=[bST])
                        rstd_from_ss(ST[:, 4:6], ST[:, 6:8], ST[:, 8:10], 2, 1.0 / 128, [bST])
                        for hh in range(2):
                            T.op("dve", lambda e, hh=hh, ST=ST, knm=knm, ksb=ksb: e.scalar_tensor_tensor(
                                out=knm[:, hh, :], in0=ksb[:, hh * 128:(hh + 1) * 128], scalar=ST[:, 8 + hh:9 + hh], in1=kn_bc[:],
                                op0=ALU.mult, op1=ALU.mult), reads=[b_k, bST, b_const], writes=[b_k])
                        rope(T, knm, kt1, kt2, krot, rC[i2], rS[i2], 2, [b_k, b_rope[i2]], [b_k])
                        pk, pkb = rot_k.get()
                        pkv = pk[:].bitcast(BF16)
                        for hh in range(2):
                            T.op("pe", lambda e, pkv=pkv, hh=hh, krot=krot: e.transpose(out=pkv[:, hh * 128:(hh + 1) * 128], in_=krot[:, hh, :],
                                                                             identity=ident[:]), reads=[b_k, b_const], writes=[pkb])
                        for hh in range(2):
                            T.op("pe", lambda e, pkv=pkv, hh=hh, kbb=kbb: e.transpose(out=pkv[:, 256 + hh * 128:256 + (hh + 1) * 128],
                                                                             in_=kbb[:, hh * 128:(hh + 1) * 128], identity=ident[:]),
                                 reads=[b_kbb, b_const], writes=[pkb])
                        T.op("dve", lambda e, pkv=pkv, j=j: e.tensor_copy(
                            out=KAT[:, :, j * 128:(j + 1) * 128], in_=pkv[:, 0:256].rearrange("p (h t) -> p h t", t=128)),
                            reads=[pkb], writes=[b_kat[j]])
                        KBT, bKBT = kbTt[i2], b_kbTt[i2]
                        T.op("act", lambda e, pkv=pkv, KBT=KBT: e.copy(out=KBT[:], in_=pkv[:, 256:512].rearrange("p (h t) -> p h t", t=128)),
                             reads=[pkb], writes=[bKBT])
                        T.op("pool", lambda e, KBT=KBT, j=j: e.dma_start(
                            out=kbT_s[:, :, j * 128:(j + 1) * 128].rearrange("h p t -> p h t"), in_=KBT[:]),
                            reads=[bKBT], writes=[b_kb[j]], dma=True, nd=300)

                Lp = p1_head(0)
                for j in range(nt1):
                    Ln_ = p1_head(j + 1) if j + 1 < nt1 else None
                    p1_tail(Lp)
                    Lp = Ln_
                T.sync_all()
                if stop == 1:
                    return nc

            b_oT = [B("oT%d" % i) for i in range(16)]
            with ExitStack() as p2:
                s2 = lambda n, s, d: p2.enter_context(nc.sbuf_tensor("sb_" + n, s, d))
                hTb = s2("hTb", [128, KC, 512], BF16); b_hTb = B("hTb")
                wq = [s2("wq%d" % i, [128, KC, 512], BF16) for i in range(2)]; b_wq = [B("wq0"), B("wq1")]
                qAT = s2("qAT", [128, 8, 512], BF16); b_qAT = B("qAT")
                qBT = s2("qBT", [128, 8, 512], BF16); b_qBT = B("qBT")
                oAT = s2("oAT", [128, 8, 512], BF16); b_oAT = B("oAT")
                oBT = s2("oBT", [128, 8, 512], BF16); b_oBT = B("oBT")
                biasB = s2("biasB", [128, 8, 384], F32); b_bias = B("biasB")
                emk = s2("emk", [128, 384], F32); b_emk = B("emk")
                qsb_ = [s2("qsb%d" % i, [128, 512], F32) for i in range(2)]; qnm_ = [s2("qnm%d" % i, [128, 4, 128], F32) for i in range(2)]
                qt1_ = [s2("qt1%d" % i, [128, 4, 128], F32) for i in range(2)]; qt2_ = [s2("qt2%d" % i, [128, 4, 128], F32) for i in range(2)]
                qrot_ = [s2("qrot%d" % i, [128, 4, 128], BF16) for i in range(2)]
                b_q_ = [B("qscratch0"), B("qscratch1")]
                st2_ = [s2("st2%d" % i, [128, 16], F32) for i in range(2)]; b_st2_ = [B("st20"), B("st21")]
                qunit = 0
                rC2 = [s2("rC2%d" % i, [128, 128], F32) for i in range(2)]; rS2 = [s2("rS2%d" % i, [128, 128], F32) for i in range(2)]
                b_rope2 = [B("rope20"), B("rope21")]
                PT = [s2("PT%d" % i, [128, 512], BF16) for i in range(4)]; b_PT = [B("PT%d" % i) for i in range(4)]
                rD = [s2("rD0", [128, 512], F32)] * 2; b_rD = [B("rD0")] * 2
                kbw = s2("kbw", [128, 2, 768], BF16); vbw = s2("vbw", [128, 6, 256], BF16); b_kbw = B("kbw"); b_vbw = B("vbw")
                ssb = s2("ssb", [128, 4, 384], F32); pbf = s2("pbf", [128, 4, 384], BF16)
                pTs = s2("pTs", [128, 12, 128], BF16); b_bs = B("b_s"); b_be = B("b_e"); b_bp = B("b_p"); b_bpT = B("b_pT")
                stB = s2("stB", [128, 32], F32); b_stB = B("stB")

                T.op("sp", lambda e: e.dma_start(out=biasB[:], in_=biasB_d), writes=[b_bias], dma=True)
                T.op("sp", lambda e: e.dma_start(out=emk[:], in_=band_d), writes=[b_emk], dma=True)
                T.op("dve", lambda e: e.tensor_tensor(out=biasB[:], in0=biasB[:], in1=emk[:].unsqueeze(1).to_broadcast([128, 8, 384]),
                                                      op=ALU.add), reads=[b_bias, b_emk], writes=[b_bias])
                rot_q = Rot([6, 7]); rot_s = Rot([0, 1, 2])

                for bi in range(16):
                    t0 = bi * 512
                    if bi == 0:
                        T.op("sp", lambda e: e.dma_start(out=hTb[:], in_=hT_s[0]), reads=b_hT[0:4], writes=[b_hTb], dma=True)
                        T.op("sp", lambda e: e.dma_start(out=wq[0][:], in_=wq_s[0]), reads=[b_wqs[0]], writes=[b_wq[0]], dma=True)
                        T.op("sp", lambda e: e.dma_start(out=wq[1][:], in_=wq_s[2]), reads=[b_wqs[2]], writes=[b_wq[1]], dma=True)
                    jl = (bi * 4 - 1) % NT
                    jr = (bi * 4 + 4) % NT
                    segs = [(0, jl, 1), (1, bi * 4, 4), (5, jr, 1)]
                    for (w0, j0, n) in segs:
                        T.op("sp", lambda e, w0=w0, j0=j0, n=n: e.dma_start(
                            out=kbw[:, :, w0 * 128:(w0 + n) * 128],
                            in_=kbT_s[:, :, j0 * 128:(j0 + n) * 128].rearrange("h p t -> p h t")),
                            reads=b_kb[j0:j0 + n], writes=[b_kbw], dma=True)
                        T.op("sp", lambda e, w0=w0, j0=j0, n=n: e.dma_start(
                            out=vbw[:, w0:w0 + n, :],
                            in_=vb_s[j0 * 128:(j0 + n) * 128, :].rearrange("(t p) c -> p t c", p=128)),
                            reads=b_vb[j0:j0 + n], writes=[b_vbw], dma=True)
                    pendq = None

                    def finish_unit(u):
                        (qrot_u, b_q_u, cg_u, tt_u) = u
                        pt, ptb = rot_s.get()
                        ptv = pt[:].bitcast(BF16)
                        for hh in range(4):
                            T.op("pe", lambda e, ptv=ptv, hh=hh: e.transpose(out=ptv[:, hh * 128:(hh + 1) * 128], in_=qrot_u[:, hh, :],
                                                                             identity=ident[:]), reads=[b_q_u, b_const], writes=[ptb])
                        T.op("dve", lambda e, ptv=ptv: e.tensor_copy(
                            out=qAT[:, cg_u * 4:(cg_u + 1) * 4, tt_u * 128:(tt_u + 1) * 128],
                            in_=ptv[:, 0:512].rearrange("p (h t) -> p h t", t=128)), reads=[ptb], writes=[b_qAT])

                    rot_qb = Rot([3, 4])
                    for cg in range(2):
                        WA, bWA = wq[0], b_wq[0]
                        WB, bWB = wq[1], b_wq[1]
                        if cg == 1:
                            T.op("sp", lambda e: e.dma_start(out=WA[:], in_=wq_s[1]), reads=[b_wqs[1]], writes=[bWA], dma=True)
                            T.op("sp", lambda e: e.dma_start(out=WB[:], in_=wq_s[3]), reads=[b_wqs[3]], writes=[bWB], dma=True)
                        W, bW = WA, bWA
                        for tt in range(4):
                            j = bi * 4 + tt
                            i2 = (cg * 4 + tt) % 2
                            T.op("sp", lambda e, j=j, i2=i2: e.dma_start(out=rC2[i2][:], in_=ropeC[j * 128:(j + 1) * 128, :]),
                                 writes=[b_rope2[i2]], dma=True)
                            T.op("sp", lambda e, j=j, i2=i2: e.dma_start(out=rS2[i2][:], in_=ropeS[j * 128:(j + 1) * 128, :]),
                                 writes=[b_rope2[i2]], dma=True)
                            us = qunit % 2; qunit += 1
                            qsb, qnm, qt1, qt2, qrot, b_q, st2, b_st2 = qsb_[us], qnm_[us], qt1_[us], qt2_[us], qrot_[us], b_q_[us], st2_[us], b_st2_[us]
                            pq, pqb = rot_q.get()
                            for k in range(KC):
                                T.op("pe", lambda e, pq=pq, k=k, tt=tt, W=W: e.matmul(
                                    pq[:], lhsT=hTb[:, k, tt * 128:(tt + 1) * 128], rhs=W[:, k, :], start=(k == 0), stop=(k == KC - 1)),
                                    reads=[b_hTb, bW], writes=[pqb])
                            if pendq is not None:
                                finish_unit(pendq)
                            for hh in range(4):
                                T.op("act", lambda e, pq=pq, hh=hh: e.activation(out=qsb[:, hh * 128:(hh + 1) * 128], in_=pq[:, hh * 128:(hh + 1) * 128],
                                                                                 func=AF.Square, accum_out=st2[:, hh:hh + 1]),
                                     reads=[pqb], writes=[b_q, b_st2])
                            T.op("dve", lambda e: e.tensor_scalar(out=st2[:, 4:8], in0=st2[:, 0:4], scalar1=1.0 / 128, scalar2=EPS,
                                                                  op0=ALU.mult, op1=ALU.add), reads=[b_st2], writes=[b_st2])
                            T.op("act", lambda e: e.activation(out=st2[:, 4:8], in_=st2[:, 4:8], func=AF.Ln), reads=[b_st2], writes=[b_st2])
                            T.op("act", lambda e: e.activation(out=st2[:, 8:12], in_=st2[:, 4:8], func=AF.Exp, scale=-0.5), reads=[b_st2], writes=[b_st2])
                            for hh in range(4):
                                T.op("dve", lambda e, hh=hh, pq=pq: e.scalar_tensor_tensor(
                                    out=qnm[:, hh, :], in0=pq[:, hh * 128:(hh + 1) * 128], scalar=st2[:, 8 + hh:9 + hh], in1=qn_bc[:],
                                    op0=ALU.mult, op1=ALU.mult), reads=[pqb, b_st2, b_const], writes=[b_q])
                            rope(T, qnm, qt1, qt2, qrot, rC2[i2], rS2[i2], 4, [b_q, b_rope2[i2]], [b_q])
                            pendq = (qrot, b_q, cg, tt)
                            hB = cg * 4 + tt
                            pqB, pqBb = rot_qb.get()
                            for k in range(KC):
                                T.op("pe", lambda e, pqB=pqB, k=k, tt=tt: e.matmul(
                                    pqB[:], lhsT=WB[:, k, tt * 128:(tt + 1) * 128], rhs=hTb[:, k, :], start=(k == 0), stop=(k == KC - 1)),
                                    reads=[b_hTb, bWB], writes=[pqBb])
                            T.op("act", lambda e, pqB=pqB, hB=hB: e.activation(out=qBT[:, hB, :], in_=pqB[:], func=AF.Copy, scale=SCALE),
                                 reads=[pqBb], writes=[b_qBT])
                    finish_unit(pendq)
                    if bi < 15:
                        T.op("sp", lambda e, bi=bi: e.dma_start(out=hTb[:], in_=hT_s[bi + 1]),
                             reads=b_hT[bi * 4 + 4:bi * 4 + 8], writes=[b_hTb], dma=True)
                        T.op("sp", lambda e: e.dma_start(out=wq[0][:], in_=wq_s[0]), reads=[b_wqs[0]], writes=[b_wq[0]], dma=True)
                        T.op("sp", lambda e: e.dma_start(out=wq[1][:], in_=wq_s[2]), reads=[b_wqs[2]], writes=[b_wq[1]], dma=True)
                    pB7, pB7b = banks[7], bankb[7]

                    def b_stages(tt, kvhb):
                        j = bi * 4 + tt
                        special = j in SPECIAL
                        st = []

                        def s_head(hh):
                            h = kvhb * 4 + hh
                            if hh == 0 and special:
                                si = SPECIAL.index(j)
                                T.op("sp", lambda e: e.dma_start(out=emk[:], in_=emask_d[si:si + 1, :].partition_broadcast(128)),
                                     writes=[b_emk], dma=True)
                            T.op("pe", lambda e: e.matmul(pB7[:, 0:384], lhsT=qBT[:, h, tt * 128:(tt + 1) * 128],
                                                          rhs=kbw[:, kvhb, tt * 128:tt * 128 + 384], start=True, stop=True),
                                 reads=[b_qBT, b_kbw], writes=[pB7b])
                            T.op("dve", lambda e: e.tensor_tensor(out=ssb[:, hh, :], in0=pB7[:, 0:384], in1=biasB[:, h, :], op=ALU.add),
                                 reads=[pB7b, b_bias], writes=[b_bs])
                        for hh in range(4):
                            st.append(lambda hh=hh: s_head(hh))

                        def chain():
                            if special:
                                T.op("dve", lambda e: e.tensor_tensor(out=ssb[:], in0=ssb[:], in1=emk[:].unsqueeze(1).to_broadcast([128, 4, 384]),
                                                                      op=ALU.add), reads=[b_bs, b_emk], writes=[b_bs])
                            T.op("dve", lambda e: e.tensor_reduce(out=stB[:, 0:4], in_=ssb[:], axis=AX.X, op=ALU.max, negate=True),
                                 reads=[b_bs], writes=[b_stB])
                            T.op("dve", lambda e: e.tensor_tensor(out=stB[:, 4:8], in0=stB[:, 0:4], in1=nsink_bc[:, kvhb * 4:kvhb * 4 + 4],
                                                                  op=ALU.min), reads=[b_stB, b_const], writes=[b_stB])
                            for hh in range(4):
                                T.op("act", lambda e, hh=hh: e.activation(out=ssb[:, hh, :], in_=ssb[:, hh, :], func=AF.Exp,
                                                                          bias=stB[:, 4 + hh:5 + hh], scale=1.0, accum_out=stB[:, 8 + hh:9 + hh]),
                                     reads=[b_bs, b_stB], writes=[b_bs, b_stB])
                            T.op("dve", lambda e: e.tensor_tensor(out=stB[:, 12:16], in0=stB[:, 4:8], in1=sink_bc[:, kvhb * 4:kvhb * 4 + 4],
                                                                  op=ALU.add), reads=[b_stB, b_const], writes=[b_stB])
                            T.op("act", lambda e: e.activation(out=stB[:, 12:16], in_=stB[:, 12:16], func=AF.Exp), reads=[b_stB], writes=[b_stB])
                            T.op("dve", lambda e: e.tensor_tensor(out=stB[:, 16:20], in0=stB[:, 8:12], in1=stB[:, 12:16], op=ALU.add),
                                 reads=[b_stB], writes=[b_stB])
                            T.op("dve", lambda e: e.reciprocal(out=stB[:, 20:24], in_=stB[:, 16:20]), reads=[b_stB], writes=[b_stB])
                            T.op("dve", lambda e: e.tensor_tensor(out=pbf[:], in0=ssb[:], in1=stB[:, 20:24].unsqueeze(2).to_broadcast([128, 4, 384]),
                                                                  op=ALU.mult), reads=[b_bs, b_stB], writes=[b_bp])
                        st.append(chain)

                        def transposes(half):
                            ptv = pB7[:].bitcast(BF16)
                            for hq in range(2):
                                hh = half * 2 + hq
                                for jj in range(3):
                                    T.op("pe", lambda e, hq=hq, hh=hh, jj=jj: e.transpose(
                                        out=ptv[:, (hq * 3 + jj) * 128:(hq * 3 + jj + 1) * 128], in_=pbf[:, hh, jj * 128:(jj + 1) * 128],
                                        identity=ident[:]), reads=[b_bp, b_const], writes=[pB7b])
                            T.op("dve", lambda e: e.tensor_copy(out=pTs[:, half * 6:half * 6 + 6, :],
                                                                in_=ptv[:, 0:768].rearrange("p (j t) -> p j t", t=128)),
                                 reads=[pB7b], writes=[b_bpT])
                        st.append(lambda: transposes(0))
                        st.append(lambda: transposes(1))

                        def pv():
                            for hh in range(4):
                                for jj in range(3):
                                    T.op("pe", lambda e, hh=hh, jj=jj: e.matmul(
                                        pB7[:, hh * 128:(hh + 1) * 128], lhsT=vbw[:, tt + jj, kvhb * 128:(kvhb + 1) * 128],
                                        rhs=pTs[:, hh * 3 + jj, :], start=(jj == 0), stop=(jj == 2), skip_group_check=True),
                                        reads=[b_vbw, b_bpT], writes=[pB7b])
                            T.op("dve", lambda e: e.tensor_copy(
                                out=oBT[:, kvhb * 4:(kvhb + 1) * 4, tt * 128:(tt + 1) * 128], in_=pB7[:].rearrange("p (h t) -> p h t", t=128)),
                                reads=[pB7b], writes=[b_oBT])
                        st.append(pv)
                        return st

                    BSLOT = {2: 0, 6: 1, 10: 2, 14: 3, 15: 4, 40: 5, 46: 6, 54: 7}
                    for h in range(8):
                        kvh = h // 4
                        pO, pOb = banks[3 + (h % 2)], bankb[3 + (h % 2)]
                        pD, pDb = banks[5 + (h % 2)], bankb[5 + (h % 2)]
                        pend = []
                        LA = 2
                        bst = b_stages(h % 4, h // 4)
                        if h == 1:
                            for _ in range(6):
                                if deferred:
                                    deferred.pop(0)()
                            cast_expert(bi)
                        for kb in range(NT + LA):
                            if kb in BSLOT:
                                bst[BSLOT[kb]]()
                            if kb < NT:
                                ps, psb = rot_s.get()
                                T.op("pe", lambda e, ps=ps, kvh=kvh, kb=kb, h=h: e.matmul(
                                    ps[:], lhsT=KAT[:, kvh, kb * 128:(kb + 1) * 128], rhs=qAT[:, h, :], start=True, stop=True),
                                    reads=[b_kat[kb], b_qAT], writes=[psb])
                                P_, bP_ = PT[kb % 4], b_PT[kb % 4]
                                T.op("act", lambda e, P_=P_, ps=ps: e.activation(out=P_[:], in_=ps[:], func=AF.Exp, scale=SCALE),
                                     reads=[psb], writes=[bP_])
                                pend.append((kb, P_, bP_))
                            if kb >= LA:
                                (kb0, P0, bP0) = pend.pop(0)
                                T.op("pe", lambda e, pO=pO, kb0=kb0, kvh=kvh, P0=P0: e.matmul(
                                    pO[:], lhsT=VA[:, kb0, kvh * 128:(kvh + 1) * 128], rhs=P0[:], start=(kb0 == 0), stop=(kb0 == NT - 1)),
                                    reads=[b_va[kb0], bP0], writes=[pOb])
                                T.op("pe", lambda e, pD=pD, kb0=kb0, P0=P0: e.matmul(
                                    pD[:], lhsT=ones[:], rhs=P0[:], start=(kb0 == 0), stop=(kb0 == NT - 1)),
                                    reads=[b_const, bP0], writes=[pDb])
                        R_, bR_ = rD[h % 2], b_rD[h % 2]
                        T.op("dve", lambda e, R_=R_, pD=pD: e.reciprocal(out=R_[:], in_=pD[:]), reads=[pDb], writes=[bR_])
                        T.op("dve", lambda e, R_=R_, pO=pO, h=h: e.tensor_tensor(out=oAT[:, h, :], in0=pO[:], in1=R_[:], op=ALU.mult),
                             reads=[pOb, bR_], writes=[b_oAT])
                    T.op("pool", lambda e, t0=t0: e.dma_start(out=oT_s[0, bi], in_=oAT[:]),
                         reads=[b_oAT], writes=[b_oT[bi]], dma=True)
                    T.op("pool", lambda e, t0=t0: e.dma_start(out=oT_s[1, bi], in_=oBT[:]),
                         reads=[b_oBT], writes=[b_oT[bi]], dma=True)
                while deferred:
                    deferred.pop(0)()
                T.sync_all()
                if stop == 2:
                    return nc

        b_x1 = [B("x1_%d" % j) for j in range(NOWN)]
        b_h2 = [B("h2_%d" % j) for j in range(NOWN)]
        with ExitStack() as p3:
            s3 = lambda n, s, d: p3.enter_context(nc.sbuf_tensor("sb_" + n, s, d))
            g_bc = s3("g_bc2", [128, D], F32); b_g = B("g_bc2")
            wrs = s3("wrs", [128, KC, NE], BF16); b_wrs = B("wrs")
            hTb = s3("hTb2", [128, KC, 512], BF16); b_hTb = B("hTb2")
            oAT = s3("oAT2", [128, 8, 512], BF16); oBT = s3("oBT2", [128, 8, 512], BF16); b_o = B("o2")
            mT = s3("mT", [128, KC, 512], BF16); b_mT = B("mT")
            wgs = [s3("wgs%d" % i, [128, 2, KC, 128], BF16) for i in range(3)]
            wps = [s3("wps%d" % i, [128, 2, 8, 128], BF16) for i in range(3)]
            b_ws = [B("ws%d" % i) for i in range(3)]
            wos = [s3("wos%d" % i, [128, KC, 512], BF16) for i in range(2)]; b_wos = [B("wos0"), B("wos1")]
            gsb = [s3("gsb%d" % i, [128, 512], F32) for i in range(2)]; b_gsb = [B("gsb0"), B("gsb1")]
            m1 = s3("m1", [128, 512], F32); b_m1 = B("m1")
            x1blk = [s3("x1blk%d" % i, [128, D], F32) for i in range(4)]; b_x1blk = [B("x1blk%d" % i) for i in range(4)]
            junk = s3("junk2", [128, D], BF16); b_junk = B("junk2")
            h2b = [s3("h2b%d" % i, [128, D], BF16) for i in range(2)]; b_h2b = [B("h2b0"), B("h2b1")]
            h2T = s3("h2T", [128, KC, 128], BF16); b_h2T = B("h2T")
            st = s3("st3", [128, 16], F32); b_st = B("st3")
            lg = s3("lg", [128, NE], F32); b_lg = B("lg")
            rot_a = Rot([0, 1]); rot_b = Rot([2, 3]); rot_o = Rot([4, 5]); rot_t = Rot([6, 7])

            T.op("sp", lambda e: e.dma_start(out=g_bc[:], in_=g_ffn.partition_broadcast(128)), writes=[b_g], dma=True)
            T.op("sp", lambda e: e.dma_start(out=wrs[:], in_=wr_bf.rearrange("(k p) c -> p k c", p=128)), reads=[b_wr], writes=[b_wrs], dma=True)
            nslot = 0
            for bi in range(16):
                t0 = bi * 512
                T.op("sp", lambda e, t0=t0: e.dma_start(out=hTb[:], in_=hT_s[bi]),
                     reads=b_hT[bi * 4:bi * 4 + 4], writes=[b_hTb], dma=True)
                T.op("sp", lambda e, t0=t0: e.dma_start(out=oAT[:], in_=oT_s[0, bi]),
                     reads=[b_oT[bi]], writes=[b_o], dma=True)
                T.op("sp", lambda e, t0=t0: e.dma_start(out=oBT[:], in_=oT_s[1, bi]),
                     reads=[b_oT[bi]], writes=[b_o], dma=True)
                for n in range(KC):
                    sl = nslot % 3; nslot += 1
                    WG, WP, bW = wgs[sl], wps[sl], b_ws[sl]
                    for ab in range(2):
                        T.op("sp", lambda e, WG=WG, ab=ab, n=n: e.dma_start(out=WG[:, ab], in_=wgt_s[ab * 16 + n]),
                             reads=[b_wgt[ab * 16 + n]], writes=[bW], dma=True)
                        T.op("sp", lambda e, WP=WP, ab=ab, n=n: e.dma_start(out=WP[:, ab], in_=wp_s[ab, n]),
                             reads=[b_wp[ab * 16 + n]], writes=[bW], dma=True)
                    for ab in range(2):
                        pg, pgb = rot_a.get()
                        for k in range(KC):
                            T.op("pe", lambda e, pg=pg, WG=WG, ab=ab, k=k: e.matmul(pg[:], lhsT=WG[:, ab, k, :], rhs=hTb[:, k, :],
                                                                                    start=(k == 0), stop=(k == KC - 1)),
                                 reads=[bW, b_hTb], writes=[pgb])
                        G_, bG_ = gsb[ab], b_gsb[ab]
                        T.op("act", lambda e, G_=G_, pg=pg, ab=ab, n=n: e.activation(out=G_[:], in_=pg[:], func=AF.Sigmoid,
                                                                                     bias=bgate[:, ab * 16 + n:ab * 16 + n + 1], scale=1.0),
                             reads=[pgb, b_const], writes=[bG_])
                        pp, ppb = rot_b.get()
                        osrc = oAT if ab == 0 else oBT
                        for hh in range(8):
                            T.op("pe", lambda e, pp=pp, WP=WP, ab=ab, hh=hh, osrc=osrc: e.matmul(
                                pp[:], lhsT=WP[:, ab, hh, :], rhs=osrc[:, hh, :], start=(hh == 0), stop=(hh == 7)),
                                reads=[bW, b_o], writes=[ppb])
                        if ab == 0:
                            T.op("dve", lambda e, pp=pp, G_=G_: e.tensor_tensor(out=m1[:], in0=pp[:], in1=G_[:], op=ALU.mult),
                                 reads=[ppb, bG_], writes=[b_m1])
                        else:
                            T.op("dve", lambda e, pp=pp, G_=G_: e.tensor_tensor(out=G_[:], in0=pp[:], in1=G_[:], op=ALU.mult),
                                 reads=[ppb, bG_], writes=[bG_])
                            T.op("dve", lambda e, G_=G_, n=n: e.tensor_tensor(out=mT[:, n, :], in0=m1[:], in1=G_[:], op=ALU.add),
                                 reads=[b_m1, bG_], writes=[b_mT])
                for tt in range(4):
                    j = bi * 4 + tt
                    T.op("sp", lambda e, tt=tt, j=j: e.dma_start(out=x1blk[tt][:], in_=x[j * 128:(j + 1) * 128, :]),
                         writes=[b_x1blk[tt]], dma=True)
                for cg in range(4):
                    WO, bWO = wos[cg % 2], b_wos[cg % 2]
                    T.op("sp", lambda e, WO=WO, cg=cg: e.dma_start(
                        out=WO[:], in_=wo_s[cg]), reads=[b_wosc[cg]], writes=[bWO], dma=True)
                    for tt in range(4):
                        po, pob = rot_o.get()
                        for k in range(KC):
                            T.op("pe", lambda e, po=po, k=k, tt=tt, WO=WO: e.matmul(
                                po[:], lhsT=mT[:, k, tt * 128:(tt + 1) * 128], rhs=WO[:, k, :], start=(k == 0), stop=(k == KC - 1)),
                                reads=[b_mT, bWO], writes=[pob])
                        T.op("dve", lambda e, po=po, tt=tt, cg=cg: e.tensor_tensor(
                            out=x1blk[tt][:, cg * 512:(cg + 1) * 512], in0=po[:], in1=x1blk[tt][:, cg * 512:(cg + 1) * 512], op=ALU.add),
                            reads=[pob, b_x1blk[tt]], writes=[b_x1blk[tt]])
                for tt in range(4):
                    j = bi * 4 + tt
                    XB, bXB = x1blk[tt], b_x1blk[tt]
                    if j < NOWN:
                        T.op("pool", lambda e, XB=XB, j=j: e.dma_start(out=x1_s[j * 128:(j + 1) * 128, :], in_=XB[:]),
                             reads=[bXB], writes=[b_x1[j]], dma=True)
                    T.op("act", lambda e, XB=XB: e.activation(out=junk[:], in_=XB[:], func=AF.Square, accum_out=st[:, 0:1]),
                         reads=[bXB], writes=[b_junk, b_st])
                    rstd_from_ss(st[:, 0:1], st[:, 1:2], st[:, 2:3], 1, 1.0 / D, [b_st])
                    H2, bH2 = h2b[tt % 2], b_h2b[tt % 2]
                    T.op("dve", lambda e, H2=H2, XB=XB: e.scalar_tensor_tensor(out=H2[:], in0=XB[:], scalar=st[:, 2:3], in1=g_bc[:],
                                                                                op0=ALU.mult, op1=ALU.mult),
                         reads=[bXB, b_st, b_g], writes=[bH2])
                    if j < NOWN:
                        T.op("pool", lambda e, H2=H2, j=j: e.dma_start(out=h2_s[j * 128:(j + 1) * 128, :], in_=H2[:]),
                             reads=[bH2], writes=[b_h2[j]], dma=True)
                    for q4 in range(4):
                        pb, pbb = rot_t.get()
                        pv = pb[:].bitcast(BF16)
                        for kk in range(4):
                            k = q4 * 4 + kk
                            T.op("pe", lambda e, pv=pv, kk=kk, H2=H2, k=k: e.transpose(
                                out=pv[:, kk * 128:(kk + 1) * 128], in_=H2[:, k * 128:(k + 1) * 128], identity=ident[:]),
                                reads=[bH2, b_const], writes=[pbb])
                        T.op("act", lambda e, q4=q4, pv=pv: e.copy(out=h2T[:, q4 * 4:(q4 + 1) * 4, :],
                                                                   in_=pv[:, 0:512].rearrange("p (k t) -> p k t", t=128)),
                             reads=[pbb], writes=[b_h2T])
                    pl, plb = rot_t.get()
                    for k in range(KC):
                        T.op("pe", lambda e, pl=pl, k=k: e.matmul(pl[:, 0:NE], lhsT=h2T[:, k, :], rhs=wrs[:, k, :],
                                                                  start=(k == 0), stop=(k == KC - 1)),
                             reads=[b_h2T, b_wrs], writes=[plb])
                    T.op("dve", lambda e, pl=pl: e.tensor_reduce(out=st[:, 4:5], in_=pl[:, 0:NE], axis=AX.X, op=ALU.max, negate=True),
                         reads=[plb], writes=[b_st])
                    T.op("act", lambda e, pl=pl: e.activation(out=lg[:], in_=pl[:, 0:NE], func=AF.Exp, bias=st[:, 4:5], scale=1.0,
                                                              accum_out=st[:, 5:6]), reads=[plb, b_st], writes=[b_lg, b_st])
                    T.op("dve", lambda e: e.reciprocal(out=st[:, 6:7], in_=st[:, 5:6]), reads=[b_st], writes=[b_st])
                    T.op("dve", lambda e, j=j: e.tensor_scalar(out=aff[:, j, :], in0=lg[:], scalar1=st[:, 6:7], scalar2=None, op0=ALU.mult),
                         reads=[b_lg, b_st], writes=[b_aff[j]])
            T.sync_all()
            if "aff_dbg" in dbg:
                aff_dbg = nc.dram_tensor("aff_dbg", [128, NT * NE], F32, kind="ExternalOutput").ap()
                T.op("sp", lambda e: e.dma_start(out=aff_dbg, in_=aff[:].rearrange("p j e -> p (j e)")), reads=b_aff, writes=[B("affd")], dma=True)
                T.sync_all()
            if stop == 3:
                return nc

        b_Xg = [B("Xg%d" % i) for i in range(NE)]
        b_Yg = [B("Yg%d" % i) for i in range(NE)]
        with ExitStack() as p4:
            s4 = lambda n, s, d: p4.enter_context(nc.sbuf_tensor("sb_" + n, s, d))
            gt = s4("gt", [128, NOWN, NE], F32); b_gt = B("gt")
            idx_i = s4("idx_i", [128, NOWN * NE], I32); b_idx = B("idx")
            bc_reg = nc.gpsimd.to_reg(CAP - 1)
            with ExitStack() as p4a:
                sa = lambda n, s, d: p4a.enter_context(nc.sbuf_tensor("sb_" + n, s, d))
                cmp_ = sa("cmp", [128, NT, NE], F32); b_cmp = B("cmp")
                lo = sa("lo", [128, NE], F32); mid = sa("mid", [128, NE], F32); ge = sa("ge", [128, NE], F32)
                cntp = sa("cntp", [128, NE], BF16); b_bis = B("bis")
                msk = sa("msk", [128, NOWN, NE], F32); mskb = sa("mskb", [128, NOWN * NE], BF16); b_msk = B("msk")
                cc = sa("cc", [128, NOWN, NE], F32); offs = sa("offs", [128, NOWN, NE], F32); b_offs = B("offs")
                posf = sa("posf", [128, NOWN * NE], F32); tmpf = sa("tmpf", [128, NOWN * NE], F32); b_pos = B("pos")
                pc, pcb = banks[0], bankb[0]
                T.op("dve", lambda e: e.memset(lo[:], 0.0), writes=[b_bis])
                for it in range(NBISECT):
                    c_ = 2.0 ** (-(it + 1))
                    T.op("dve", lambda e, c_=c_: e.tensor_scalar(out=mid[:], in0=lo[:], scalar1=c_, scalar2=None, op0=ALU.add),
                         reads=[b_bis], writes=[b_bis])
                    T.op("dve", lambda e: e.tensor_tensor(out=cmp_[:], in0=aff[:], in1=mid[:].unsqueeze(1).to_broadcast([128, NT, NE]),
                                                          op=ALU.is_ge), reads=b_aff + [b_bis], writes=[b_cmp])
                    with nc.allow_low_precision(reason="integer counts <= 64 are exact in bf16"):
                        T.op("dve", lambda e: e.tensor_reduce(out=cntp[:], in_=cmp_[:].rearrange("p j e -> p e j"), axis=AX.X, op=ALU.add),
                             reads=[b_cmp], writes=[b_bis])
                    T.op("pe", lambda e: e.matmul(pc[:, 0:NE], lhsT=ones[:], rhs=cntp[:], start=True, stop=True),
                         reads=[b_bis, b_const], writes=[pcb])
                    T.op("dve", lambda e: e.tensor_scalar(out=ge[:], in0=pc[:, 0:NE], scalar1=float(CAP) - 0.5, scalar2=None, op0=ALU.is_ge),
                         reads=[pcb], writes=[b_bis])
                    T.op("dve", lambda e, c_=c_: e.scalar_tensor_tensor(out=lo[:], in0=ge[:], scalar=c_, in1=lo[:], op0=ALU.mult, op1=ALU.add),
                         reads=[b_bis], writes=[b_bis])
                T.op("dve", lambda e: e.tensor_tensor(out=msk[:], in0=aff[:, 0:NOWN, :], in1=lo[:].unsqueeze(1).to_broadcast([128, NOWN, NE]),
                                                      op=ALU.is_ge), reads=b_aff + [b_bis], writes=[b_msk])
                T.op("dve", lambda e: e.tensor_tensor(out=gt[:], in0=aff[:, 0:NOWN, :], in1=msk[:], op=ALU.mult), reads=b_aff + [b_msk], writes=[b_gt])
                T.op("dve", lambda e: e.tensor_copy(out=mskb[:], in_=msk[:].rearrange("p j e -> p (j e)")), reads=[b_msk], writes=[b_msk])
                pp_, ppb_ = banks[1], bankb[1]
                pc2, pc2b = banks[2], bankb[2]
                T.op("pe", lambda e: e.matmul(pp_[:], lhsT=triU[:], rhs=mskb[:], start=True, stop=True), reads=[b_msk, b_const], writes=[ppb_])
                T.op("pe", lambda e: e.matmul(pc2[:], lhsT=ones[:], rhs=mskb[:], start=True, stop=True), reads=[b_msk, b_const], writes=[pc2b])
                T.op("act", lambda e: e.copy(out=cc[:], in_=pc2[:].rearrange("p (j e) -> p j e", e=NE)), reads=[pc2b], writes=[b_offs])
                T.op("dve", lambda e: e.memset(offs[:, 0, :], 0.0), reads=[b_offs], writes=[b_offs])
                for j in range(1, NOWN):
                    T.op("dve", lambda e, j=j: e.tensor_tensor(out=offs[:, j, :], in0=offs[:, j - 1, :], in1=cc[:, j - 1, :], op=ALU.add),
                         reads=[b_offs], writes=[b_offs])
                T.op("dve", lambda e: e.tensor_tensor(out=posf[:], in0=pp_[:], in1=offs[:].rearrange("p j e -> p (j e)"), op=ALU.add),
                     reads=[ppb_, b_offs], writes=[b_pos])
                T.op("dve", lambda e: e.tensor_scalar(out=tmpf[:], in0=msk[:].rearrange("p j e -> p (j e)"), scalar1=-4096.0, scalar2=4096.0,
                                                      op0=ALU.mult, op1=ALU.add), reads=[b_msk], writes=[b_pos])
                T.op("dve", lambda e: e.tensor_tensor(out=posf[:], in0=posf[:], in1=tmpf[:], op=ALU.add), reads=[b_pos], writes=[b_pos])
                T.op("dve", lambda e: e.tensor_copy(out=idx_i[:], in_=posf[:]), reads=[b_pos], writes=[b_idx])
                T.sync_all()
            with ExitStack() as p4b:
                sbx = lambda n, s, d: p4b.enter_context(nc.sbuf_tensor("sb_" + n, s, d))
                Wg = sbx("Wg", [128, KC, FF], BF16); Wu = sbx("Wu", [128, KC, FF], BF16); Wd = sbx("Wd", [128, 8, D], BF16)
                b_Wg = B("Wg"); b_Wu = B("Wu"); b_Wd = B("Wd")
                XgT = sbx("XgT", [128, KC, CAP], BF16); b_XgT = [B("XgT%d" % i) for i in range(8)]
                HT = sbx("HT", [128, 8, CAP], BF16); b_HT = [B("HT0"), B("HT1")]
                xg = [sbx("xg%d" % i, [128, D], BF16) for i in range(2)]; b_xg = [B("xg0"), B("xg1")]
                ysb = [sbx("ysb%d" % i, [128, D], BF16) for i in range(2)]; b_ysb = [B("ysb0"), B("ysb1")]
                sg = [sbx("sg%d" % i, [128, 512], F32) for i in range(2)]; b_sg = [B("sg0"), B("sg1")]
                rot_t = Rot([0, 1]); rot_a = Rot([2, 3]); rot_u = Rot([4, 5]); rot_y = Rot([6, 7])
                hsc = [sbx("hsc%d" % i, [128, D], BF16) for i in range(4)]; b_hsc = [B("hsc%d" % i) for i in range(4)]
                sc_tok = [[] for _ in range(NE)]
                nsc = [0]

                def scatter_expert(ex):
                    for j in range(NOWN):
                        Hs, bHs = hsc[nsc[0] % 4], b_hsc[nsc[0] % 4]; nsc[0] += 1
                        T.op("sp", lambda e, Hs=Hs, j=j: e.dma_start(out=Hs[:], in_=h2_s[j * 128:(j + 1) * 128, :]),
                             reads=[b_h2[j]], writes=[bHs], dma=True)
                        sc_tok[ex].append(T.op("pool", lambda e, Hs=Hs, j=j: e.indirect_dma_start(
                            out=Xg[ex], out_offset=bass.IndirectOffsetOnAxis(ap=idx_i[:, j * NE + ex:j * NE + ex + 1], axis=0),
                            in_=Hs[:, :], in_offset=None, bounds_check=bc_reg, oob_is_err=False),
                            reads=[bHs, b_idx], writes=[], dma=True, nd=160))

                def load_w(ex, which):
                    if "g" in which:
                        T.op("sp", lambda e: e.dma_start(out=Wg[:], in_=wg_bf[ex].rearrange("(k p) f -> p k f", p=128)),
                             reads=[b_wg[ex]], writes=[b_Wg], dma=True)
                        T.op("sp", lambda e: e.dma_start(out=Wu[:], in_=wu_bf[ex].rearrange("(k p) f -> p k f", p=128)),
                             reads=[b_wu[ex]], writes=[b_Wu], dma=True)
                    if "d" in which:
                        T.op("sp", lambda e: e.dma_start(out=Wd[:], in_=wd_bf[ex].rearrange("(k p) f -> p k f", p=128)),
                             reads=[b_wd[ex]], writes=[b_Wd], dma=True)

                load_w(0, "gd")
                scatter_expert(0)
                for e_ in range(NE):
                    for tok in sc_tok[e_]:
                        T._wait("sp", tok[0], tok[1])
                    for st_ in range(8):
                        XGt, bXGt = xg[st_ % 2], b_xg[st_ % 2]
                        T.op("sp", lambda e, XGt=XGt, e_=e_, st_=st_: e.dma_start(out=XGt[:], in_=Xg[e_][st_ * 128:(st_ + 1) * 128, :]),
                             reads=[b_Xg[e_]], writes=[bXGt], dma=True)
                        for q4 in range(4):
                            pb, pbb = rot_t.get()
                            pv = pb[:].bitcast(BF16)
                            for kk in range(4):
                                k = q4 * 4 + kk
                                T.op("pe", lambda e, pv=pv, kk=kk, XGt=XGt, k=k: e.transpose(
                                    out=pv[:, kk * 128:(kk + 1) * 128], in_=XGt[:, k * 128:(k + 1) * 128], identity=ident[:]),
                                    reads=[bXGt, b_const], writes=[pbb])
                            if q4 % 2 == 0:
                                T.op("act", lambda e, pv=pv, q4=q4, st_=st_: e.copy(
                                    out=XgT[:, q4 * 4:(q4 + 1) * 4, st_ * 128:(st_ + 1) * 128],
                                    in_=pv[:, 0:512].rearrange("p (k t) -> p k t", t=128)), reads=[pbb], writes=[b_XgT[st_]])
                            else:
                                T.op("dve", lambda e, pv=pv, q4=q4, st_=st_: e.tensor_copy(
                                    out=XgT[:, q4 * 4:(q4 + 1) * 4, st_ * 128:(st_ + 1) * 128],
                                    in_=pv[:, 0:512].rearrange("p (k t) -> p k t", t=128)), reads=[pbb], writes=[b_XgT[st_]])
                    if e_ + 1 < NE:
                        scatter_expert(e_ + 1)
                    for sh in range(2):
                        for fc in range(8):
                            pa, pab = rot_a.get(); pu, pub = rot_u.get()
                            for k in range(KC):
                                T.op("pe", lambda e, pa=pa, k=k, fc=fc, sh=sh: e.matmul(
                                    pa[:], lhsT=Wg[:, k, fc * 128:(fc + 1) * 128], rhs=XgT[:, k, sh * 512:(sh + 1) * 512],
                                    start=(k == 0), stop=(k == KC - 1)), reads=[b_Wg] + b_XgT[sh * 4:sh * 4 + 4], writes=[pab])
                            for k in range(KC):
                                T.op("pe", lambda e, pu=pu, k=k, fc=fc, sh=sh: e.matmul(
                                    pu[:], lhsT=Wu[:, k, fc * 128:(fc + 1) * 128], rhs=XgT[:, k, sh * 512:(sh + 1) * 512],
                                    start=(k == 0), stop=(k == KC - 1)), reads=[b_Wu] + b_XgT[sh * 4:sh * 4 + 4], writes=[pub])
                            SG, bSG = sg[fc % 2], b_sg[fc % 2]
                            T.op("act", lambda e, SG=SG, pa=pa: e.activation(out=SG[:], in_=pa[:], func=AF.Silu), reads=[pab], writes=[bSG])
                            T.op("dve", lambda e, SG=SG, pu=pu, fc=fc, sh=sh: e.tensor_tensor(
                                out=HT[:, fc, sh * 512:(sh + 1) * 512], in0=pu[:], in1=SG[:], op=ALU.mult),
                                reads=[pub, bSG], writes=[b_HT[sh]])
                    if e_ + 1 < NE:
                        load_w(e_ + 1, "g")
                    for st_ in range(8):
                        Y, bY = ysb[st_ % 2], b_ysb[st_ % 2]
                        for dg in range(4):
                            py, pyb = rot_y.get()
                            for fc in range(8):
                                T.op("pe", lambda e, py=py, fc=fc, st_=st_, dg=dg: e.matmul(
                                    py[:], lhsT=HT[:, fc, st_ * 128:(st_ + 1) * 128], rhs=Wd[:, fc, dg * 512:(dg + 1) * 512],
                                    start=(fc == 0), stop=(fc == 7)), reads=[b_HT[st_ // 4], b_Wd], writes=[pyb])
                            if dg % 2 == 0:
                                T.op("act", lambda e, Y=Y, py=py, dg=dg: e.copy(out=Y[:, dg * 512:(dg + 1) * 512], in_=py[:]),
                                     reads=[pyb], writes=[bY])
                            else:
                                T.op("dve", lambda e, Y=Y, py=py, dg=dg: e.tensor_copy(out=Y[:, dg * 512:(dg + 1) * 512], in_=py[:]),
                                     reads=[pyb], writes=[bY])
                        T.op("sp", lambda e, Y=Y, e_=e_, st_=st_: e.dma_start(out=Yg[e_][st_ * 128:(st_ + 1) * 128, :], in_=Y[:]),
                             reads=[bY], writes=[b_Yg[e_]], dma=True)
                    if e_ + 1 < NE:
                        load_w(e_ + 1, "d")
                T.sync_all()
                if stop == 5:
                    return nc
            with ExitStack() as p4c:
                sc = lambda n, s, d: p4c.enter_context(nc.sbuf_tensor("sb_" + n, s, d))
                g_bc = sc("g_bc3", [128, D], F32); b_g = B("g_bc3")
                acc = [sc("acc%d" % i, [128, D], F32) for i in range(2)]; b_acc = [B("acc0"), B("acc1")]
                NG = 12
                G = [sc("G%d" % i, [128, D], BF16) for i in range(NG)]; b_G = [B("G%d" % i) for i in range(NG)]
                dgm = [sc("dgm%d" % i, [128, 128], BF16) for i in range(NG)]; b_dgm = [B("dgm%d" % i) for i in range(NG)]
                ob = [sc("ob%d" % i, [128, D], F32) for i in range(2)]; b_ob = [B("ob0"), B("ob1")]
                junk = sc("junk3", [128, D], BF16); b_junk = B("junk3")
                st = sc("st4", [128, 8], F32); b_st = B("st4")
                T.op("sp", lambda e: e.dma_start(out=g_bc[:], in_=g_fin.partition_broadcast(128)), writes=[b_g], dma=True)
                for i in range(NG):
                    T.op("dve", lambda e, i=i: e.memset(G[i][:], 0.0), writes=[b_G[i]])
                ng = 0
                b_out = B("out")
                for j in range(NOWN):
                    A, bA = acc[j % 2], b_acc[j % 2]
                    T.op("sp", lambda e, A=A, j=j: e.dma_start(out=A[:], in_=x1_s[j * 128:(j + 1) * 128, :]), reads=[b_x1[j]], writes=[bA], dma=True)
                    pbk = [(banks[(j % 2) * 4 + c], bankb[(j % 2) * 4 + c]) for c in range(4)]
                    for e_ in range(NE):
                        Gb, bGb = G[ng % NG], b_G[ng % NG]
                        Dg, bDg = dgm[ng % NG], b_dgm[ng % NG]; ng += 1
                        T.op("pool", lambda e, Gb=Gb, j=j, e_=e_: e.indirect_dma_start(
                            out=Gb[:, :], out_offset=None, in_=Yg[e_],
                            in_offset=bass.IndirectOffsetOnAxis(ap=idx_i[:, j * NE + e_:j * NE + e_ + 1], axis=0),
                            bounds_check=bc_reg, oob_is_err=False), reads=[b_Yg[e_], b_idx], writes=[bGb], dma=True, nd=160)
                        T.op("dve", lambda e, Dg=Dg, j=j, e_=e_: e.tensor_scalar(out=Dg[:], in0=ident[:], scalar1=gt[:, j, e_:e_ + 1], scalar2=None,
                                                                                op0=ALU.mult), reads=[b_const, b_gt], writes=[bDg])
                        for c in range(4):
                            T.op("pe", lambda e, c=c, Dg=Dg, Gb=Gb, e_=e_: e.matmul(
                                pbk[c][0][:], lhsT=Dg[:], rhs=Gb[:, c * 512:(c + 1) * 512], start=(e_ == 0), stop=(e_ == NE - 1)),
                                reads=[bDg, bGb], writes=[pbk[c][1]])
                    for c in range(4):
                        T.op("dve", lambda e, c=c, A=A: e.tensor_tensor(out=A[:, c * 512:(c + 1) * 512], in0=pbk[c][0][:],
                                                                         in1=A[:, c * 512:(c + 1) * 512], op=ALU.add),
                             reads=[pbk[c][1], bA], writes=[bA])
                    T.op("act", lambda e, A=A: e.activation(out=junk[:], in_=A[:], func=AF.Square, accum_out=st[:, 0:1]),
                         reads=[bA], writes=[b_junk, b_st])
                    rstd_from_ss(st[:, 0:1], st[:, 1:2], st[:, 2:3], 1, 1.0 / D, [b_st])
                    O_, bO_ = ob[j % 2], b_ob[j % 2]
                    T.op("dve", lambda e, O_=O_, A=A: e.scalar_tensor_tensor(out=O_[:], in0=A[:], scalar=st[:, 2:3], in1=g_bc[:],
                                                                            op0=ALU.mult, op1=ALU.mult), reads=[bA, b_st, b_g], writes=[bO_])
                    T.op("sp", lambda e, O_=O_, j=j: e.dma_start(out=out[j * 128:(j + 1) * 128, :], in_=O_[:]), reads=[bO_], writes=[b_out], dma=True)
                T.sync_all()
        print("ops:", T.cnt, "dmas:", T.dcnt, "waits:", T.nwait, "sems:", len(T.sems))
    return nc
```

```python
import math
from contextlib import ExitStack
import numpy as np
import ml_dtypes
import concourse.bass as bass
import concourse.mybir as mybir
from concourse.bass_utils import run_bass_kernel_spmd

F32 = mybir.dt.float32
BF16 = mybir.dt.bfloat16
I32 = mybir.dt.int32
AF = mybir.ActivationFunctionType
ALU = mybir.AluOpType
AX = mybir.AxisListType

S = 8192
D = 2048
NT = 64
NOWN = 32
KC = 16
NE = 16
CAP = 1024
FF = 1024
EPS = 1e-6
SCALE = 1.0 / math.sqrt(128.0)
NEG = -1e30
NBISECT = 26
SPECIAL = [0, 31, 32, 63]


class Buf:
    __slots__ = ("name", "w", "r", "excl")

    def __init__(self, name, excl=False):
        self.name = name
        self.w = None
        self.r = {}
        self.excl = excl


class Trk:
    EPOCH = 12000
    RING = 16

    def __init__(self, nc, stack):
        self.nc = nc
        self.stack = stack
        self.eng = {"pe": nc.tensor, "act": nc.scalar, "dve": nc.vector, "pool": nc.gpsimd, "sp": nc.sync}
        self.sems = {}
        self.cnt = {e: 0 for e in self.eng}
        self.dcnt = {e: 0 for e in self.eng}
        self.obs = {e: {} for e in self.eng}
        self.owner = {}
        self.latest = {}
        self.nwait = 0
        self.pq = []
        self.pq_sum = 0
        self.nop = 0
        self.limit = None
        self.log = []

    def _sem(self, key, owner):
        if key not in self.sems:
            self.sems[key] = self.stack.enter_context(self.nc.semaphore("s_%s_%s" % key))
            self.owner[key] = owner
        return self.sems[key]

    def _wait(self, e, key, val):
        if self.obs[e].get(key, 0) >= val:
            return
        self.eng[e].wait_ge(self.sems[key], val)
        self.obs[e][key] = val
        self.nwait += 1

    def op(self, e, fn, reads=(), writes=(), dma=False, nd=2048):
        self.nop += 1
        if self.limit is not None and self.nop > self.limit:
            return None
        if self.limit is not None:
            import inspect
            self.log.append((self.nop, e, inspect.stack()[1].lineno))
        deps = {}
        if dma and e == "pool":
            while self.pq and self.pq_sum + nd > 9000:
                otok, ond = self.pq.pop(0)
                self._wait("pool", otok[0], otok[1])
                self.pq_sum -= ond

        def add(tok):
            if tok is None:
                return
            k, v = tok
            if deps.get(k, 0) < v:
                deps[k] = v

        for b in reads:
            add(b.w)
            if b.excl:
                for k, v in b.r.items():
                    if self.owner[k][0] != e:
                        add((k, v))
        for b in writes:
            add(b.w)
            for k, v in b.r.items():
                add((k, v))
        for k, v in deps.items():
            if e == "pe" and self.owner[k] == ("pe", False):
                continue
            self._wait(e, k, v)
        if dma:
            n = self.dcnt[e]
            self.dcnt[e] += 1
            key = ("d" + e, n % self.RING)
            val = 16 * (n // self.RING + 1)
            sem = self._sem(key, (e, True))
            fn(self.eng[e]).then_inc(sem, 16)
        else:
            n = self.cnt[e]
            self.cnt[e] += 1
            key = (e, n // self.EPOCH)
            val = n % self.EPOCH + 1
            sem = self._sem(key, (e, False))
            fn(self.eng[e]).then_inc(sem, 1)
        tok = (key, val)
        if dma and e == "pool":
            self.pq.append((tok, nd))
            self.pq_sum += nd
        self.latest[key] = val
        for b in reads:
            if b.r.get(key, 0) < val:
                b.r[key] = val
        for b in writes:
            b.w = tok
            b.r = {}
        return tok

    def sync_all(self, engines=None):
        for e in (engines or list(self.eng)):
            for k, v in self.latest.items():
                self._wait(e, k, v)


def rope(T, src, t1, t2, dst, C, S_, nh, reads, writes):
    T.op("dve", lambda e: e.tensor_tensor(out=t1[:], in0=src[:], in1=C[:].unsqueeze(1).to_broadcast([128, nh, 128]), op=ALU.mult),
         reads=reads, writes=writes)
    s5 = src[:].rearrange("p h (a b c) -> p h a b c", a=2, b=2, c=32)
    t5 = t2[:].rearrange("p h (a b c) -> p h a b c", a=2, b=2, c=32)
    S4 = S_[:].rearrange("p (a b c) -> p a b c", a=2, b=2, c=32)
    for b in range(2):
        T.op("dve", lambda e, b=b: e.tensor_tensor(out=t5[:, :, :, b, :], in0=s5[:, :, :, 1 - b, :],
                                                   in1=S4[:, :, b, :].unsqueeze(1).to_broadcast([128, nh, 2, 32]), op=ALU.mult),
             reads=reads, writes=writes)
    T.op("dve", lambda e: e.tensor_tensor(out=dst[:], in0=t1[:], in1=t2[:], op=ALU.add), reads=reads, writes=writes)


def kernel(x, g_mix, w_in, b_gate, qn_a, kn_a, w_proj_a, sink_b, rel_bias, w_proj_b,
           w_o, g_ffn, w_router, w_gate_e, w_up_e, w_down_e, g_final):
    nc = build_program()
    in_maps = make_inputs(x, g_mix, w_in, b_gate, qn_a, kn_a, w_proj_a, sink_b, rel_bias, w_proj_b,
                          w_o, g_ffn, w_router, w_gate_e, w_up_e, w_down_e, g_final)
    res = run_bass_kernel_spmd(nc, in_maps, core_ids=list(range(8)))
    out = np.empty((4, S, D), np.float32)
    for c in range(8):
        b, hf = c // 2, c % 2
        out[b, hf * 4096:(hf + 1) * 4096] = res.results[c]["out"]
    return out


def _t5_bucket(rel):
    half = 16
    ret = np.where(rel > 0, half, 0)
    n = np.abs(rel)
    max_exact = 8
    nf = np.maximum(n, 1).astype(np.float32)
    large = max_exact + (np.log(nf / max_exact) / math.log(128 / max_exact) * (half - max_exact)).astype(np.int32)
    large = np.minimum(large, half - 1)
    return ret + np.where(n < max_exact, n, large)


def make_inputs(x, g_mix, w_in, b_gate, qn_a, kn_a, w_proj_a, sink_b, rel_bias, w_proj_b,
                w_o, g_ffn, w_router, w_gate_e, w_up_e, w_down_e, g_final):
    f = lambda a: np.ascontiguousarray(np.asarray(a, dtype=np.float32))
    x = f(x)
    pos = np.arange(S)
    r = (pos // 64).astype(np.float32)
    c = (pos % 64).astype(np.float32)
    inv = (1.0 / (10000.0 ** (np.arange(0, 64, 2, dtype=np.float32) / 64.0))).astype(np.float32)
    ar = r[:, None] * inv[None, :]
    ac = c[:, None] * inv[None, :]
    ropeC = np.concatenate([np.cos(ar), np.cos(ar), np.cos(ac), np.cos(ac)], axis=1).astype(np.float32)
    ropeS = np.concatenate([-np.sin(ar), np.sin(ar), -np.sin(ac), np.sin(ac)], axis=1).astype(np.float32)
    rel = (np.arange(384) - 128)[None, :] - np.arange(128)[:, None]
    bucket = _t5_bucket(rel)
    band = np.where(np.abs(rel) <= 128, 0.0, NEG).astype(np.float32)
    biasB = np.ascontiguousarray(f(rel_bias)[bucket].transpose(0, 2, 1))
    ident = np.eye(128, dtype=np.float32).astype(ml_dtypes.bfloat16)
    triU = np.triu(np.ones((128, 128), np.float32), 1).astype(ml_dtypes.bfloat16)
    bg = np.ascontiguousarray(f(b_gate).reshape(32, 128).T)
    shared = {
        "w_in": f(w_in)[0], "w_pa": f(w_proj_a)[0], "w_pb": f(w_proj_b)[0], "w_o": f(w_o)[0],
        "w_r": f(w_router)[0], "w_g": f(w_gate_e)[0], "w_u": f(w_up_e)[0], "w_d": f(w_down_e)[0],
        "g_mix": f(g_mix).reshape(1, D), "g_ffn": f(g_ffn).reshape(1, D), "g_fin": f(g_final).reshape(1, D),
        "qn": f(qn_a).reshape(1, 128), "kn": f(kn_a).reshape(1, 128), "sink": f(sink_b).reshape(1, 8),
        "bgate": bg, "biasB": biasB, "band": band, "ident": ident, "triU": triU,
    }
    maps = []
    for core in range(8):
        b, hf = core // 2, core % 2
        perm = (np.arange(S) + hf * 4096) % S
        em = np.zeros((4, 384), np.float32)
        for i, t in enumerate(SPECIAL):
            orig = (t + hf * 32) % 64
            if orig == 0:
                em[i, 0:128] = NEG
            if orig == 63:
                em[i, 256:384] = NEG
        m = dict(shared)
        m["x"] = np.ascontiguousarray(x[b][perm])
        m["ropeC"] = np.ascontiguousarray(ropeC[perm])
        m["ropeS"] = np.ascontiguousarray(ropeS[perm])
        m["emask"] = em
        maps.append(m)
    return maps


def build_program(stop=None, dbg=(), nt1=NT, flags=(), limit=None):
    nc = bass.Bass("TRN2", target_bir_lowering=False)
    dt_in = lambda n, s, d=F32: nc.dram_tensor(n, s, d, kind="ExternalInput").ap()
    dt_sc = lambda n, s, d: nc.dram_tensor(n, s, d, kind=("ExternalOutput" if n in dbg else "Internal")).ap()
    x = dt_in("x", [S, D])
    w_in = dt_in("w_in", [D, 7168]); w_pa = dt_in("w_pa", [1024, D]); w_pb = dt_in("w_pb", [1024, D])
    w_o = dt_in("w_o", [D, D]); w_r = dt_in("w_r", [D, NE])
    w_g = dt_in("w_g", [NE, D, FF]); w_u = dt_in("w_u", [NE, D, FF]); w_d = dt_in("w_d", [NE, FF, D])
    g_mix = dt_in("g_mix", [1, D]); g_ffn = dt_in("g_ffn", [1, D]); g_fin = dt_in("g_fin", [1, D])
    qn = dt_in("qn", [1, 128]); kn = dt_in("kn", [1, 128]); sink = dt_in("sink", [1, 8])
    bgate_d = dt_in("bgate", [128, 32]); biasB_d = dt_in("biasB", [128, 8, 384]); band_d = dt_in("band", [128, 384])
    ident_d = dt_in("ident", [128, 128], BF16); triU_d = dt_in("triU", [128, 128], BF16)
    ropeC = dt_in("ropeC", [S, 128]); ropeS = dt_in("ropeS", [S, 128]); emask_d = dt_in("emask", [4, 384])
    out = nc.dram_tensor("out", [4096, D], F32, kind="ExternalOutput").ap()

    w_in_bf = dt_sc("w_in_bf", [D, 7168], BF16)
    wgt_s = dt_sc("wgt_s", [32, 128, KC, 128], BF16)
    wp_s = dt_sc("wp_s", [2, 16, 128, 8, 128], BF16)
    wr_bf = dt_sc("wr_bf", [D, NE], BF16)
    wg_bf = dt_sc("wg_bf", [NE, D, FF], BF16); wu_bf = dt_sc("wu_bf", [NE, D, FF], BF16)
    wd_bf = dt_sc("wd_bf", [NE, FF, D], BF16)
    hT_s = dt_sc("hT_s", [16, 128, KC, 512], BF16)
    wq_s = dt_sc("wq_s", [4, 128, KC, 512], BF16)
    wo_s = dt_sc("wo_s", [4, 128, KC, 512], BF16)
    kbT_s = dt_sc("kbT_s", [2, 128, S], BF16)
    vb_s = dt_sc("vb_s", [S, 256], BF16)
    oT_s = dt_sc("oT_s", [2, 16, 128, 8, 512], BF16)
    x1_s = dt_sc("x1_s", [4096, D], F32)
    h2_s = dt_sc("h2_s", [4096, D], BF16)
    Xg = [dt_sc("Xg%d" % i, [CAP, D], BF16) for i in range(NE)]
    Yg = [dt_sc("Yg%d" % i, [CAP, D], BF16) for i in range(NE)]

    with ExitStack() as stack:
        T = Trk(nc, stack)
        T.limit = limit
        build_program.T = T
        sb = lambda n, s, d: stack.enter_context(nc.sbuf_tensor("sb_" + n, s, d))
        B = Buf

        banks = [stack.enter_context(nc.psum_tensor("ps%d" % i, [128, 512], F32)) for i in range(8)]
        bankb = [B("bank%d" % i, True) for i in range(8)]

        class Rot:
            def __init__(self, ids):
                self.ids = ids; self.i = 0

            def get(self):
                k = self.ids[self.i % len(self.ids)]; self.i += 1
                return banks[k], bankb[k]

        b_win = [B("win%d" % i) for i in range(4)]
        for i in range(4):
            T.op("pool", lambda e, i=i: e.dma_start(
                out=w_in_bf[i * 512:(i + 1) * 512, :].rearrange("r (a c) -> r a c", c=1024),
                in_=w_in[i * 512:(i + 1) * 512, :].rearrange("r (a c) -> r a c", c=1024)), writes=[b_win[i]], dma=True, nd=3584)
        b_wr = B("wr")
        b_wqs = [B("wqs%d" % i) for i in range(4)]; b_wosc = [B("wos_s%d" % i) for i in range(4)]
        b_wgt = [B("wgt%d" % i) for i in range(32)]
        b_wp = [B("wp%d" % i) for i in range(32)]
        for g_, c0 in enumerate((0, 512, 1536, 2048)):
            T.op("pool", lambda e, g_=g_, c0=c0: e.dma_start(
                out=wq_s[g_], in_=w_in[:, c0:c0 + 512].rearrange("(k p) c -> p k c", p=128)), writes=[b_wqs[g_]], dma=True)
        deferred = []
        for g_ in range(4):
            deferred.append(lambda g_=g_: T.op("pool", lambda e: e.dma_start(
                out=wo_s[g_], in_=w_o[:, g_ * 512:(g_ + 1) * 512].rearrange("(k p) c -> p k c", p=128)), writes=[b_wosc[g_]], dma=True))
        deferred.append(lambda: T.op("pool", lambda e: e.dma_start(out=wr_bf, in_=w_r), writes=[b_wr], dma=True))
        for n_ in range(32):
            deferred.append(lambda n_=n_: T.op("pool", lambda e: e.dma_start(
                out=wgt_s[n_], in_=w_in[:, 3072 + n_ * 128:3072 + (n_ + 1) * 128].rearrange("(k p) c -> p k c", p=128)),
                writes=[b_wgt[n_]], dma=True))
        for ab, wsrc in enumerate((w_pa, w_pb)):
            for n_ in range(16):
                deferred.append(lambda ab=ab, n_=n_, wsrc=wsrc: T.op("pool", lambda e: e.dma_start(
                    out=wp_s[ab, n_], in_=wsrc[:, n_ * 128:(n_ + 1) * 128].rearrange("(h p) c -> p h c", p=128)),
                    writes=[b_wp[ab * 16 + n_]], dma=True))
        b_wg = [B("wg%d" % i) for i in range(NE)]; b_wu = [B("wu%d" % i) for i in range(NE)]
        b_wd = [B("wd%d" % i) for i in range(NE)]

        def cast_expert(e_):
            T.op("pool", lambda e: e.dma_start(out=wg_bf[e_], in_=w_g[e_]), writes=[b_wg[e_]], dma=True)
            T.op("pool", lambda e: e.dma_start(out=wu_bf[e_], in_=w_u[e_]), writes=[b_wu[e_]], dma=True)
            T.op("pool", lambda e: e.dma_start(out=wd_bf[e_].rearrange("r (a c) -> r a c", c=1024),
                                               in_=w_d[e_].rearrange("r (a c) -> r a c", c=1024)),
                 writes=[b_wd[e_]], dma=True)

        ident = sb("ident", [128, 128], BF16); ones = sb("ones", [128, 128], BF16); triU = sb("triU", [128, 128], BF16)
        qn_bc = sb("qn_bc", [128, 128], F32); kn_bc = sb("kn_bc", [128, 128], F32)
        bgate = sb("bgate", [128, 32], F32); sink_bc = sb("sink_bc", [128, 8], F32); nsink_bc = sb("nsink_bc", [128, 8], F32)
        aff = sb("aff", [128, NT, NE], F32)
        b_const = B("const"); b_aff = [B("aff%d" % j) for j in range(NT)]
        T.op("sp", lambda e: e.dma_start(out=ident[:], in_=ident_d), writes=[b_const], dma=True)
        T.op("sp", lambda e: e.dma_start(out=triU[:], in_=triU_d), writes=[b_const], dma=True)
        T.op("sp", lambda e: e.dma_start(out=qn_bc[:], in_=qn.partition_broadcast(128)), writes=[b_const], dma=True)
        T.op("sp", lambda e: e.dma_start(out=kn_bc[:], in_=kn.partition_broadcast(128)), writes=[b_const], dma=True)
        T.op("sp", lambda e: e.dma_start(out=bgate[:], in_=bgate_d), writes=[b_const], dma=True)
        T.op("sp", lambda e: e.dma_start(out=sink_bc[:], in_=sink.partition_broadcast(128)), writes=[b_const], dma=True)
        T.op("dve", lambda e: e.memset(ones[:], 1.0), writes=[b_const])
        T.op("dve", lambda e: e.tensor_scalar(out=nsink_bc[:], in0=sink_bc[:], scalar1=-1.0, scalar2=None, op0=ALU.mult),
             reads=[b_const], writes=[b_const])

        if stop == 0:
            T.sync_all()
            return nc

        def rstd_from_ss(ss, tmp, rstd, n, inv_n, bufs):
            T.op("dve", lambda e: e.tensor_scalar(out=tmp[:, 0:n], in0=ss[:, 0:n], scalar1=inv_n, scalar2=EPS,
                                                  op0=ALU.mult, op1=ALU.add), reads=bufs, writes=bufs)
            T.op("act", lambda e: e.activation(out=tmp[:, 0:n], in_=tmp[:, 0:n], func=AF.Ln), reads=bufs, writes=bufs)
            T.op("act", lambda e: e.activation(out=rstd[:, 0:n], in_=tmp[:, 0:n], func=AF.Exp, scale=-0.5), reads=bufs, writes=bufs)

        with ExitStack() as kvstack:
            sbk = lambda n, s, d: kvstack.enter_context(nc.sbuf_tensor("sb_" + n, s, d))
            KAT = sbk("KAT", [128, 2, S], BF16)
            VA = sbk("VA", [128, NT, 256], BF16)
            b_kat = [B("kat%d" % j) for j in range(NT)]
            b_va = [B("va%d" % j) for j in range(NT)]
            b_hT = [B("hTs%d" % j) for j in range(NT)]
            b_kb = [B("kbs%d" % j) for j in range(NT)]
            b_vb = [B("vbs%d" % j) for j in range(NT)]

            with ExitStack() as p1:
                s1 = lambda n, s, d: p1.enter_context(nc.sbuf_tensor("sb_" + n, s, d))
                NB1 = 3
                g_bc = s1("g_bc", [128, D], F32); b_g = B("g_bc")
                wkv = s1("wkv", [128, KC, 1024], BF16); b_wkv = B("wkv")
                xt = [s1("xt%d" % i, [128, D], F32) for i in range(NB1)]; b_xt = [B("xt%d" % i) for i in range(NB1)]
                junk = s1("junk", [128, D], BF16); b_junk = B("junk")
                hb = [s1("hb%d" % i, [128, D], BF16) for i in range(NB1)]; b_hb = [B("hb%d" % i) for i in range(NB1)]
                hTt = [s1("hTt%d" % i, [128, KC, 128], BF16) for i in range(NB1)]; b_hTt = [B("hTt%d" % i) for i in range(NB1)]
                st = [s1("st%d" % i, [128, 16], F32) for i in range(NB1)]; b_st = [B("st%d" % i) for i in range(NB1)]
                ksb_ = [s1("ksb%d" % i, [128, 256], F32) for i in range(NB1)]; ksq_ = [s1("ksq%d" % i, [128, 256], F32) for i in range(NB1)]
                knm_ = [s1("knm%d" % i, [128, 2, 128], F32) for i in range(NB1)]
                kt1_ = [s1("kt1%d" % i, [128, 2, 128], F32) for i in range(NB1)]; kt2_ = [s1("kt2%d" % i, [128, 2, 128], F32) for i in range(NB1)]
                krot_ = [s1("krot%d" % i, [128, 2, 128], BF16) for i in range(NB1)]; b_k_ = [B("kscratch%d" % i) for i in range(NB1)]
                rC = [s1("rC%d" % i, [128, 128], F32) for i in range(NB1)]; rS = [s1("rS%d" % i, [128, 128], F32) for i in range(NB1)]
                b_rope = [B("rope%d" % i) for i in range(NB1)]
                kbb_ = [s1("kbb%d" % i, [128, 256], BF16) for i in range(NB1)]; b_kbb_ = [B("kbb%d" % i) for i in range(NB1)]
                kbTt = [s1("kbTt%d" % i, [128, 2, 128], BF16) for i in range(NB1)]; b_kbTt = [B("kbTt%d" % i) for i in range(NB1)]
                vbb = [s1("vbb%d" % i, [128, 256], BF16) for i in range(NB1)]; b_vbb = [B("vbb%d" % i) for i in range(NB1)]
                rot_t = Rot([0, 1]); rot_m = Rot([2, 3, 4, 5]); rot_k = Rot([6, 7])

                T.op("sp", lambda e: e.dma_start(out=g_bc[:], in_=g_mix.partition_broadcast(128)), writes=[b_g], dma=True)
                for hh, c0 in enumerate((1024, 2560)):
                    T.op("sp", lambda e, hh=hh, c0=c0: e.dma_start(
                        out=wkv[:, :, hh * 512:(hh + 1) * 512],
                        in_=w_in_bf[:, c0:c0 + 512].rearrange("(k p) c -> p k c", p=128)),
                        reads=b_win, writes=[b_wkv], dma=True)

                def p1_head(j):
                        i2 = j % NB1
                        X, bX = xt[i2], b_xt[i2]
                        ksb, ksq, knm, kt1, kt2, krot, b_k = ksb_[i2], ksq_[i2], knm_[i2], kt1_[i2], kt2_[i2], krot_[i2], b_k_[i2]
                        kbb, b_kbb = kbb_[i2], b_kbb_[i2]
                        T.op("sp", lambda e, X=X, j=j: e.dma_start(out=X[:], in_=x[j * 128:(j + 1) * 128, :]), writes=[bX], dma=True)
                        T.op("sp", lambda e, j=j, i2=i2: e.dma_start(out=rC[i2][:], in_=ropeC[j * 128:(j + 1) * 128, :]),
                             writes=[b_rope[i2]], dma=True)
                        T.op("sp", lambda e, j=j, i2=i2: e.dma_start(out=rS[i2][:], in_=ropeS[j * 128:(j + 1) * 128, :]),
                             writes=[b_rope[i2]], dma=True)
                        ST, bST = st[i2], b_st[i2]
                        T.op("act", lambda e, X=X, ST=ST: e.activation(out=junk[:], in_=X[:], func=AF.Square, accum_out=ST[:, 0:1]),
                             reads=[bX], writes=[b_junk, bST])
                        rstd_from_ss(ST[:, 0:1], ST[:, 1:2], ST[:, 2:3], 1, 1.0 / D, [bST])
                        H, bH = hb[i2], b_hb[i2]
                        T.op("dve", lambda e, H=H, X=X, ST=ST: e.scalar_tensor_tensor(
                            out=H[:], in0=X[:], scalar=ST[:, 2:3], in1=g_bc[:], op0=ALU.mult, op1=ALU.mult),
                            reads=[bX, bST, b_g], writes=[bH])
                        HT, bHT = hTt[i2], b_hTt[i2]
                        for q4 in range(4):
                            pb, pbb = rot_t.get()
                            pv = pb[:].bitcast(BF16)
                            for kk in range(4):
                                k = q4 * 4 + kk
                                T.op("pe", lambda e, pv=pv, kk=kk, H=H, k=k: e.transpose(
                                    out=pv[:, kk * 128:(kk + 1) * 128], in_=H[:, k * 128:(k + 1) * 128], identity=ident[:]),
                                    reads=[bH, b_const], writes=[pbb])
                            ce = "act" if q4 % 2 == 0 else "dve"
                            if ce == "act":
                                T.op("act", lambda e, HT=HT, q4=q4, pv=pv: e.copy(
                                    out=HT[:, q4 * 4:(q4 + 1) * 4, :], in_=pv[:, 0:512].rearrange("p (k t) -> p k t", t=128)),
                                    reads=[pbb], writes=[bHT])
                            else:
                                T.op("dve", lambda e, HT=HT, q4=q4, pv=pv: e.tensor_copy(
                                    out=HT[:, q4 * 4:(q4 + 1) * 4, :], in_=pv[:, 0:512].rearrange("p (k t) -> p k t", t=128)),
                                    reads=[pbb], writes=[bHT])
                        T.op("pool", lambda e, HT=HT, j=j: e.dma_start(
                            out=hT_s[j // 4, :, :, (j % 4) * 128:(j % 4 + 1) * 128], in_=HT[:]),
                            reads=[bHT], writes=[b_hT[j]], dma=True)
                        pA, pAb = rot_m.get(); pB, pBb = rot_m.get()
                        for k in range(KC):
                            T.op("pe", lambda e, pA=pA, HT=HT, k=k: e.matmul(pA[:], lhsT=HT[:, k, :], rhs=wkv[:, k, 0:512],
                                                                              start=(k == 0), stop=(k == KC - 1)),
                                 reads=[bHT, b_wkv], writes=[pAb])
                        for k in range(KC):
                            T.op("pe", lambda e, pB=pB, HT=HT, k=k: e.matmul(pB[:], lhsT=HT[:, k, :], rhs=wkv[:, k, 512:1024],
                                                                              start=(k == 0), stop=(k == KC - 1)),
                                 reads=[bHT, b_wkv], writes=[pBb])
                        return (j, i2, X, bX, ksb, ksq, knm, kt1, kt2, krot, b_k, kbb, b_kbb, ST, bST, H, bH, HT, bHT, pA, pAb, pB, pBb)

                def p1_tail(L):
                        (j, i2, X, bX, ksb, ksq, knm, kt1, kt2, krot, b_k, kbb, b_kbb, ST, bST, H, bH, HT, bHT, pA, pAb, pB, pBb) = L
                        T.op("act", lambda e, pA=pA, j=j: e.copy(out=VA[:, j, :], in_=pA[:, 256:512]), reads=[pAb], writes=[b_va[j]])
                        T.op("act", lambda e, pB=pB, kbb=kbb: e.copy(out=kbb[:], in_=pB[:, 0:256]), reads=[pBb], writes=[b_kbb])
                        VB_, bVB_ = vbb[i2], b_vbb[i2]
                        T.op("act", lambda e, pB=pB, VB_=VB_: e.copy(out=VB_[:], in_=pB[:, 256:512]), reads=[pBb], writes=[bVB_])
                        T.op("pool", lambda e, VB_=VB_, j=j: e.dma_start(out=vb_s[j * 128:(j + 1) * 128, :], in_=VB_[:]),
                             reads=[bVB_], writes=[b_vb[j]], dma=True, nd=160)
                        T.op("dve", lambda e, pA=pA, ksb=ksb: e.tensor_copy(out=ksb[:], in_=pA[:, 0:256]), reads=[pAb], writes=[b_k])
                        T.op("dve", lambda e, ksq=ksq, ksb=ksb: e.tensor_tensor(out=ksq[:], in0=ksb[:], in1=ksb[:], op=ALU.mult), reads=[b_k], writes=[b_k])
                        T.op("dve", lambda e, ST=ST, ksq=ksq: e.tensor_reduce(out=ST[:, 4:6], in_=ksq[:].rearrange("p (h d) -> p h d", d=128),
                                                                      axis=AX.X, op=ALU.add), reads=[b_k], writes=[bST])
                        rstd_from_ss(ST[:, 4:6], ST[:, 6:8], ST[:, 8:10], 2, 1.0 / 128, [bST])
                        for hh in range(2):
                            T.op("dve", lambda e, hh=hh, ST=ST, knm=knm, ksb=ksb: e.scalar_tensor_tensor(
                                out=knm[:, hh, :], in0=ksb[:, hh * 128:(hh + 1) * 128], scalar=ST[:, 8 + hh:9 + hh], in1=kn_bc[:],
                                op0=ALU.mult, op1=ALU.mult), reads=[b_k, bST, b_const], writes=[b_k])
                        rope(T, knm, kt1, kt2, krot, rC[i2], rS[i2], 2, [b_k, b_rope[i2]], [b_k])
                        pk, pkb = rot_k.get()
                        pkv = pk[:].bitcast(BF16)
                        for hh in range(2):
                            T.op("pe", lambda e, pkv=pkv, hh=hh, krot=krot: e.transpose(out=pkv[:, hh * 128:(hh + 1) * 128], in_=krot[:, hh, :],
                                                                             identity=ident[:]), reads=[b_k, b_const], writes=[pkb])
                        for hh in range(2):
                            T.op("pe", lambda e, pkv=pkv, hh=hh, kbb=kbb: e.transpose(out=pkv[:, 256 + hh * 128:256 + (hh + 1) * 128],
                                                                             in_=kbb[:, hh * 128:(hh + 1) * 128], identity=ident[:]),
                                 reads=[b_kbb, b_const], writes=[pkb])
                        T.op("dve", lambda e, pkv=pkv, j=j: e.tensor_copy(
                            out=KAT[:, :, j * 128:(j + 1) * 128], in_=pkv[:, 0:256].rearrange("p (h t) -> p h t", t=128)),
                            reads=[pkb], writes=[b_kat[j]])
                        KBT, bKBT = kbTt[i2], b_kbTt[i2]
                        T.op("act", lambda e, pkv=pkv, KBT=KBT: e.copy(out=KBT[:], in_=pkv[:, 256:512].rearrange("p (h t) -> p h t", t=128)),
                             reads=[pkb], writes=[bKBT])
                        T.op("pool", lambda e, KBT=KBT, j=j: e.dma_start(
                            out=kbT_s[:, :, j * 128:(j + 1) * 128].rearrange("h p t -> p h t"), in_=KBT[:]),
                            reads=[bKBT], writes=[b_kb[j]], dma=True, nd=300)

                Lp = p1_head(0)
                for j in range(nt1):
                    Ln_ = p1_head(j + 1) if j + 1 < nt1 else None
                    p1_tail(Lp)
                    Lp = Ln_
                T.sync_all()
                if stop == 1:
                    return nc

            b_oT = [B("oT%d" % i) for i in range(16)]
            with ExitStack() as p2:
                s2 = lambda n, s, d: p2.enter_context(nc.sbuf_tensor("sb_" + n, s, d))
                hTb = s2("hTb", [128, KC, 512], BF16); b_hTb = B("hTb")
                wq = [s2("wq%d" % i, [128, KC, 512], BF16) for i in range(2)]; b_wq = [B("wq0"), B("wq1")]
                qAT = s2("qAT", [128, 8, 512], BF16); b_qAT = B("qAT")
                qBT = s2("qBT", [128, 8, 512], BF16); b_qBT = B("qBT")
                oAT = s2("oAT", [128, 8, 512], BF16); b_oAT = B("oAT")
                oBT = s2("oBT", [128, 8, 512], BF16); b_oBT = B("oBT")
                biasB = s2("biasB", [128, 8, 384], F32); b_bias = B("biasB")
                emk = s2("emk", [128, 384], F32); b_emk = B("emk")
                qsb_ = [s2("qsb%d" % i, [128, 512], F32) for i in range(2)]; qnm_ = [s2("qnm%d" % i, [128, 4, 128], F32) for i in range(2)]
                qt1_ = [s2("qt1%d" % i, [128, 4, 128], F32) for i in range(2)]; qt2_ = [s2("qt2%d" % i, [128, 4, 128], F32) for i in range(2)]
                qrot_ = [s2("qrot%d" % i, [128, 4, 128], BF16) for i in range(2)]
                b_q_ = [B("qscratch0"), B("qscratch1")]
                st2_ = [s2("st2%d" % i, [128, 16], F32) for i in range(2)]; b_st2_ = [B("st20"), B("st21")]
                qunit = 0
                rC2 = [s2("rC2%d" % i, [128, 128], F32) for i in range(2)]; rS2 = [s2("rS2%d" % i, [128, 128], F32) for i in range(2)]
                b_rope2 = [B("rope20"), B("rope21")]
                PT = [s2("PT%d" % i, [128, 512], BF16) for i in range(4)]; b_PT = [B("PT%d" % i) for i in range(4)]
                rD = [s2("rD0", [128, 512], F32)] * 2; b_rD = [B("rD0")] * 2
                kbw = s2("kbw", [128, 2, 768], BF16); vbw = s2("vbw", [128, 6, 256], BF16); b_kbw = B("kbw"); b_vbw = B("vbw")
                ssb = s2("ssb", [128, 4, 384], F32); pbf = s2("pbf", [128, 4, 384], BF16)
                pTs = s2("pTs", [128, 12, 128], BF16); b_bs = B("b_s"); b_be = B("b_e"); b_bp = B("b_p"); b_bpT = B("b_pT")
                stB = s2("stB", [128, 32], F32); b_stB = B("stB")

                T.op("sp", lambda e: e.dma_start(out=biasB[:], in_=biasB_d), writes=[b_bias], dma=True)
                T.op("sp", lambda e: e.dma_start(out=emk[:], in_=band_d), writes=[b_emk], dma=True)
                T.op("dve", lambda e: e.tensor_tensor(out=biasB[:], in0=biasB[:], in1=emk[:].unsqueeze(1).to_broadcast([128, 8, 384]),
                                                      op=ALU.add), reads=[b_bias, b_emk], writes=[b_bias])
                rot_q = Rot([6, 7]); rot_s = Rot([0, 1, 2])

                for bi in range(16):
                    t0 = bi * 512
                    if bi == 0:
                        T.op("sp", lambda e: e.dma_start(out=hTb[:], in_=hT_s[0]), reads=b_hT[0:4], writes=[b_hTb], dma=True)
                        T.op("sp", lambda e: e.dma_start(out=wq[0][:], in_=wq_s[0]), reads=[b_wqs[0]], writes=[b_wq[0]], dma=True)
                        T.op("sp", lambda e: e.dma_start(out=wq[1][:], in_=wq_s[2]), reads=[b_wqs[2]], writes=[b_wq[1]], dma=True)
                    jl = (bi * 4 - 1) % NT
                    jr = (bi * 4 + 4) % NT
                    segs = [(0, jl, 1), (1, bi * 4, 4), (5, jr, 1)]
                    for (w0, j0, n) in segs:
                        T.op("sp", lambda e, w0=w0, j0=j0, n=n: e.dma_start(
                            out=kbw[:, :, w0 * 128:(w0 + n) * 128],
                            in_=kbT_s[:, :, j0 * 128:(j0 + n) * 128].rearrange("h p t -> p h t")),
                            reads=b_kb[j0:j0 + n], writes=[b_kbw], dma=True)
                        T.op("sp", lambda e, w0=w0, j0=j0, n=n: e.dma_start(
                            out=vbw[:, w0:w0 + n, :],
                            in_=vb_s[j0 * 128:(j0 + n) * 128, :].rearrange("(t p) c -> p t c", p=128)),
                            reads=b_vb[j0:j0 + n], writes=[b_vbw], dma=True)
                    pendq = None

                    def finish_unit(u):
                        (qrot_u, b_q_u, cg_u, tt_u) = u
                        pt, ptb = rot_s.get()
                        ptv = pt[:].bitcast(BF16)
                        for hh in range(4):
                            T.op("pe", lambda e, ptv=ptv, hh=hh: e.transpose(out=ptv[:, hh * 128:(hh + 1) * 128], in_=qrot_u[:, hh, :],
                                                                             identity=ident[:]), reads=[b_q_u, b_const], writes=[ptb])
                        T.op("dve", lambda e, ptv=ptv: e.tensor_copy(
                            out=qAT[:, cg_u * 4:(cg_u + 1) * 4, tt_u * 128:(tt_u + 1) * 128],
                            in_=ptv[:, 0:512].rearrange("p (h t) -> p h t", t=128)), reads=[ptb], writes=[b_qAT])

                    rot_qb = Rot([3, 4])
                    for cg in range(2):
                        WA, bWA = wq[0], b_wq[0]
                        WB, bWB = wq[1], b_wq[1]
                        if cg == 1:
                            T.op("sp", lambda e: e.dma_start(out=WA[:], in_=wq_s[1]), reads=[b_wqs[1]], writes=[bWA], dma=True)
                            T.op("act", lambda e: e.dma_start(out=WB[:], in_=wq_s[3]), reads=[b_wqs[3]], writes=[bWB], dma=True)
                        W, bW = WA, bWA
                        for tt in range(4):
                            j = bi * 4 + tt
                            i2 = (cg * 4 + tt) % 2
                            T.op("sp", lambda e, j=j, i2=i2: e.dma_start(out=rC2[i2][:], in_=ropeC[j * 128:(j + 1) * 128, :]),
                                 writes=[b_rope2[i2]], dma=True)
                            T.op("sp", lambda e, j=j, i2=i2: e.dma_start(out=rS2[i2][:], in_=ropeS[j * 128:(j + 1) * 128, :]),
                                 writes=[b_rope2[i2]], dma=True)
                            us = qunit % 2; qunit += 1
                            qsb, qnm, qt1, qt2, qrot, b_q, st2, b_st2 = qsb_[us], qnm_[us], qt1_[us], qt2_[us], qrot_[us], b_q_[us], st2_[us], b_st2_[us]
                            pq, pqb = rot_q.get()
                            for k in range(KC):
                                T.op("pe", lambda e, pq=pq, k=k, tt=tt, W=W: e.matmul(
                                    pq[:], lhsT=hTb[:, k, tt * 128:(tt + 1) * 128], rhs=W[:, k, :], start=(k == 0), stop=(k == KC - 1)),
                                    reads=[b_hTb, bW], writes=[pqb])
                            if pendq is not None:
                                finish_unit(pendq)
                            for hh in range(4):
                                T.op("act", lambda e, pq=pq, hh=hh: e.activation(out=qsb[:, hh * 128:(hh + 1) * 128], in_=pq[:, hh * 128:(hh + 1) * 128],
                                                                                 func=AF.Square, accum_out=st2[:, hh:hh + 1]),
                                     reads=[pqb], writes=[b_q, b_st2])
                            T.op("dve", lambda e: e.tensor_scalar(out=st2[:, 4:8], in0=st2[:, 0:4], scalar1=1.0 / 128, scalar2=EPS,
                                                                  op0=ALU.mult, op1=ALU.add), reads=[b_st2], writes=[b_st2])
                            T.op("act", lambda e: e.activation(out=st2[:, 4:8], in_=st2[:, 4:8], func=AF.Ln), reads=[b_st2], writes=[b_st2])
                            T.op("act", lambda e: e.activation(out=st2[:, 8:12], in_=st2[:, 4:8], func=AF.Exp, scale=-0.5), reads=[b_st2], writes=[b_st2])
                            for hh in range(4):
                                T.op("dve", lambda e, hh=hh, pq=pq: e.scalar_tensor_tensor(
                                    out=qnm[:, hh, :], in0=pq[:, hh * 128:(hh + 1) * 128], scalar=st2[:, 8 + hh:9 + hh], in1=qn_bc[:],
                                    op0=ALU.mult, op1=ALU.mult), reads=[pqb, b_st2, b_const], writes=[b_q])
                            rope(T, qnm, qt1, qt2, qrot, rC2[i2], rS2[i2], 4, [b_q, b_rope2[i2]], [b_q])
                            pendq = (qrot, b_q, cg, tt)
                            hB = cg * 4 + tt
                            pqB, pqBb = rot_qb.get()
                            for k in range(KC):
                                T.op("pe", lambda e, pqB=pqB, k=k, tt=tt: e.matmul(
                                    pqB[:], lhsT=WB[:, k, tt * 128:(tt + 1) * 128], rhs=hTb[:, k, :], start=(k == 0), stop=(k == KC - 1)),
                                    reads=[b_hTb, bWB], writes=[pqBb])
                            T.op("act", lambda e, pqB=pqB, hB=hB: e.activation(out=qBT[:, hB, :], in_=pqB[:], func=AF.Copy, scale=SCALE),
                                 reads=[pqBb], writes=[b_qBT])
                    finish_unit(pendq)
                    if bi < 15:
                        T.op("sp", lambda e, bi=bi: e.dma_start(out=hTb[:], in_=hT_s[bi + 1]),
                             reads=b_hT[bi * 4 + 4:bi * 4 + 8], writes=[b_hTb], dma=True)
                        T.op("sp", lambda e: e.dma_start(out=wq[0][:], in_=wq_s[0]), reads=[b_wqs[0]], writes=[b_wq[0]], dma=True)
                        T.op("sp", lambda e: e.dma_start(out=wq[1][:], in_=wq_s[2]), reads=[b_wqs[2]], writes=[b_wq[1]], dma=True)
                    pB7, pB7b = banks[7], bankb[7]

                    def b_stages(tt, kvhb):
                        j = bi * 4 + tt
                        special = j in SPECIAL
                        st = []

                        def s_head(hh):
                            h = kvhb * 4 + hh
                            if hh == 0 and special:
                                si = SPECIAL.index(j)
                                T.op("sp", lambda e: e.dma_start(out=emk[:], in_=emask_d[si:si + 1, :].partition_broadcast(128)),
                                     writes=[b_emk], dma=True)
                            T.op("pe", lambda e: e.matmul(pB7[:, 0:384], lhsT=qBT[:, h, tt * 128:(tt + 1) * 128],
                                                          rhs=kbw[:, kvhb, tt * 128:tt * 128 + 384], start=True, stop=True),
                                 reads=[b_qBT, b_kbw], writes=[pB7b])
                            T.op("dve", lambda e: e.tensor_tensor(out=ssb[:, hh, :], in0=pB7[:, 0:384], in1=biasB[:, h, :], op=ALU.add),
                                 reads=[pB7b, b_bias], writes=[b_bs])
                        for hh in range(4):
                            st.append(lambda hh=hh: s_head(hh))

                        def chain():
                            if special:
                                T.op("dve", lambda e: e.tensor_tensor(out=ssb[:], in0=ssb[:], in1=emk[:].unsqueeze(1).to_broadcast([128, 4, 384]),
                                                                      op=ALU.add), reads=[b_bs, b_emk], writes=[b_bs])
                            T.op("dve", lambda e: e.tensor_reduce(out=stB[:, 0:4], in_=ssb[:], axis=AX.X, op=ALU.max, negate=True),
                                 reads=[b_bs], writes=[b_stB])
                            T.op("dve", lambda e: e.tensor_tensor(out=stB[:, 4:8], in0=stB[:, 0:4], in1=nsink_bc[:, kvhb * 4:kvhb * 4 + 4],
                                                                  op=ALU.min), reads=[b_stB, b_const], writes=[b_stB])
                            for hh in range(4):
                                T.op("act", lambda e, hh=hh: e.activation(out=ssb[:, hh, :], in_=ssb[:, hh, :], func=AF.Exp,
                                                                          bias=stB[:, 4 + hh:5 + hh], scale=1.0, accum_out=stB[:, 8 + hh:9 + hh]),
                                     reads=[b_bs, b_stB], writes=[b_bs, b_stB])
                            T.op("dve", lambda e: e.tensor_tensor(out=stB[:, 12:16], in0=stB[:, 4:8], in1=sink_bc[:, kvhb * 4:kvhb * 4 + 4],
                                                                  op=ALU.add), reads=[b_stB, b_const], writes=[b_stB])
                            T.op("act", lambda e: e.activation(out=stB[:, 12:16], in_=stB[:, 12:16], func=AF.Exp), reads=[b_stB], writes=[b_stB])
                            T.op("dve", lambda e: e.tensor_tensor(out=stB[:, 16:20], in0=stB[:, 8:12], in1=stB[:, 12:16], op=ALU.add),
                                 reads=[b_stB], writes=[b_stB])
                            T.op("dve", lambda e: e.reciprocal(out=stB[:, 20:24], in_=stB[:, 16:20]), reads=[b_stB], writes=[b_stB])
                            T.op("dve", lambda e: e.tensor_tensor(out=pbf[:], in0=ssb[:], in1=stB[:, 20:24].unsqueeze(2).to_broadcast([128, 4, 384]),
                                                                  op=ALU.mult), reads=[b_bs, b_stB], writes=[b_bp])
                        st.append(chain)

                        def transposes(half):
                            ptv = pB7[:].bitcast(BF16)
                            for hq in range(2):
                                hh = half * 2 + hq
                                for jj in range(3):
                                    T.op("pe", lambda e, hq=hq, hh=hh, jj=jj: e.transpose(
                                        out=ptv[:, (hq * 3 + jj) * 128:(hq * 3 + jj + 1) * 128], in_=pbf[:, hh, jj * 128:(jj + 1) * 128],
                                        identity=ident[:]), reads=[b_bp, b_const], writes=[pB7b])
                            T.op("dve", lambda e: e.tensor_copy(out=pTs[:, half * 6:half * 6 + 6, :],
                                                                in_=ptv[:, 0:768].rearrange("p (j t) -> p j t", t=128)),
                                 reads=[pB7b], writes=[b_bpT])
                        st.append(lambda: transposes(0))
                        st.append(lambda: transposes(1))

                        def pv():
                            for hh in range(4):
                                for jj in range(3):
                                    T.op("pe", lambda e, hh=hh, jj=jj: e.matmul(
                                        pB7[:, hh * 128:(hh + 1) * 128], lhsT=vbw[:, tt + jj, kvhb * 128:(kvhb + 1) * 128],
                                        rhs=pTs[:, hh * 3 + jj, :], start=(jj == 0), stop=(jj == 2), skip_group_check=True),
                                        reads=[b_vbw, b_bpT], writes=[pB7b])
                            T.op("dve", lambda e: e.tensor_copy(
                                out=oBT[:, kvhb * 4:(kvhb + 1) * 4, tt * 128:(tt + 1) * 128], in_=pB7[:].rearrange("p (h t) -> p h t", t=128)),
                                reads=[pB7b], writes=[b_oBT])
                        st.append(pv)
                        return st

                    BSLOT = {2: 0, 6: 1, 10: 2, 14: 3, 15: 4, 40: 5, 46: 6, 54: 7}
                    for h in range(8):
                        kvh = h // 4
                        pO, pOb = banks[3 + (h % 2)], bankb[3 + (h % 2)]
                        pD, pDb = banks[5 + (h % 2)], bankb[5 + (h % 2)]
                        pend = []
                        LA = 2
                        bst = b_stages(h % 4, h // 4)
                        if h == 1:
                            for _ in range(6):
                                if deferred:
                                    deferred.pop(0)()
                            cast_expert(bi)
                        for kb in range(NT + LA):
                            if kb in BSLOT:
                                bst[BSLOT[kb]]()
                            if kb < NT:
                                ps, psb = rot_s.get()
                                T.op("pe", lambda e, ps=ps, kvh=kvh, kb=kb, h=h: e.matmul(
                                    ps[:], lhsT=KAT[:, kvh, kb * 128:(kb + 1) * 128], rhs=qAT[:, h, :], start=True, stop=True),
                                    reads=[b_kat[kb], b_qAT], writes=[psb])
                                P_, bP_ = PT[kb % 4], b_PT[kb % 4]
                                T.op("act", lambda e, P_=P_, ps=ps: e.activation(out=P_[:], in_=ps[:], func=AF.Exp, scale=SCALE),
                                     reads=[psb], writes=[bP_])
                                pend.append((kb, P_, bP_))
                            if kb >= LA:
                                (kb0, P0, bP0) = pend.pop(0)
                                T.op("pe", lambda e, pO=pO, kb0=kb0, kvh=kvh, P0=P0: e.matmul(
                                    pO[:], lhsT=VA[:, kb0, kvh * 128:(kvh + 1) * 128], rhs=P0[:], start=(kb0 == 0), stop=(kb0 == NT - 1)),
                                    reads=[b_va[kb0], bP0], writes=[pOb])
                                T.op("pe", lambda e, pD=pD, kb0=kb0, P0=P0: e.matmul(
                                    pD[:], lhsT=ones[:], rhs=P0[:], start=(kb0 == 0), stop=(kb0 == NT - 1)),
                                    reads=[b_const, bP0], writes=[pDb])
                        R_, bR_ = rD[h % 2], b_rD[h % 2]
                        T.op("dve", lambda e, R_=R_, pD=pD: e.reciprocal(out=R_[:], in_=pD[:]), reads=[pDb], writes=[bR_])
                        T.op("dve", lambda e, R_=R_, pO=pO, h=h: e.tensor_tensor(out=oAT[:, h, :], in0=pO[:], in1=R_[:], op=ALU.mult),
                             reads=[pOb, bR_], writes=[b_oAT])
                    T.op("pool", lambda e, t0=t0: e.dma_start(out=oT_s[0, bi], in_=oAT[:]),
                         reads=[b_oAT], writes=[b_oT[bi]], dma=True)
                    T.op("pool", lambda e, t0=t0: e.dma_start(out=oT_s[1, bi], in_=oBT[:]),
                         reads=[b_oBT], writes=[b_oT[bi]], dma=True)
                while deferred:
                    deferred.pop(0)()
                T.sync_all()
                if stop == 2:
                    return nc

        b_x1 = [B("x1_%d" % j) for j in range(NOWN)]
        b_h2 = [B("h2_%d" % j) for j in range(NOWN)]
        with ExitStack() as p3:
            s3 = lambda n, s, d: p3.enter_context(nc.sbuf_tensor("sb_" + n, s, d))
            g_bc = s3("g_bc2", [128, D], F32); b_g = B("g_bc2")
            wrs = s3("wrs", [128, KC, NE], BF16); b_wrs = B("wrs")
            hTb = s3("hTb2", [128, KC, 512], BF16); b_hTb = B("hTb2")
            oAT = s3("oAT2", [128, 8, 512], BF16); oBT = s3("oBT2", [128, 8, 512], BF16); b_o = B("o2")
            mT = s3("mT", [128, KC, 512], BF16); b_mT = B("mT")
            wgs = [s3("wgs%d" % i, [128, 2, KC, 128], BF16) for i in range(3)]
            wps = [s3("wps%d" % i, [128, 2, 8, 128], BF16) for i in range(3)]
            b_ws = [B("ws%d" % i) for i in range(3)]
            wos = [s3("wos%d" % i, [128, KC, 512], BF16) for i in range(2)]; b_wos = [B("wos0"), B("wos1")]
            gsb = [s3("gsb%d" % i, [128, 512], F32) for i in range(2)]; b_gsb = [B("gsb0"), B("gsb1")]
            m1 = s3("m1", [128, 512], F32); b_m1 = B("m1")
            x1blk = [s3("x1blk%d" % i, [128, D], F32) for i in range(4)]; b_x1blk = [B("x1blk%d" % i) for i in range(4)]
            junk = s3("junk2", [128, D], BF16); b_junk = B("junk2")
            h2b = [s3("h2b%d" % i, [128, D], BF16) for i in range(2)]; b_h2b = [B("h2b0"), B("h2b1")]
            h2T = s3("h2T", [128, KC, 128], BF16); b_h2T = B("h2T")
            st = s3("st3", [128, 16], F32); b_st = B("st3")
            lg = s3("lg", [128, NE], F32); b_lg = B("lg")
            rot_a = Rot([0, 1]); rot_b = Rot([2, 3]); rot_o = Rot([4, 5]); rot_t = Rot([6, 7])

            T.op("sp", lambda e: e.dma_start(out=g_bc[:], in_=g_ffn.partition_broadcast(128)), writes=[b_g], dma=True)
            T.op("sp", lambda e: e.dma_start(out=wrs[:], in_=wr_bf.rearrange("(k p) c -> p k c", p=128)), reads=[b_wr], writes=[b_wrs], dma=True)
            nslot = 0
            for bi in range(16):
                t0 = bi * 512
                T.op("sp", lambda e, t0=t0: e.dma_start(out=hTb[:], in_=hT_s[bi]),
                     reads=b_hT[bi * 4:bi * 4 + 4], writes=[b_hTb], dma=True)
                T.op("sp", lambda e, t0=t0: e.dma_start(out=oAT[:], in_=oT_s[0, bi]),
                     reads=[b_oT[bi]], writes=[b_o], dma=True)
                T.op("sp", lambda e, t0=t0: e.dma_start(out=oBT[:], in_=oT_s[1, bi]),
                     reads=[b_oT[bi]], writes=[b_o], dma=True)
                for n in range(KC):
                    sl = nslot % 3; nslot += 1
                    WG, WP, bW = wgs[sl], wps[sl], b_ws[sl]
                    for ab in range(2):
                        T.op("sp", lambda e, WG=WG, ab=ab, n=n: e.dma_start(out=WG[:, ab], in_=wgt_s[ab * 16 + n]),
                             reads=[b_wgt[ab * 16 + n]], writes=[bW], dma=True)
                        T.op("sp", lambda e, WP=WP, ab=ab, n=n: e.dma_start(out=WP[:, ab], in_=wp_s[ab, n]),
                             reads=[b_wp[ab * 16 + n]], writes=[bW], dma=True)
                    for ab in range(2):
                        pg, pgb = rot_a.get()
                        for k in range(KC):
                            T.op("pe", lambda e, pg=pg, WG=WG, ab=ab, k=k: e.matmul(pg[:], lhsT=WG[:, ab, k, :], rhs=hTb[:, k, :],
                                                                                    start=(k == 0), stop=(k == KC - 1)),
                                 reads=[bW, b_hTb], writes=[pgb])
                        G_, bG_ = gsb[ab], b_gsb[ab]
                        T.op("act", lambda e, G_=G_, pg=pg, ab=ab, n=n: e.activation(out=G_[:], in_=pg[:], func=AF.Sigmoid,
                                                                                     bias=bgate[:, ab * 16 + n:ab * 16 + n + 1], scale=1.0),
                             reads=[pgb, b_const], writes=[bG_])
                        pp, ppb = rot_b.get()
                        osrc = oAT if ab == 0 else oBT
                        for hh in range(8):
                            T.op("pe", lambda e, pp=pp, WP=WP, ab=ab, hh=hh, osrc=osrc: e.matmul(
                                pp[:], lhsT=WP[:, ab, hh, :], rhs=osrc[:, hh, :], start=(hh == 0), stop=(hh == 7)),
                                reads=[bW, b_o], writes=[ppb])
                        if ab == 0:
                            T.op("dve", lambda e, pp=pp, G_=G_: e.tensor_tensor(out=m1[:], in0=pp[:], in1=G_[:], op=ALU.mult),
                                 reads=[ppb, bG_], writes=[b_m1])
                        else:
                            T.op("dve", lambda e, pp=pp, G_=G_: e.tensor_tensor(out=G_[:], in0=pp[:], in1=G_[:], op=ALU.mult),
                                 reads=[ppb, bG_], writes=[bG_])
                            T.op("dve", lambda e, G_=G_, n=n: e.tensor_tensor(out=mT[:, n, :], in0=m1[:], in1=G_[:], op=ALU.add),
                                 reads=[b_m1, bG_], writes=[b_mT])
                for tt in range(4):
                    j = bi * 4 + tt
                    T.op("sp", lambda e, tt=tt, j=j: e.dma_start(out=x1blk[tt][:], in_=x[j * 128:(j + 1) * 128, :]),
                         writes=[b_x1blk[tt]], dma=True)
                for cg in range(4):
                    WO, bWO = wos[cg % 2], b_wos[cg % 2]
                    T.op("sp", lambda e, WO=WO, cg=cg: e.dma_start(
                        out=WO[:], in_=wo_s[cg]), reads=[b_wosc[cg]], writes=[bWO], dma=True)
                    for tt in range(4):
                        po, pob = rot_o.get()
                        for k in range(KC):
                            T.op("pe", lambda e, po=po, k=k, tt=tt, WO=WO: e.matmul(
                                po[:], lhsT=mT[:, k, tt * 128:(tt + 1) * 128], rhs=WO[:, k, :], start=(k == 0), stop=(k == KC - 1)),
                                reads=[b_mT, bWO], writes=[pob])
                        T.op("dve", lambda e, po=po, tt=tt, cg=cg: e.tensor_tensor(
                            out=x1blk[tt][:, cg * 512:(cg + 1) * 512], in0=po[:], in1=x1blk[tt][:, cg * 512:(cg + 1) * 512], op=ALU.add),
                            reads=[pob, b_x1blk[tt]], writes=[b_x1blk[tt]])
                for tt in range(4):
                    j = bi * 4 + tt
                    XB, bXB = x1blk[tt], b_x1blk[tt]
                    if j < NOWN:
                        T.op("pool", lambda e, XB=XB, j=j: e.dma_start(out=x1_s[j * 128:(j + 1) * 128, :], in_=XB[:]),
                             reads=[bXB], writes=[b_x1[j]], dma=True)
                    T.op("act", lambda e, XB=XB: e.activation(out=junk[:], in_=XB[:], func=AF.Square, accum_out=st[:, 0:1]),
                         reads=[bXB], writes=[b_junk, b_st])
                    rstd_from_ss(st[:, 0:1], st[:, 1:2], st[:, 2:3], 1, 1.0 / D, [b_st])
                    H2, bH2 = h2b[tt % 2], b_h2b[tt % 2]
                    T.op("dve", lambda e, H2=H2, XB=XB: e.scalar_tensor_tensor(out=H2[:], in0=XB[:], scalar=st[:, 2:3], in1=g_bc[:],
                                                                                op0=ALU.mult, op1=ALU.mult),
                         reads=[bXB, b_st, b_g], writes=[bH2])
                    if j < NOWN:
                        T.op("pool", lambda e, H2=H2, j=j: e.dma_start(out=h2_s[j * 128:(j + 1) * 128, :], in_=H2[:]),
                             reads=[bH2], writes=[b_h2[j]], dma=True)
                    for q4 in range(4):
                        pb, pbb = rot_t.get()
                        pv = pb[:].bitcast(BF16)
                        for kk in range(4):
                            k = q4 * 4 + kk
                            T.op("pe", lambda e, pv=pv, kk=kk, H2=H2, k=k: e.transpose(
                                out=pv[:, kk * 128:(kk + 1) * 128], in_=H2[:, k * 128:(k + 1) * 128], identity=ident[:]),
                                reads=[bH2, b_const], writes=[pbb])
                        T.op("act", lambda e, q4=q4, pv=pv: e.copy(out=h2T[:, q4 * 4:(q4 + 1) * 4, :],
                                                                   in_=pv[:, 0:512].rearrange("p (k t) -> p k t", t=128)),
                             reads=[pbb], writes=[b_h2T])
                    pl, plb = rot_t.get()
                    for k in range(KC):
                        T.op("pe", lambda e, pl=pl, k=k: e.matmul(pl[:, 0:NE], lhsT=h2T[:, k, :], rhs=wrs[:, k, :],
                                                                  start=(k == 0), stop=(k == KC - 1)),
                             reads=[b_h2T, b_wrs], writes=[plb])
                    T.op("dve", lambda e, pl=pl: e.tensor_reduce(out=st[:, 4:5], in_=pl[:, 0:NE], axis=AX.X, op=ALU.max, negate=True),
                         reads=[plb], writes=[b_st])
                    T.op("act", lambda e, pl=pl: e.activation(out=lg[:], in_=pl[:, 0:NE], func=AF.Exp, bias=st[:, 4:5], scale=1.0,
                                                              accum_out=st[:, 5:6]), reads=[plb, b_st], writes=[b_lg, b_st])
                    T.op("dve", lambda e: e.reciprocal(out=st[:, 6:7], in_=st[:, 5:6]), reads=[b_st], writes=[b_st])
                    T.op("dve", lambda e, j=j: e.tensor_scalar(out=aff[:, j, :], in0=lg[:], scalar1=st[:, 6:7], scalar2=None, op0=ALU.mult),
                         reads=[b_lg, b_st], writes=[b_aff[j]])
            T.sync_all()
            if "aff_dbg" in dbg:
                aff_dbg = nc.dram_tensor("aff_dbg", [128, NT * NE], F32, kind="ExternalOutput").ap()
                T.op("sp", lambda e: e.dma_start(out=aff_dbg, in_=aff[:].rearrange("p j e -> p (j e)")), reads=b_aff, writes=[B("affd")], dma=True)
                T.sync_all()
            if stop == 3:
                return nc

        b_Xg = [B("Xg%d" % i) for i in range(NE)]
        b_Yg = [B("Yg%d" % i) for i in range(NE)]
        with ExitStack() as p4:
            s4 = lambda n, s, d: p4.enter_context(nc.sbuf_tensor("sb_" + n, s, d))
            gt = s4("gt", [128, NOWN, NE], F32); b_gt = B("gt")
            idx_i = s4("idx_i", [128, NOWN * NE], I32); b_idx = B("idx")
            bc_reg = nc.gpsimd.to_reg(CAP - 1)
            with ExitStack() as p4a:
                sa = lambda n, s, d: p4a.enter_context(nc.sbuf_tensor("sb_" + n, s, d))
                cmp_ = sa("cmp", [128, NT, NE], F32); b_cmp = B("cmp")
                lo = sa("lo", [128, NE], F32); mid = sa("mid", [128, NE], F32); ge = sa("ge", [128, NE], F32)
                cntp = sa("cntp", [128, NE], BF16); b_bis = B("bis")
                msk = sa("msk", [128, NOWN, NE], F32); mskb = sa("mskb", [128, NOWN * NE], BF16); b_msk = B("msk")
                cc = sa("cc", [128, NOWN, NE], F32); offs = sa("offs", [128, NOWN, NE], F32); b_offs = B("offs")
                posf = sa("posf", [128, NOWN * NE], F32); tmpf = sa("tmpf", [128, NOWN * NE], F32); b_pos = B("pos")
                pc, pcb = banks[0], bankb[0]
                T.op("dve", lambda e: e.memset(lo[:], 0.0), writes=[b_bis])
                for it in range(NBISECT):
                    c_ = 2.0 ** (-(it + 1))
                    T.op("dve", lambda e, c_=c_: e.tensor_scalar(out=mid[:], in0=lo[:], scalar1=c_, scalar2=None, op0=ALU.add),
                         reads=[b_bis], writes=[b_bis])
                    T.op("dve", lambda e: e.tensor_tensor(out=cmp_[:], in0=aff[:], in1=mid[:].unsqueeze(1).to_broadcast([128, NT, NE]),
                                                          op=ALU.is_ge), reads=b_aff + [b_bis], writes=[b_cmp])
                    with nc.allow_low_precision(reason="integer counts <= 64 are exact in bf16"):
                        T.op("dve", lambda e: e.tensor_reduce(out=cntp[:], in_=cmp_[:].rearrange("p j e -> p e j"), axis=AX.X, op=ALU.add),
                             reads=[b_cmp], writes=[b_bis])
                    T.op("pe", lambda e: e.matmul(pc[:, 0:NE], lhsT=ones[:], rhs=cntp[:], start=True, stop=True),
                         reads=[b_bis, b_const], writes=[pcb])
                    T.op("dve", lambda e: e.tensor_scalar(out=ge[:], in0=pc[:, 0:NE], scalar1=float(CAP) - 0.5, scalar2=None, op0=ALU.is_ge),
                         reads=[pcb], writes=[b_bis])
                    T.op("dve", lambda e, c_=c_: e.scalar_tensor_tensor(out=lo[:], in0=ge[:], scalar=c_, in1=lo[:], op0=ALU.mult, op1=ALU.add),
                         reads=[b_bis], writes=[b_bis])
                T.op("dve", lambda e: e.tensor_tensor(out=msk[:], in0=aff[:, 0:NOWN, :], in1=lo[:].unsqueeze(1).to_broadcast([128, NOWN, NE]),
                                                      op=ALU.is_ge), reads=b_aff + [b_bis], writes=[b_msk])
                T.op("dve", lambda e: e.tensor_tensor(out=gt[:], in0=aff[:, 0:NOWN, :], in1=msk[:], op=ALU.mult), reads=b_aff + [b_msk], writes=[b_gt])
                T.op("dve", lambda e: e.tensor_copy(out=mskb[:], in_=msk[:].rearrange("p j e -> p (j e)")), reads=[b_msk], writes=[b_msk])
                pp_, ppb_ = banks[1], bankb[1]
                pc2, pc2b = banks[2], bankb[2]
                T.op("pe", lambda e: e.matmul(pp_[:], lhsT=triU[:], rhs=mskb[:], start=True, stop=True), reads=[b_msk, b_const], writes=[ppb_])
                T.op("pe", lambda e: e.matmul(pc2[:], lhsT=ones[:], rhs=mskb[:], start=True, stop=True), reads=[b_msk, b_const], writes=[pc2b])
                T.op("act", lambda e: e.copy(out=cc[:], in_=pc2[:].rearrange("p (j e) -> p j e", e=NE)), reads=[pc2b], writes=[b_offs])
                T.op("dve", lambda e: e.memset(offs[:, 0, :], 0.0), reads=[b_offs], writes=[b_offs])
                for j in range(1, NOWN):
                    T.op("dve", lambda e, j=j: e.tensor_tensor(out=offs[:, j, :], in0=offs[:, j - 1, :], in1=cc[:, j - 1, :], op=ALU.add),
                         reads=[b_offs], writes=[b_offs])
                T.op("dve", lambda e: e.tensor_tensor(out=posf[:], in0=pp_[:], in1=offs[:].rearrange("p j e -> p (j e)"), op=ALU.add),
                     reads=[ppb_, b_offs], writes=[b_pos])
                T.op("dve", lambda e: e.tensor_scalar(out=tmpf[:], in0=msk[:].rearrange("p j e -> p (j e)"), scalar1=-4096.0, scalar2=4096.0,
                                                      op0=ALU.mult, op1=ALU.add), reads=[b_msk], writes=[b_pos])
                T.op("dve", lambda e: e.tensor_tensor(out=posf[:], in0=posf[:], in1=tmpf[:], op=ALU.add), reads=[b_pos], writes=[b_pos])
                T.op("dve", lambda e: e.tensor_copy(out=idx_i[:], in_=posf[:]), reads=[b_pos], writes=[b_idx])
                T.sync_all()
            with ExitStack() as p4b:
                sbx = lambda n, s, d: p4b.enter_context(nc.sbuf_tensor("sb_" + n, s, d))
                Wg = sbx("Wg", [128, KC, FF], BF16); Wu = sbx("Wu", [128, KC, FF], BF16); Wd = sbx("Wd", [128, 8, D], BF16)
                b_Wg = B("Wg"); b_Wu = B("Wu"); b_Wd = B("Wd")
                XgT = sbx("XgT", [128, KC, CAP], BF16); b_XgT = [B("XgT%d" % i) for i in range(8)]
                HT = sbx("HT", [128, 8, CAP], BF16); b_HT = [B("HT0"), B("HT1")]
                xg = [sbx("xg%d" % i, [128, D], BF16) for i in range(2)]; b_xg = [B("xg0"), B("xg1")]
                ysb = [sbx("ysb%d" % i, [128, D], BF16) for i in range(2)]; b_ysb = [B("ysb0"), B("ysb1")]
                sg = [sbx("sg%d" % i, [128, 512], F32) for i in range(2)]; b_sg = [B("sg0"), B("sg1")]
                rot_t = Rot([0, 1]); rot_a = Rot([2, 3]); rot_u = Rot([4, 5]); rot_y = Rot([6, 7])
                hsc = [sbx("hsc%d" % i, [128, D], BF16) for i in range(4)]; b_hsc = [B("hsc%d" % i) for i in range(4)]
                sc_tok = [[] for _ in range(NE)]
                nsc = [0]

                def scatter_expert(ex):
                    for j in range(NOWN):
                        Hs, bHs = hsc[nsc[0] % 4], b_hsc[nsc[0] % 4]; nsc[0] += 1
                        T.op("sp", lambda e, Hs=Hs, j=j: e.dma_start(out=Hs[:], in_=h2_s[j * 128:(j + 1) * 128, :]),
                             reads=[b_h2[j]], writes=[bHs], dma=True)
                        sc_tok[ex].append(T.op("pool", lambda e, Hs=Hs, j=j: e.indirect_dma_start(
                            out=Xg[ex], out_offset=bass.IndirectOffsetOnAxis(ap=idx_i[:, j * NE + ex:j * NE + ex + 1], axis=0),
                            in_=Hs[:, :], in_offset=None, bounds_check=bc_reg, oob_is_err=False),
                            reads=[bHs, b_idx], writes=[], dma=True, nd=160))

                def load_w(ex, which):
                    if "g" in which:
                        T.op("sp", lambda e: e.dma_start(out=Wg[:], in_=wg_bf[ex].rearrange("(k p) f -> p k f", p=128)),
                             reads=[b_wg[ex]], writes=[b_Wg], dma=True)
                        T.op("sp", lambda e: e.dma_start(out=Wu[:], in_=wu_bf[ex].rearrange("(k p) f -> p k f", p=128)),
                             reads=[b_wu[ex]], writes=[b_Wu], dma=True)
                    if "d" in which:
                        T.op("sp", lambda e: e.dma_start(out=Wd[:], in_=wd_bf[ex].rearrange("(k p) f -> p k f", p=128)),
                             reads=[b_wd[ex]], writes=[b_Wd], dma=True)

                load_w(0, "gd")
                scatter_expert(0)
                for e_ in range(NE):
                    for tok in sc_tok[e_]:
                        T._wait("sp", tok[0], tok[1])
                    for st_ in range(8):
                        XGt, bXGt = xg[st_ % 2], b_xg[st_ % 2]
                        T.op("sp", lambda e, XGt=XGt, e_=e_, st_=st_: e.dma_start(out=XGt[:], in_=Xg[e_][st_ * 128:(st_ + 1) * 128, :]),
                             reads=[b_Xg[e_]], writes=[bXGt], dma=True)
                        for q4 in range(4):
                            pb, pbb = rot_t.get()
                            pv = pb[:].bitcast(BF16)
                            for kk in range(4):
                                k = q4 * 4 + kk
                                T.op("pe", lambda e, pv=pv, kk=kk, XGt=XGt, k=k: e.transpose(
                                    out=pv[:, kk * 128:(kk + 1) * 128], in_=XGt[:, k * 128:(k + 1) * 128], identity=ident[:]),
                                    reads=[bXGt, b_const], writes=[pbb])
                            if q4 % 2 == 0:
                                T.op("act", lambda e, pv=pv, q4=q4, st_=st_: e.copy(
                                    out=XgT[:, q4 * 4:(q4 + 1) * 4, st_ * 128:(st_ + 1) * 128],
                                    in_=pv[:, 0:512].rearrange("p (k t) -> p k t", t=128)), reads=[pbb], writes=[b_XgT[st_]])
                            else:
                                T.op("dve", lambda e, pv=pv, q4=q4, st_=st_: e.tensor_copy(
                                    out=XgT[:, q4 * 4:(q4 + 1) * 4, st_ * 128:(st_ + 1) * 128],
                                    in_=pv[:, 0:512].rearrange("p (k t) -> p k t", t=128)), reads=[pbb], writes=[b_XgT[st_]])
                    if e_ + 1 < NE:
                        scatter_expert(e_ + 1)
                    for sh in range(2):
                        for fc in range(8):
                            pa, pab = rot_a.get(); pu, pub = rot_u.get()
                            for k in range(KC):
                                T.op("pe", lambda e, pa=pa, k=k, fc=fc, sh=sh: e.matmul(
                                    pa[:], lhsT=Wg[:, k, fc * 128:(fc + 1) * 128], rhs=XgT[:, k, sh * 512:(sh + 1) * 512],
                                    start=(k == 0), stop=(k == KC - 1)), reads=[b_Wg] + b_XgT[sh * 4:sh * 4 + 4], writes=[pab])
                            for k in range(KC):
                                T.op("pe", lambda e, pu=pu, k=k, fc=fc, sh=sh: e.matmul(
                                    pu[:], lhsT=Wu[:, k, fc * 128:(fc + 1) * 128], rhs=XgT[:, k, sh * 512:(sh + 1) * 512],
                                    start=(k == 0), stop=(k == KC - 1)), reads=[b_Wu] + b_XgT[sh * 4:sh * 4 + 4], writes=[pub])
                            SG, bSG = sg[fc % 2], b_sg[fc % 2]
                            T.op("act", lambda e, SG=SG, pa=pa: e.activation(out=SG[:], in_=pa[:], func=AF.Silu), reads=[pab], writes=[bSG])
                            T.op("dve", lambda e, SG=SG, pu=pu, fc=fc, sh=sh: e.tensor_tensor(
                                out=HT[:, fc, sh * 512:(sh + 1) * 512], in0=pu[:], in1=SG[:], op=ALU.mult),
                                reads=[pub, bSG], writes=[b_HT[sh]])
                    if e_ + 1 < NE:
                        load_w(e_ + 1, "g")
                    for st_ in range(8):
                        Y, bY = ysb[st_ % 2], b_ysb[st_ % 2]
                        for dg in range(4):
                            py, pyb = rot_y.get()
                            for fc in range(8):
                                T.op("pe", lambda e, py=py, fc=fc, st_=st_, dg=dg: e.matmul(
                                    py[:], lhsT=HT[:, fc, st_ * 128:(st_ + 1) * 128], rhs=Wd[:, fc, dg * 512:(dg + 1) * 512],
                                    start=(fc == 0), stop=(fc == 7)), reads=[b_HT[st_ // 4], b_Wd], writes=[pyb])
                            if dg % 2 == 0:
                                T.op("act", lambda e, Y=Y, py=py, dg=dg: e.copy(out=Y[:, dg * 512:(dg + 1) * 512], in_=py[:]),
                                     reads=[pyb], writes=[bY])
                            else:
                                T.op("dve", lambda e, Y=Y, py=py, dg=dg: e.tensor_copy(out=Y[:, dg * 512:(dg + 1) * 512], in_=py[:]),
                                     reads=[pyb], writes=[bY])
                        T.op("sp", lambda e, Y=Y, e_=e_, st_=st_: e.dma_start(out=Yg[e_][st_ * 128:(st_ + 1) * 128, :], in_=Y[:]),
                             reads=[bY], writes=[b_Yg[e_]], dma=True)
                    if e_ + 1 < NE:
                        load_w(e_ + 1, "d")
                T.sync_all()
                if stop == 5:
                    return nc
            with ExitStack() as p4c:
                sc = lambda n, s, d: p4c.enter_context(nc.sbuf_tensor("sb_" + n, s, d))
                g_bc = sc("g_bc3", [128, D], F32); b_g = B("g_bc3")
                acc = [sc("acc%d" % i, [128, D], F32) for i in range(2)]; b_acc = [B("acc0"), B("acc1")]
                NG = 12
                G = [sc("G%d" % i, [128, D], BF16) for i in range(NG)]; b_G = [B("G%d" % i) for i in range(NG)]
                dgm = [sc("dgm%d" % i, [128, 128], BF16) for i in range(NG)]; b_dgm = [B("dgm%d" % i) for i in range(NG)]
                ob = [sc("ob%d" % i, [128, D], F32) for i in range(2)]; b_ob = [B("ob0"), B("ob1")]
                junk = sc("junk3", [128, D], BF16); b_junk = B("junk3")
                st = sc("st4", [128, 8], F32); b_st = B("st4")
                T.op("sp", lambda e: e.dma_start(out=g_bc[:], in_=g_fin.partition_broadcast(128)), writes=[b_g], dma=True)
                for i in range(NG):
                    T.op("dve", lambda e, i=i: e.memset(G[i][:], 0.0), writes=[b_G[i]])
                ng = 0
                b_out = B("out")
                for j in range(NOWN):
                    A, bA = acc[j % 2], b_acc[j % 2]
                    T.op("sp", lambda e, A=A, j=j: e.dma_start(out=A[:], in_=x1_s[j * 128:(j + 1) * 128, :]), reads=[b_x1[j]], writes=[bA], dma=True)
                    pbk = [(banks[(j % 2) * 4 + c], bankb[(j % 2) * 4 + c]) for c in range(4)]
                    for e_ in range(NE):
                        Gb, bGb = G[ng % NG], b_G[ng % NG]
                        Dg, bDg = dgm[ng % NG], b_dgm[ng % NG]; ng += 1
                        T.op("pool", lambda e, Gb=Gb, j=j, e_=e_: e.indirect_dma_start(
                            out=Gb[:, :], out_offset=None, in_=Yg[e_],
                            in_offset=bass.IndirectOffsetOnAxis(ap=idx_i[:, j * NE + e_:j * NE + e_ + 1], axis=0),
                            bounds_check=bc_reg, oob_is_err=False), reads=[b_Yg[e_], b_idx], writes=[bGb], dma=True, nd=160)
                        T.op("dve", lambda e, Dg=Dg, j=j, e_=e_: e.tensor_scalar(out=Dg[:], in0=ident[:], scalar1=gt[:, j, e_:e_ + 1], scalar2=None,
                                                                                op0=ALU.mult), reads=[b_const, b_gt], writes=[bDg])
                        for c in range(4):
                            T.op("pe", lambda e, c=c, Dg=Dg, Gb=Gb, e_=e_: e.matmul(
                                pbk[c][0][:], lhsT=Dg[:], rhs=Gb[:, c * 512:(c + 1) * 512], start=(e_ == 0), stop=(e_ == NE - 1)),
                                reads=[bDg, bGb], writes=[pbk[c][1]])
                    for c in range(4):
                        T.op("dve", lambda e, c=c, A=A: e.tensor_tensor(out=A[:, c * 512:(c + 1) * 512], in0=pbk[c][0][:],
                                                                         in1=A[:, c * 512:(c + 1) * 512], op=ALU.add),
                             reads=[pbk[c][1], bA], writes=[bA])
                    T.op("act", lambda e, A=A: e.activation(out=junk[:], in_=A[:], func=AF.Square, accum_out=st[:, 0:1]),
                         reads=[bA], writes=[b_junk, b_st])
                    rstd_from_ss(st[:, 0:1], st[:, 1:2], st[:, 2:3], 1, 1.0 / D, [b_st])
                    O_, bO_ = ob[j % 2], b_ob[j % 2]
                    T.op("dve", lambda e, O_=O_, A=A: e.scalar_tensor_tensor(out=O_[:], in0=A[:], scalar=st[:, 2:3], in1=g_bc[:],
                                                                            op0=ALU.mult, op1=ALU.mult), reads=[bA, b_st, b_g], writes=[bO_])
                    T.op("sp", lambda e, O_=O_, j=j: e.dma_start(out=out[j * 128:(j + 1) * 128, :], in_=O_[:]), reads=[bO_], writes=[b_out], dma=True)
                T.sync_all()
        print("ops:", T.cnt, "dmas:", T.dcnt, "waits:", T.nwait, "sems:", len(T.sems))
    return nc
```

```python
import math
from contextlib import ExitStack
import numpy as np
import ml_dtypes
import concourse.bass as bass
import concourse.mybir as mybir
from concourse.bass_utils import run_bass_kernel_spmd

F32 = mybir.dt.float32
BF16 = mybir.dt.bfloat16
I32 = mybir.dt.int32
AF = mybir.ActivationFunctionType
ALU = mybir.AluOpType
AX = mybir.AxisListType

S = 8192
D = 2048
NT = 64
NOWN = 32
KC = 16
NE = 16
CAP = 1024
FF = 1024
EPS = 1e-6
SCALE = 1.0 / math.sqrt(128.0)
NEG = -1e30
NBISECT = 26
SPECIAL = [0, 31, 32, 63]


class Buf:
    __slots__ = ("name", "w", "r", "excl")

    def __init__(self, name, excl=False):
        self.name = name
        self.w = None
        self.r = {}
        self.excl = excl


class Trk:
    EPOCH = 12000
    RING = 16

    def __init__(self, nc, stack):
        self.nc = nc
        self.stack = stack
        self.eng = {"pe": nc.tensor, "act": nc.scalar, "dve": nc.vector, "pool": nc.gpsimd, "sp": nc.sync}
        self.sems = {}
        self.cnt = {e: 0 for e in self.eng}
        self.dcnt = {e: 0 for e in self.eng}
        self.obs = {e: {} for e in self.eng}
        self.owner = {}
        self.latest = {}
        self.nwait = 0
        self.pq = []
        self.pq_sum = 0
        self.nop = 0
        self.limit = None
        self.log = []

    def _sem(self, key, owner):
        if key not in self.sems:
            self.sems[key] = self.stack.enter_context(self.nc.semaphore("s_%s_%s" % key))
            self.owner[key] = owner
        return self.sems[key]

    def _wait(self, e, key, val):
        if self.obs[e].get(key, 0) >= val:
            return
        self.eng[e].wait_ge(self.sems[key], val)
        self.obs[e][key] = val
        self.nwait += 1

    def op(self, e, fn, reads=(), writes=(), dma=False, nd=2048):
        self.nop += 1
        if self.limit is not None and self.nop > self.limit:
            return None
        if self.limit is not None:
            import inspect
            self.log.append((self.nop, e, inspect.stack()[1].lineno))
        deps = {}
        if dma and e == "pool":
            while self.pq and self.pq_sum + nd > 14000:
                otok, ond = self.pq.pop(0)
                self._wait("pool", otok[0], otok[1])
                self.pq_sum -= ond

        def add(tok):
            if tok is None:
                return
            k, v = tok
            if deps.get(k, 0) < v:
                deps[k] = v

        for b in reads:
            add(b.w)
            if b.excl:
                for k, v in b.r.items():
                    if self.owner[k][0] != e:
                        add((k, v))
        for b in writes:
            add(b.w)
            for k, v in b.r.items():
                add((k, v))
        for k, v in deps.items():
            if e == "pe" and self.owner[k] == ("pe", False):
                continue
            self._wait(e, k, v)
        if dma:
            n = self.dcnt[e]
            self.dcnt[e] += 1
            key = ("d" + e, n % self.RING)
            val = 16 * (n // self.RING + 1)
            sem = self._sem(key, (e, True))
            fn(self.eng[e]).then_inc(sem, 16)
        else:
            n = self.cnt[e]
            self.cnt[e] += 1
            key = (e, n // self.EPOCH)
            val = n % self.EPOCH + 1
            sem = self._sem(key, (e, False))
            fn(self.eng[e]).then_inc(sem, 1)
        tok = (key, val)
        if dma and e == "pool":
            self.pq.append((tok, nd))
            self.pq_sum += nd
        self.latest[key] = val
        for b in reads:
            if b.r.get(key, 0) < val:
                b.r[key] = val
        for b in writes:
            b.w = tok
            b.r = {}
        return tok

    def sync_all(self, engines=None):
        for e in (engines or list(self.eng)):
            for k, v in self.latest.items():
                self._wait(e, k, v)


def rope(T, src, t1, t2, dst, C, S_, nh, reads, writes):
    T.op("dve", lambda e: e.tensor_tensor(out=t1[:], in0=src[:], in1=C[:].unsqueeze(1).to_broadcast([128, nh, 128]), op=ALU.mult),
         reads=reads, writes=writes)
    s5 = src[:].rearrange("p h (a b c) -> p h a b c", a=2, b=2, c=32)
    t5 = t2[:].rearrange("p h (a b c) -> p h a b c", a=2, b=2, c=32)
    S4 = S_[:].rearrange("p (a b c) -> p a b c", a=2, b=2, c=32)
    for b in range(2):
        T.op("dve", lambda e, b=b: e.tensor_tensor(out=t5[:, :, :, b, :], in0=s5[:, :, :, 1 - b, :],
                                                   in1=S4[:, :, b, :].unsqueeze(1).to_broadcast([128, nh, 2, 32]), op=ALU.mult),
             reads=reads, writes=writes)
    T.op("dve", lambda e: e.tensor_tensor(out=dst[:], in0=t1[:], in1=t2[:], op=ALU.add), reads=reads, writes=writes)


def kernel(x, g_mix, w_in, b_gate, qn_a, kn_a, w_proj_a, sink_b, rel_bias, w_proj_b,
           w_o, g_ffn, w_router, w_gate_e, w_up_e, w_down_e, g_final):
    nc = build_program()
    in_maps = make_inputs(x, g_mix, w_in, b_gate, qn_a, kn_a, w_proj_a, sink_b, rel_bias, w_proj_b,
                          w_o, g_ffn, w_router, w_gate_e, w_up_e, w_down_e, g_final)
    res = run_bass_kernel_spmd(nc, in_maps, core_ids=list(range(8)))
    out = np.empty((4, S, D), np.float32)
    for c in range(8):
        b, hf = c // 2, c % 2
        out[b, hf * 4096:(hf + 1) * 4096] = res.results[c]["out"]
    return out


def _t5_bucket(rel):
    half = 16
    ret = np.where(rel > 0, half, 0)
    n = np.abs(rel)
    max_exact = 8
    nf = np.maximum(n, 1).astype(np.float32)
    large = max_exact + (np.log(nf / max_exact) / math.log(128 / max_exact) * (half - max_exact)).astype(np.int32)
    large = np.minimum(large, half - 1)
    return ret + np.where(n < max_exact, n, large)


def make_inputs(x, g_mix, w_in, b_gate, qn_a, kn_a, w_proj_a, sink_b, rel_bias, w_proj_b,
                w_o, g_ffn, w_router, w_gate_e, w_up_e, w_down_e, g_final):
    f = lambda a: np.ascontiguousarray(np.asarray(a, dtype=np.float32))
    x = f(x)
    pos = np.arange(S)
    r = (pos // 64).astype(np.float32)
    c = (pos % 64).astype(np.float32)
    inv = (1.0 / (10000.0 ** (np.arange(0, 64, 2, dtype=np.float32) / 64.0))).astype(np.float32)
    ar = r[:, None] * inv[None, :]
    ac = c[:, None] * inv[None, :]
    ropeC = np.concatenate([np.cos(ar), np.cos(ar), np.cos(ac), np.cos(ac)], axis=1).astype(np.float32)
    ropeS = np.concatenate([-np.sin(ar), np.sin(ar), -np.sin(ac), np.sin(ac)], axis=1).astype(np.float32)
    rel = (np.arange(384) - 128)[None, :] - np.arange(128)[:, None]
    bucket = _t5_bucket(rel)
    band = np.where(np.abs(rel) <= 128, 0.0, NEG).astype(np.float32)
    biasB = np.ascontiguousarray(f(rel_bias)[bucket].transpose(0, 2, 1))
    ident = np.eye(128, dtype=np.float32).astype(ml_dtypes.bfloat16)
    triU = np.triu(np.ones((128, 128), np.float32), 1).astype(ml_dtypes.bfloat16)
    bg = np.ascontiguousarray(f(b_gate).reshape(32, 128).T)
    shared = {
        "w_in": f(w_in)[0], "w_pa": f(w_proj_a)[0], "w_pb": f(w_proj_b)[0], "w_o": f(w_o)[0],
        "w_r": f(w_router)[0], "w_g": f(w_gate_e)[0], "w_u": f(w_up_e)[0], "w_d": f(w_down_e)[0],
        "g_mix": f(g_mix).reshape(1, D), "g_ffn": f(g_ffn).reshape(1, D), "g_fin": f(g_final).reshape(1, D),
        "qn": f(qn_a).reshape(1, 128), "kn": f(kn_a).reshape(1, 128), "sink": f(sink_b).reshape(1, 8),
        "bgate": bg, "biasB": biasB, "band": band, "ident": ident, "triU": triU,
    }
    maps = []
    for core in range(8):
        b, hf = core // 2, core % 2
        perm = (np.arange(S) + hf * 4096) % S
        em = np.zeros((4, 384), np.float32)
        for i, t in enumerate(SPECIAL):
            orig = (t + hf * 32) % 64
            if orig == 0:
                em[i, 0:128] = NEG
            if orig == 63:
                em[i, 256:384] = NEG
        m = dict(shared)
        m["x"] = np.ascontiguousarray(x[b][perm])
        m["ropeC"] = np.ascontiguousarray(ropeC[perm])
        m["ropeS"] = np.ascontiguousarray(ropeS[perm])
        m["emask"] = em
        maps.append(m)
    return maps


def build_program(stop=None, dbg=(), nt1=NT, flags=(), limit=None):
    nc = bass.Bass("TRN2", target_bir_lowering=False)
    dt_in = lambda n, s, d=F32: nc.dram_tensor(n, s, d, kind="ExternalInput").ap()
    dt_sc = lambda n, s, d: nc.dram_tensor(n, s, d, kind=("ExternalOutput" if n in dbg else "Internal")).ap()
    x = dt_in("x", [S, D])
    w_in = dt_in("w_in", [D, 7168]); w_pa = dt_in("w_pa", [1024, D]); w_pb = dt_in("w_pb", [1024, D])
    w_o = dt_in("w_o", [D, D]); w_r = dt_in("w_r", [D, NE])
    w_g = dt_in("w_g", [NE, D, FF]); w_u = dt_in("w_u", [NE, D, FF]); w_d = dt_in("w_d", [NE, FF, D])
    g_mix = dt_in("g_mix", [1, D]); g_ffn = dt_in("g_ffn", [1, D]); g_fin = dt_in("g_fin", [1, D])
    qn = dt_in("qn", [1, 128]); kn = dt_in("kn", [1, 128]); sink = dt_in("sink", [1, 8])
    bgate_d = dt_in("bgate", [128, 32]); biasB_d = dt_in("biasB", [128, 8, 384]); band_d = dt_in("band", [128, 384])
    ident_d = dt_in("ident", [128, 128], BF16); triU_d = dt_in("triU", [128, 128], BF16)
    ropeC = dt_in("ropeC", [S, 128]); ropeS = dt_in("ropeS", [S, 128]); emask_d = dt_in("emask", [4, 384])
    out = nc.dram_tensor("out", [4096, D], F32, kind="ExternalOutput").ap()

    w_in_bf = dt_sc("w_in_bf", [D, 7168], BF16)
    wgt_s = dt_sc("wgt_s", [32, 128, KC, 128], BF16)
    wp_s = dt_sc("wp_s", [2, 16, 128, 8, 128], BF16)
    wr_bf = dt_sc("wr_bf", [D, NE], BF16)
    wg_bf = dt_sc("wg_bf", [NE, D, FF], BF16); wu_bf = dt_sc("wu_bf", [NE, D, FF], BF16)
    wd_bf = dt_sc("wd_bf", [NE, FF, D], BF16)
    hT_s = dt_sc("hT_s", [16, 128, KC, 512], BF16)
    wq_s = dt_sc("wq_s", [4, 128, KC, 512], BF16)
    wo_s = dt_sc("wo_s", [4, 128, KC, 512], BF16)
    kbT_s = dt_sc("kbT_s", [2, 128, S], BF16)
    vb_s = dt_sc("vb_s", [S, 256], BF16)
    oT_s = dt_sc("oT_s", [2, 16, 128, 8, 512], BF16)
    x1_s = dt_sc("x1_s", [4096, D], F32)
    h2_s = dt_sc("h2_s", [4096, D], BF16)
    Xg = [dt_sc("Xg%d" % i, [CAP, D], BF16) for i in range(NE)]
    Yg = [dt_sc("Yg%d" % i, [CAP, D], BF16) for i in range(NE)]

    with ExitStack() as stack:
        T = Trk(nc, stack)
        T.limit = limit
        build_program.T = T
        sb = lambda n, s, d: stack.enter_context(nc.sbuf_tensor("sb_" + n, s, d))
        B = Buf

        banks = [stack.enter_context(nc.psum_tensor("ps%d" % i, [128, 512], F32)) for i in range(8)]
        bankb = [B("bank%d" % i, True) for i in range(8)]

        class Rot:
            def __init__(self, ids):
                self.ids = ids; self.i = 0

            def get(self):
                k = self.ids[self.i % len(self.ids)]; self.i += 1
                return banks[k], bankb[k]

        b_win = [B("win%d" % i) for i in range(4)]
        for i in range(4):
            T.op("pool", lambda e, i=i: e.dma_start(
                out=w_in_bf[i * 512:(i + 1) * 512, :].rearrange("r (a c) -> r a c", c=1024),
                in_=w_in[i * 512:(i + 1) * 512, :].rearrange("r (a c) -> r a c", c=1024)), writes=[b_win[i]], dma=True, nd=3584)
        b_wr = B("wr")
        b_wqs = [B("wqs%d" % i) for i in range(4)]; b_wosc = [B("wos_s%d" % i) for i in range(4)]
        b_wgt = [B("wgt%d" % i) for i in range(32)]
        b_wp = [B("wp%d" % i) for i in range(32)]
        for g_, c0 in enumerate((0, 512, 1536, 2048)):
            T.op("pool", lambda e, g_=g_, c0=c0: e.dma_start(
                out=wq_s[g_], in_=w_in[:, c0:c0 + 512].rearrange("(k p) c -> p k c", p=128)), writes=[b_wqs[g_]], dma=True)
        deferred = []
        for g_ in range(4):
            deferred.append(lambda g_=g_: T.op("pool", lambda e: e.dma_start(
                out=wo_s[g_], in_=w_o[:, g_ * 512:(g_ + 1) * 512].rearrange("(k p) c -> p k c", p=128)), writes=[b_wosc[g_]], dma=True))
        deferred.append(lambda: T.op("pool", lambda e: e.dma_start(out=wr_bf, in_=w_r), writes=[b_wr], dma=True))
        for n_ in range(32):
            deferred.append(lambda n_=n_: T.op("pool", lambda e: e.dma_start(
                out=wgt_s[n_], in_=w_in[:, 3072 + n_ * 128:3072 + (n_ + 1) * 128].rearrange("(k p) c -> p k c", p=128)),
                writes=[b_wgt[n_]], dma=True))
        for ab, wsrc in enumerate((w_pa, w_pb)):
            for n_ in range(16):
                deferred.append(lambda ab=ab, n_=n_, wsrc=wsrc: T.op("pool", lambda e: e.dma_start(
                    out=wp_s[ab, n_], in_=wsrc[:, n_ * 128:(n_ + 1) * 128].rearrange("(h p) c -> p h c", p=128)),
                    writes=[b_wp[ab * 16 + n_]], dma=True))
        b_wg = [B("wg%d" % i) for i in range(NE)]; b_wu = [B("wu%d" % i) for i in range(NE)]
        b_wd = [B("wd%d" % i) for i in range(NE)]

        def cast_expert(e_):
            T.op("pool", lambda e: e.dma_start(out=wg_bf[e_], in_=w_g[e_]), writes=[b_wg[e_]], dma=True)
            T.op("pool", lambda e: e.dma_start(out=wu_bf[e_], in_=w_u[e_]), writes=[b_wu[e_]], dma=True)
            T.op("pool", lambda e: e.dma_start(out=wd_bf[e_].rearrange("r (a c) -> r a c", c=1024),
                                               in_=w_d[e_].rearrange("r (a c) -> r a c", c=1024)),
                 writes=[b_wd[e_]], dma=True)

        ident = sb("ident", [128, 128], BF16); ones = sb("ones", [128, 128], BF16); triU = sb("triU", [128, 128], BF16)
        qn_bc = sb("qn_bc", [128, 128], F32); kn_bc = sb("kn_bc", [128, 128], F32)
        bgate = sb("bgate", [128, 32], F32); sink_bc = sb("sink_bc", [128, 8], F32); nsink_bc = sb("nsink_bc", [128, 8], F32)
        aff = sb("aff", [128, NT, NE], F32)
        b_const = B("const"); b_aff = [B("aff%d" % j) for j in range(NT)]
        T.op("sp", lambda e: e.dma_start(out=ident[:], in_=ident_d), writes=[b_const], dma=True)
        T.op("sp", lambda e: e.dma_start(out=triU[:], in_=triU_d), writes=[b_const], dma=True)
        T.op("sp", lambda e: e.dma_start(out=qn_bc[:], in_=qn.partition_broadcast(128)), writes=[b_const], dma=True)
        T.op("sp", lambda e: e.dma_start(out=kn_bc[:], in_=kn.partition_broadcast(128)), writes=[b_const], dma=True)
        T.op("sp", lambda e: e.dma_start(out=bgate[:], in_=bgate_d), writes=[b_const], dma=True)
        T.op("sp", lambda e: e.dma_start(out=sink_bc[:], in_=sink.partition_broadcast(128)), writes=[b_const], dma=True)
        T.op("dve", lambda e: e.memset(ones[:], 1.0), writes=[b_const])
        T.op("dve", lambda e: e.tensor_scalar(out=nsink_bc[:], in0=sink_bc[:], scalar1=-1.0, scalar2=None, op0=ALU.mult),
             reads=[b_const], writes=[b_const])

        if stop == 0:
            T.sync_all()
            return nc

        def rstd_from_ss(ss, tmp, rstd, n, inv_n, bufs):
            T.op("dve", lambda e: e.tensor_scalar(out=tmp[:, 0:n], in0=ss[:, 0:n], scalar1=inv_n, scalar2=EPS,
                                                  op0=ALU.mult, op1=ALU.add), reads=bufs, writes=bufs)
            T.op("act", lambda e: e.activation(out=tmp[:, 0:n], in_=tmp[:, 0:n], func=AF.Ln), reads=bufs, writes=bufs)
            T.op("act", lambda e: e.activation(out=rstd[:, 0:n], in_=tmp[:, 0:n], func=AF.Exp, scale=-0.5), reads=bufs, writes=bufs)

        with ExitStack() as kvstack:
            sbk = lambda n, s, d: kvstack.enter_context(nc.sbuf_tensor("sb_" + n, s, d))
            KAT = sbk("KAT", [128, 2, S], BF16)
            VA = sbk("VA", [128, NT, 256], BF16)
            b_kat = [B("kat%d" % j) for j in range(NT)]
            b_va = [B("va%d" % j) for j in range(NT)]
            b_hT = [B("hTs%d" % j) for j in range(NT)]
            b_kb = [B("kbs%d" % j) for j in range(NT)]
            b_vb = [B("vbs%d" % j) for j in range(NT)]

            with ExitStack() as p1:
                s1 = lambda n, s, d: p1.enter_context(nc.sbuf_tensor("sb_" + n, s, d))
                NB1 = 3
                g_bc = s1("g_bc", [128, D], F32); b_g = B("g_bc")
                wkv = s1("wkv", [128, KC, 1024], BF16); b_wkv = B("wkv")
                xt = [s1("xt%d" % i, [128, D], F32) for i in range(NB1)]; b_xt = [B("xt%d" % i) for i in range(NB1)]
                junk = s1("junk", [128, D], BF16); b_junk = B("junk")
                hb = [s1("hb%d" % i, [128, D], BF16) for i in range(NB1)]; b_hb = [B("hb%d" % i) for i in range(NB1)]
                hTt = [s1("hTt%d" % i, [128, KC, 128], BF16) for i in range(NB1)]; b_hTt = [B("hTt%d" % i) for i in range(NB1)]
                st = [s1("st%d" % i, [128, 16], F32) for i in range(NB1)]; b_st = [B("st%d" % i) for i in range(NB1)]
                ksb_ = [s1("ksb%d" % i, [128, 256], F32) for i in range(NB1)]; ksq_ = [s1("ksq%d" % i, [128, 256], F32) for i in range(NB1)]
                knm_ = [s1("knm%d" % i, [128, 2, 128], F32) for i in range(NB1)]
                kt1_ = [s1("kt1%d" % i, [128, 2, 128], F32) for i in range(NB1)]; kt2_ = [s1("kt2%d" % i, [128, 2, 128], F32) for i in range(NB1)]
                krot_ = [s1("krot%d" % i, [128, 2, 128], BF16) for i in range(NB1)]; b_k_ = [B("kscratch%d" % i) for i in range(NB1)]
                rC = [s1("rC%d" % i, [128, 128], F32) for i in range(NB1)]; rS = [s1("rS%d" % i, [128, 128], F32) for i in range(NB1)]
                b_rope = [B("rope%d" % i) for i in range(NB1)]
                kbb_ = [s1("kbb%d" % i, [128, 256], BF16) for i in range(NB1)]; b_kbb_ = [B("kbb%d" % i) for i in range(NB1)]
                kbTt = [s1("kbTt%d" % i, [128, 2, 128], BF16) for i in range(NB1)]; b_kbTt = [B("kbTt%d" % i) for i in range(NB1)]
                vbb = [s1("vbb%d" % i, [128, 256], BF16) for i in range(NB1)]; b_vbb = [B("vbb%d" % i) for i in range(NB1)]
                rot_t = Rot([0, 1]); rot_m = Rot([2, 3, 4, 5]); rot_k = Rot([6, 7])

                T.op("sp", lambda e: e.dma_start(out=g_bc[:], in_=g_mix.partition_broadcast(128)), writes=[b_g], dma=True)
                for hh, c0 in enumerate((1024, 2560)):
                    T.op("sp", lambda e, hh=hh, c0=c0: e.dma_start(
                        out=wkv[:, :, hh * 512:(hh + 1) * 512],
                        in_=w_in_bf[:, c0:c0 + 512].rearrange("(k p) c -> p k c", p=128)),
                        reads=b_win, writes=[b_wkv], dma=True)

                def p1_head(j):
                        i2 = j % NB1
                        X, bX = xt[i2], b_xt[i2]
                        ksb, ksq, knm, kt1, kt2, krot, b_k = ksb_[i2], ksq_[i2], knm_[i2], kt1_[i2], kt2_[i2], krot_[i2], b_k_[i2]
                        kbb, b_kbb = kbb_[i2], b_kbb_[i2]
                        T.op("sp", lambda e, X=X, j=j: e.dma_start(out=X[:], in_=x[j * 128:(j + 1) * 128, :]), writes=[bX], dma=True)
                        T.op("sp", lambda e, j=j, i2=i2: e.dma_start(out=rC[i2][:], in_=ropeC[j * 128:(j + 1) * 128, :]),
                             writes=[b_rope[i2]], dma=True)
                        T.op("sp", lambda e, j=j, i2=i2: e.dma_start(out=rS[i2][:], in_=ropeS[j * 128:(j + 1) * 128, :]),
                             writes=[b_rope[i2]], dma=True)
                        ST, bST = st[i2], b_st[i2]
                        T.op("act", lambda e, X=X, ST=ST: e.activation(out=junk[:], in_=X[:], func=AF.Square, accum_out=ST[:, 0:1]),
                             reads=[bX], writes=[b_junk, bST])
                        rstd_from_ss(ST[:, 0:1], ST[:, 1:2], ST[:, 2:3], 1, 1.0 / D, [bST])
                        H, bH = hb[i2], b_hb[i2]
                        T.op("dve", lambda e, H=H, X=X, ST=ST: e.scalar_tensor_tensor(
                            out=H[:], in0=X[:], scalar=ST[:, 2:3], in1=g_bc[:], op0=ALU.mult, op1=ALU.mult),
                            reads=[bX, bST, b_g], writes=[bH])
                        HT, bHT = hTt[i2], b_hTt[i2]
                        for q4 in range(4):
                            pb, pbb = rot_t.get()
                            pv = pb[:].bitcast(BF16)
                            for kk in range(4):
                                k = q4 * 4 + kk
                                T.op("pe", lambda e, pv=pv, kk=kk, H=H, k=k: e.transpose(
                                    out=pv[:, kk * 128:(kk + 1) * 128], in_=H[:, k * 128:(k + 1) * 128], identity=ident[:]),
                                    reads=[bH, b_const], writes=[pbb])
                            ce = "act" if q4 % 2 == 0 else "dve"
                            if ce == "act":
                                T.op("act", lambda e, HT=HT, q4=q4, pv=pv: e.copy(
                                    out=HT[:, q4 * 4:(q4 + 1) * 4, :], in_=pv[:, 0:512].rearrange("p (k t) -> p k t", t=128)),
                                    reads=[pbb], writes=[bHT])
                            else:
                                T.op("dve", lambda e, HT=HT, q4=q4, pv=pv: e.tensor_copy(
                                    out=HT[:, q4 * 4:(q4 + 1) * 4, :], in_=pv[:, 0:512].rearrange("p (k t) -> p k t", t=128)),
                                    reads=[pbb], writes=[bHT])
                        T.op("pool", lambda e, HT=HT, j=j: e.dma_start(
                            out=hT_s[j // 4, :, :, (j % 4) * 128:(j % 4 + 1) * 128], in_=HT[:]),
                            reads=[bHT], writes=[b_hT[j]], dma=True)
                        pA, pAb = rot_m.get(); pB, pBb = rot_m.get()
                        for k in range(KC):
                            T.op("pe", lambda e, pA=pA, HT=HT, k=k: e.matmul(pA[:], lhsT=HT[:, k, :], rhs=wkv[:, k, 0:512],
                                                                              start=(k == 0), stop=(k == KC - 1)),
                                 reads=[bHT, b_wkv], writes=[pAb])
                        for k in range(KC):
                            T.op("pe", lambda e, pB=pB, HT=HT, k=k: e.matmul(pB[:], lhsT=HT[:, k, :], rhs=wkv[:, k, 512:1024],
                                                                              start=(k == 0), stop=(k == KC - 1)),
                                 reads=[bHT, b_wkv], writes=[pBb])
                        return (j, i2, X, bX, ksb, ksq, knm, kt1, kt2, krot, b_k, kbb, b_kbb, ST, bST, H, bH, HT, bHT, pA, pAb, pB, pBb)

                def p1_tail(L):
                        (j, i2, X, bX, ksb, ksq, knm, kt1, kt2, krot, b_k, kbb, b_kbb, ST, bST, H, bH, HT, bHT, pA, pAb, pB, pBb) = L
                        T.op("act", lambda e, pA=pA, j=j: e.copy(out=VA[:, j, :], in_=pA[:, 256:512]), reads=[pAb], writes=[b_va[j]])
                        T.op("act", lambda e, pB=pB, kbb=kbb: e.copy(out=kbb[:], in_=pB[:, 0:256]), reads=[pBb], writes=[b_kbb])
                        VB_, bVB_ = vbb[i2], b_vbb[i2]
                        T.op("act", lambda e, pB=pB, VB_=VB_: e.copy(out=VB_[:], in_=pB[:, 256:512]), reads=[pBb], writes=[bVB_])
                        T.op("pool", lambda e, VB_=VB_, j=j: e.dma_start(out=vb_s[j * 128:(j + 1) * 128, :], in_=VB_[:]),
                             reads=[bVB_], writes=[b_vb[j]], dma=True, nd=160)
                        T.op("dve", lambda e, pA=pA, ksb=ksb: e.tensor_copy(out=ksb[:], in_=pA[:, 0:256]), reads=[pAb], writes=[b_k])
                        T.op("dve", lambda e, ksq=ksq, ksb=ksb: e.tensor_tensor(out=ksq[:], in0=ksb[:], in1=ksb[:], op=ALU.mult), reads=[b_k], writes=[b_k])
                        T.op("dve", lambda e, ST=ST, ksq=ksq: e.tensor_reduce(out=ST[:, 4:6], in_=ksq[:].rearrange("p (h d) -> p h d", d=128),
                                                                      axis=AX.X, op=ALU.add), reads=[b_k], writes=[bST])
                        rstd_from_ss(ST[:, 4:6], ST[:, 6:8], ST[:, 8:10], 2, 1.0 / 128, [bST])
                        for hh in range(2):
                            T.op("dve", lambda e, hh=hh, ST=ST, knm=knm, ksb=ksb: e.scalar_tensor_tensor(
                                out=knm[:, hh, :], in0=ksb[:, hh * 128:(hh + 1) * 128], scalar=ST[:, 8 + hh:9 + hh], in1=kn_bc[:],
                                op0=ALU.mult, op1=ALU.mult), reads=[b_k, bST, b_const], writes=[b_k])
                        rope(T, knm, kt1, kt2, krot, rC[i2], rS[i2], 2, [b_k, b_rope[i2]], [b_k])
                        pk, pkb = rot_k.get()
                        pkv = pk[:].bitcast(BF16)
                        for hh in range(2):
                            T.op("pe", lambda e, pkv=pkv, hh=hh, krot=krot: e.transpose(out=pkv[:, hh * 128:(hh + 1) * 128], in_=krot[:, hh, :],
                                                                             identity=ident[:]), reads=[b_k, b_const], writes=[pkb])
                        for hh in range(2):
                            T.op("pe", lambda e, pkv=pkv, hh=hh, kbb=kbb: e.transpose(out=pkv[:, 256 + hh * 128:256 + (hh + 1) * 128],
                                                                             in_=kbb[:, hh * 128:(hh + 1) * 128], identity=ident[:]),
                                 reads=[b_kbb, b_const], writes=[pkb])
                        T.op("dve", lambda e, pkv=pkv, j=j: e.tensor_copy(
                            out=KAT[:, :, j * 128:(j + 1) * 128], in_=pkv[:, 0:256].rearrange("p (h t) -> p h t", t=128)),
                            reads=[pkb], writes=[b_kat[j]])
                        KBT, bKBT = kbTt[i2], b_kbTt[i2]
                        T.op("act", lambda e, pkv=pkv, KBT=KBT: e.copy(out=KBT[:], in_=pkv[:, 256:512].rearrange("p (h t) -> p h t", t=128)),
                             reads=[pkb], writes=[bKBT])
                        T.op("pool", lambda e, KBT=KBT, j=j: e.dma_start(
                            out=kbT_s[:, :, j * 128:(j + 1) * 128].rearrange("h p t -> p h t"), in_=KBT[:]),
                            reads=[bKBT], writes=[b_kb[j]], dma=True, nd=300)

                Lp = p1_head(0)
                for j in range(nt1):
                    Ln_ = p1_head(j + 1) if j + 1 < nt1 else None
                    p1_tail(Lp)
                    Lp = Ln_
                T.sync_all()
                if stop == 1:
                    return nc

            b_oT = [B("oT%d" % i) for i in range(16)]
            with ExitStack() as p2:
                s2 = lambda n, s, d: p2.enter_context(nc.sbuf_tensor("sb_" + n, s, d))
                hTb = s2("hTb", [128, KC, 512], BF16); b_hTb = B("hTb")
                wq = [s2("wq%d" % i, [128, KC, 512], BF16) for i in range(2)]; b_wq = [B("wq0"), B("wq1")]
                qAT = s2("qAT", [128, 8, 512], BF16); b_qAT = B("qAT")
                qBT = s2("qBT", [128, 8, 512], BF16); b_qBT = B("qBT")
                oAT = s2("oAT", [128, 8, 512], BF16); b_oAT = B("oAT")
                oBT = s2("oBT", [128, 8, 512], BF16); b_oBT = B("oBT")
                biasB = s2("biasB", [128, 8, 384], F32); b_bias = B("biasB")
                emk = s2("emk", [128, 384], F32); b_emk = B("emk")
                qsb_ = [s2("qsb%d" % i, [128, 512], F32) for i in range(2)]; qnm_ = [s2("qnm%d" % i, [128, 4, 128], F32) for i in range(2)]
                qt1_ = [s2("qt1%d" % i, [128, 4, 128], F32) for i in range(2)]; qt2_ = [s2("qt2%d" % i, [128, 4, 128], F32) for i in range(2)]
                qrot_ = [s2("qrot%d" % i, [128, 4, 128], BF16) for i in range(2)]
                b_q_ = [B("qscratch0"), B("qscratch1")]
                st2_ = [s2("st2%d" % i, [128, 16], F32) for i in range(2)]; b_st2_ = [B("st20"), B("st21")]
                qunit = 0
                rC2 = [s2("rC2%d" % i, [128, 128], F32) for i in range(2)]; rS2 = [s2("rS2%d" % i, [128, 128], F32) for i in range(2)]
                b_rope2 = [B("rope20"), B("rope21")]
                PT = [s2("PT%d" % i, [128, 512], BF16) for i in range(4)]; b_PT = [B("PT%d" % i) for i in range(4)]
                rD = [s2("rD0", [128, 512], F32)] * 2; b_rD = [B("rD0")] * 2
                kbw = s2("kbw", [128, 2, 768], BF16); vbw = s2("vbw", [128, 6, 256], BF16); b_kbw = B("kbw"); b_vbw = B("vbw")
                ssb = s2("ssb", [128, 4, 384], F32); pbf = s2("pbf", [128, 4, 384], BF16)
                pTs = s2("pTs", [128, 12, 128], BF16); b_bs = B("b_s"); b_be = B("b_e"); b_bp = B("b_p"); b_bpT = B("b_pT")
                stB = s2("stB", [128, 32], F32); b_stB = B("stB")

                T.op("sp", lambda e: e.dma_start(out=biasB[:], in_=biasB_d), writes=[b_bias], dma=True)
                T.op("sp", lambda e: e.dma_start(out=emk[:], in_=band_d), writes=[b_emk], dma=True)
                T.op("dve", lambda e: e.tensor_tensor(out=biasB[:], in0=biasB[:], in1=emk[:].unsqueeze(1).to_broadcast([128, 8, 384]),
                                                      op=ALU.add), reads=[b_bias, b_emk], writes=[b_bias])
                rot_q = Rot([6, 7]); rot_s = Rot([0, 1, 2])

                for bi in range(16):
                    t0 = bi * 512
                    if bi == 0:
                        T.op("sp", lambda e: e.dma_start(out=hTb[:], in_=hT_s[0]), reads=b_hT[0:4], writes=[b_hTb], dma=True)
                        T.op("sp", lambda e: e.dma_start(out=wq[0][:], in_=wq_s[0]), reads=[b_wqs[0]], writes=[b_wq[0]], dma=True)
                        T.op("sp", lambda e: e.dma_start(out=wq[1][:], in_=wq_s[2]), reads=[b_wqs[2]], writes=[b_wq[1]], dma=True)
                    jl = (bi * 4 - 1) % NT
                    jr = (bi * 4 + 4) % NT
                    segs = [(0, jl, 1), (1, bi * 4, 4), (5, jr, 1)]
                    for (w0, j0, n) in segs:
                        T.op("sp", lambda e, w0=w0, j0=j0, n=n: e.dma_start(
                            out=kbw[:, :, w0 * 128:(w0 + n) * 128],
                            in_=kbT_s[:, :, j0 * 128:(j0 + n) * 128].rearrange("h p t -> p h t")),
                            reads=b_kb[j0:j0 + n], writes=[b_kbw], dma=True)
                        T.op("sp", lambda e, w0=w0, j0=j0, n=n: e.dma_start(
                            out=vbw[:, w0:w0 + n, :],
                            in_=vb_s[j0 * 128:(j0 + n) * 128, :].rearrange("(t p) c -> p t c", p=128)),
                            reads=b_vb[j0:j0 + n], writes=[b_vbw], dma=True)
                    pendq = None

                    def finish_unit(u):
                        (qrot_u, b_q_u, cg_u, tt_u) = u
                        pt, ptb = rot_s.get()
                        ptv = pt[:].bitcast(BF16)
                        for hh in range(4):
                            T.op("pe", lambda e, ptv=ptv, hh=hh: e.transpose(out=ptv[:, hh * 128:(hh + 1) * 128], in_=qrot_u[:, hh, :],
                                                                             identity=ident[:]), reads=[b_q_u, b_const], writes=[ptb])
                        T.op("dve", lambda e, ptv=ptv: e.tensor_copy(
                            out=qAT[:, cg_u * 4:(cg_u + 1) * 4, tt_u * 128:(tt_u + 1) * 128],
                            in_=ptv[:, 0:512].rearrange("p (h t) -> p h t", t=128)), reads=[ptb], writes=[b_qAT])

                    rot_qb = Rot([3, 4])
                    for cg in range(2):
                        WA, bWA = wq[0], b_wq[0]
                        WB, bWB = wq[1], b_wq[1]
                        if cg == 1:
                            T.op("sp", lambda e: e.dma_start(out=WA[:], in_=wq_s[1]), reads=[b_wqs[1]], writes=[bWA], dma=True)
                            T.op("act", lambda e: e.dma_start(out=WB[:], in_=wq_s[3]), reads=[b_wqs[3]], writes=[bWB], dma=True)
                        W, bW = WA, bWA
                        for tt in range(4):
                            j = bi * 4 + tt
                            i2 = (cg * 4 + tt) % 2
                            T.op("sp", lambda e, j=j, i2=i2: e.dma_start(out=rC2[i2][:], in_=ropeC[j * 128:(j + 1) * 128, :]),
                                 writes=[b_rope2[i2]], dma=True)
                            T.op("sp", lambda e, j=j, i2=i2: e.dma_start(out=rS2[i2][:], in_=ropeS[j * 128:(j + 1) * 128, :]),
                                 writes=[b_rope2[i2]], dma=True)
                            us = qunit % 2; qunit += 1
                            qsb, qnm, qt1, qt2, qrot, b_q, st2, b_st2 = qsb_[us], qnm_[us], qt1_[us], qt2_[us], qrot_[us], b_q_[us], st2_[us], b_st2_[us]
                            pq, pqb = rot_q.get()
                            for k in range(KC):
                                T.op("pe", lambda e, pq=pq, k=k, tt=tt, W=W: e.matmul(
                                    pq[:], lhsT=hTb[:, k, tt * 128:(tt + 1) * 128], rhs=W[:, k, :], start=(k == 0), stop=(k == KC - 1)),
                                    reads=[b_hTb, bW], writes=[pqb])
                            if pendq is not None:
                                finish_unit(pendq)
                            for hh in range(4):
                                T.op("act", lambda e, pq=pq, hh=hh: e.activation(out=qsb[:, hh * 128:(hh + 1) * 128], in_=pq[:, hh * 128:(hh + 1) * 128],
                                                                                 func=AF.Square, accum_out=st2[:, hh:hh + 1]),
                                     reads=[pqb], writes=[b_q, b_st2])
                            T.op("dve", lambda e: e.tensor_scalar(out=st2[:, 4:8], in0=st2[:, 0:4], scalar1=1.0 / 128, scalar2=EPS,
                                                                  op0=ALU.mult, op1=ALU.add), reads=[b_st2], writes=[b_st2])
                            T.op("act", lambda e: e.activation(out=st2[:, 4:8], in_=st2[:, 4:8], func=AF.Ln), reads=[b_st2], writes=[b_st2])
                            T.op("act", lambda e: e.activation(out=st2[:, 8:12], in_=st2[:, 4:8], func=AF.Exp, scale=-0.5), reads=[b_st2], writes=[b_st2])
                            for hh in range(4):
                                T.op("dve", lambda e, hh=hh, pq=pq: e.scalar_tensor_tensor(
                                    out=qnm[:, hh, :], in0=pq[:, hh * 128:(hh + 1) * 128], scalar=st2[:, 8 + hh:9 + hh], in1=qn_bc[:],
                                    op0=ALU.mult, op1=ALU.mult), reads=[pqb, b_st2, b_const], writes=[b_q])
                            rope(T, qnm, qt1, qt2, qrot, rC2[i2], rS2[i2], 4, [b_q, b_rope2[i2]], [b_q])
                            pendq = (qrot, b_q, cg, tt)
                            hB = cg * 4 + tt
                            pqB, pqBb = rot_qb.get()
                            for k in range(KC):
                                T.op("pe", lambda e, pqB=pqB, k=k, tt=tt: e.matmul(
                                    pqB[:], lhsT=WB[:, k, tt * 128:(tt + 1) * 128], rhs=hTb[:, k, :], start=(k == 0), stop=(k == KC - 1)),
                                    reads=[b_hTb, bWB], writes=[pqBb])
                            T.op("act", lambda e, pqB=pqB, hB=hB: e.activation(out=qBT[:, hB, :], in_=pqB[:], func=AF.Copy, scale=SCALE),
                                 reads=[pqBb], writes=[b_qBT])
                    finish_unit(pendq)
                    if bi < 15:
                        T.op("sp", lambda e, bi=bi: e.dma_start(out=hTb[:], in_=hT_s[bi + 1]),
                             reads=b_hT[bi * 4 + 4:bi * 4 + 8], writes=[b_hTb], dma=True)
                        T.op("sp", lambda e: e.dma_start(out=wq[0][:], in_=wq_s[0]), reads=[b_wqs[0]], writes=[b_wq[0]], dma=True)
                        T.op("sp", lambda e: e.dma_start(out=wq[1][:], in_=wq_s[2]), reads=[b_wqs[2]], writes=[b_wq[1]], dma=True)
                    pB7, pB7b = banks[7], bankb[7]

                    def b_stages(tt, kvhb):
                        j = bi * 4 + tt
                        special = j in SPECIAL
                        st = []

                        def s_head(hh):
                            h = kvhb * 4 + hh
                            if hh == 0 and special:
                                si = SPECIAL.index(j)
                                T.op("sp", lambda e: e.dma_start(out=emk[:], in_=emask_d[si:si + 1, :].partition_broadcast(128)),
                                     writes=[b_emk], dma=True)
                            T.op("pe", lambda e: e.matmul(pB7[:, 0:384], lhsT=qBT[:, h, tt * 128:(tt + 1) * 128],
                                                          rhs=kbw[:, kvhb, tt * 128:tt * 128 + 384], start=True, stop=True),
                                 reads=[b_qBT, b_kbw], writes=[pB7b])
                            T.op("dve", lambda e: e.tensor_tensor(out=ssb[:, hh, :], in0=pB7[:, 0:384], in1=biasB[:, h, :], op=ALU.add),
                                 reads=[pB7b, b_bias], writes=[b_bs])
                        for hh in range(4):
                            st.append(lambda hh=hh: s_head(hh))

                        def chain():
                            if special:
                                T.op("dve", lambda e: e.tensor_tensor(out=ssb[:], in0=ssb[:], in1=emk[:].unsqueeze(1).to_broadcast([128, 4, 384]),
                                                                      op=ALU.add), reads=[b_bs, b_emk], writes=[b_bs])
                            T.op("dve", lambda e: e.tensor_reduce(out=stB[:, 0:4], in_=ssb[:], axis=AX.X, op=ALU.max, negate=True),
                                 reads=[b_bs], writes=[b_stB])
                            T.op("dve", lambda e: e.tensor_tensor(out=stB[:, 4:8], in0=stB[:, 0:4], in1=nsink_bc[:, kvhb * 4:kvhb * 4 + 4],
                                                                  op=ALU.min), reads=[b_stB, b_const], writes=[b_stB])
                            for hh in range(4):
                                T.op("act", lambda e, hh=hh: e.activation(out=ssb[:, hh, :], in_=ssb[:, hh, :], func=AF.Exp,
                                                                          bias=stB[:, 4 + hh:5 + hh], scale=1.0, accum_out=stB[:, 8 + hh:9 + hh]),
                                     reads=[b_bs, b_stB], writes=[b_bs, b_stB])
                            T.op("dve", lambda e: e.tensor_tensor(out=stB[:, 12:16], in0=stB[:, 4:8], in1=sink_bc[:, kvhb * 4:kvhb * 4 + 4],
                                                                  op=ALU.add), reads=[b_stB, b_const], writes=[b_stB])
                            T.op("act", lambda e: e.activation(out=stB[:, 12:16], in_=stB[:, 12:16], func=AF.Exp), reads=[b_stB], writes=[b_stB])
                            T.op("dve", lambda e: e.tensor_tensor(out=stB[:, 16:20], in0=stB[:, 8:12], in1=stB[:, 12:16], op=ALU.add),
                                 reads=[b_stB], writes=[b_stB])
                            T.op("dve", lambda e: e.reciprocal(out=stB[:, 20:24], in_=stB[:, 16:20]), reads=[b_stB], writes=[b_stB])
                            T.op("dve", lambda e: e.tensor_tensor(out=pbf[:], in0=ssb[:], in1=stB[:, 20:24].unsqueeze(2).to_broadcast([128, 4, 384]),
                                                                  op=ALU.mult), reads=[b_bs, b_stB], writes=[b_bp])
                        st.append(chain)

                        def transposes(half):
                            ptv = pB7[:].bitcast(BF16)
                            for hq in range(2):
                                hh = half * 2 + hq
                                for jj in range(3):
                                    T.op("pe", lambda e, hq=hq, hh=hh, jj=jj: e.transpose(
                                        out=ptv[:, (hq * 3 + jj) * 128:(hq * 3 + jj + 1) * 128], in_=pbf[:, hh, jj * 128:(jj + 1) * 128],
                                        identity=ident[:]), reads=[b_bp, b_const], writes=[pB7b])
                            T.op("dve", lambda e: e.tensor_copy(out=pTs[:, half * 6:half * 6 + 6, :],
                                                                in_=ptv[:, 0:768].rearrange("p (j t) -> p j t", t=128)),
                                 reads=[pB7b], writes=[b_bpT])
                        st.append(lambda: transposes(0))
                        st.append(lambda: transposes(1))

                        def pv():
                            for hh in range(4):
                                for jj in range(3):
                                    T.op("pe", lambda e, hh=hh, jj=jj: e.matmul(
                                        pB7[:, hh * 128:(hh + 1) * 128], lhsT=vbw[:, tt + jj, kvhb * 128:(kvhb + 1) * 128],
                                        rhs=pTs[:, hh * 3 + jj, :], start=(jj == 0), stop=(jj == 2), skip_group_check=True),
                                        reads=[b_vbw, b_bpT], writes=[pB7b])
                            T.op("dve", lambda e: e.tensor_copy(
                                out=oBT[:, kvhb * 4:(kvhb + 1) * 4, tt * 128:(tt + 1) * 128], in_=pB7[:].rearrange("p (h t) -> p h t", t=128)),
                                reads=[pB7b], writes=[b_oBT])
                        st.append(pv)
                        return st

                    BSLOT = {2: 0, 6: 1, 10: 2, 14: 3, 15: 4, 40: 5, 46: 6, 54: 7}
                    for h in range(8):
                        kvh = h // 4
                        pO, pOb = banks[3 + (h % 2)], bankb[3 + (h % 2)]
                        pD, pDb = banks[5 + (h % 2)], bankb[5 + (h % 2)]
                        pend = []
                        LA = 2
                        bst = b_stages(h % 4, h // 4)
                        if h == 0:
                            for _ in range(6):
                                if deferred:
                                    deferred.pop(0)()
                            cast_expert(bi)
                        for kb in range(NT + LA):
                            if kb in BSLOT:
                                bst[BSLOT[kb]]()
                            if kb < NT:
                                ps, psb = rot_s.get()
                                T.op("pe", lambda e, ps=ps, kvh=kvh, kb=kb, h=h: e.matmul(
                                    ps[:], lhsT=KAT[:, kvh, kb * 128:(kb + 1) * 128], rhs=qAT[:, h, :], start=True, stop=True),
                                    reads=[b_kat[kb], b_qAT], writes=[psb])
                                P_, bP_ = PT[kb % 4], b_PT[kb % 4]
                                T.op("act", lambda e, P_=P_, ps=ps: e.activation(out=P_[:], in_=ps[:], func=AF.Exp, scale=SCALE),
                                     reads=[psb], writes=[bP_])
                                pend.append((kb, P_, bP_))
                            if kb >= LA:
                                (kb0, P0, bP0) = pend.pop(0)
                                T.op("pe", lambda e, pO=pO, kb0=kb0, kvh=kvh, P0=P0: e.matmul(
                                    pO[:], lhsT=VA[:, kb0, kvh * 128:(kvh + 1) * 128], rhs=P0[:], start=(kb0 == 0), stop=(kb0 == NT - 1)),
                                    reads=[b_va[kb0], bP0], writes=[pOb])
                                T.op("pe", lambda e, pD=pD, kb0=kb0, P0=P0: e.matmul(
                                    pD[:], lhsT=ones[:], rhs=P0[:], start=(kb0 == 0), stop=(kb0 == NT - 1)),
                                    reads=[b_const, bP0], writes=[pDb])
                        R_, bR_ = rD[h % 2], b_rD[h % 2]
                        T.op("dve", lambda e, R_=R_, pD=pD: e.reciprocal(out=R_[:], in_=pD[:]), reads=[pDb], writes=[bR_])
                        T.op("dve", lambda e, R_=R_, pO=pO, h=h: e.tensor_tensor(out=oAT[:, h, :], in0=pO[:], in1=R_[:], op=ALU.mult),
                             reads=[pOb, bR_], writes=[b_oAT])
                    T.op("pool", lambda e, t0=t0: e.dma_start(out=oT_s[0, bi], in_=oAT[:]),
                         reads=[b_oAT], writes=[b_oT[bi]], dma=True)
                    T.op("pool", lambda e, t0=t0: e.dma_start(out=oT_s[1, bi], in_=oBT[:]),
                         reads=[b_oBT], writes=[b_oT[bi]], dma=True)
                while deferred:
                    deferred.pop(0)()
                T.sync_all()
                if stop == 2:
                    return nc

        b_x1 = [B("x1_%d" % j) for j in range(NOWN)]
        b_h2 = [B("h2_%d" % j) for j in range(NOWN)]
        with ExitStack() as p3:
            s3 = lambda n, s, d: p3.enter_context(nc.sbuf_tensor("sb_" + n, s, d))
            g_bc = s3("g_bc2", [128, D], F32); b_g = B("g_bc2")
            wrs = s3("wrs", [128, KC, NE], BF16); b_wrs = B("wrs")
            hTb = s3("hTb2", [128, KC, 512], BF16); b_hTb = B("hTb2")
            oAT = s3("oAT2", [128, 8, 512], BF16); oBT = s3("oBT2", [128, 8, 512], BF16); b_o = B("o2")
            mT = s3("mT", [128, KC, 512], BF16); b_mT = B("mT")
            wgs = [s3("wgs%d" % i, [128, 2, KC, 128], BF16) for i in range(3)]
            wps = [s3("wps%d" % i, [128, 2, 8, 128], BF16) for i in range(3)]
            b_ws = [B("ws%d" % i) for i in range(3)]
            wos = [s3("wos%d" % i, [128, KC, 512], BF16) for i in range(2)]; b_wos = [B("wos0"), B("wos1")]
            gsb = [s3("gsb%d" % i, [128, 512], F32) for i in range(2)]; b_gsb = [B("gsb0"), B("gsb1")]
            m1 = s3("m1", [128, 512], F32); b_m1 = B("m1")
            x1blk = [s3("x1blk%d" % i, [128, D], F32) for i in range(4)]; b_x1blk = [B("x1blk%d" % i) for i in range(4)]
            junk = s3("junk2", [128, D], BF16); b_junk = B("junk2")
            h2b = [s3("h2b%d" % i, [128, D], BF16) for i in range(2)]; b_h2b = [B("h2b0"), B("h2b1")]
            h2T = s3("h2T", [128, KC, 128], BF16); b_h2T = B("h2T")
            st = s3("st3", [128, 16], F32); b_st = B("st3")
            lg = s3("lg", [128, NE], F32); b_lg = B("lg")
            rot_a = Rot([0, 1]); rot_b = Rot([2, 3]); rot_o = Rot([4, 5]); rot_t = Rot([6, 7])

            T.op("sp", lambda e: e.dma_start(out=g_bc[:], in_=g_ffn.partition_broadcast(128)), writes=[b_g], dma=True)
            T.op("sp", lambda e: e.dma_start(out=wrs[:], in_=wr_bf.rearrange("(k p) c -> p k c", p=128)), reads=[b_wr], writes=[b_wrs], dma=True)
            nslot = 0
            for bi in range(16):
                t0 = bi * 512
                T.op("sp", lambda e, t0=t0: e.dma_start(out=hTb[:], in_=hT_s[bi]),
                     reads=b_hT[bi * 4:bi * 4 + 4], writes=[b_hTb], dma=True)
                T.op("sp", lambda e, t0=t0: e.dma_start(out=oAT[:], in_=oT_s[0, bi]),
                     reads=[b_oT[bi]], writes=[b_o], dma=True)
                T.op("sp", lambda e, t0=t0: e.dma_start(out=oBT[:], in_=oT_s[1, bi]),
                     reads=[b_oT[bi]], writes=[b_o], dma=True)
                for n in range(KC):
                    sl = nslot % 3; nslot += 1
                    WG, WP, bW = wgs[sl], wps[sl], b_ws[sl]
                    for ab in range(2):
                        T.op("sp", lambda e, WG=WG, ab=ab, n=n: e.dma_start(out=WG[:, ab], in_=wgt_s[ab * 16 + n]),
                             reads=[b_wgt[ab * 16 + n]], writes=[bW], dma=True)
                        T.op("sp", lambda e, WP=WP, ab=ab, n=n: e.dma_start(out=WP[:, ab], in_=wp_s[ab, n]),
                             reads=[b_wp[ab * 16 + n]], writes=[bW], dma=True)
                    for ab in range(2):
                        pg, pgb = rot_a.get()
                        for k in range(KC):
                            T.op("pe", lambda e, pg=pg, WG=WG, ab=ab, k=k: e.matmul(pg[:], lhsT=WG[:, ab, k, :], rhs=hTb[:, k, :],
                                                                                    start=(k == 0), stop=(k == KC - 1)),
                                 reads=[bW, b_hTb], writes=[pgb])
                        G_, bG_ = gsb[ab], b_gsb[ab]
                        T.op("act", lambda e, G_=G_, pg=pg, ab=ab, n=n: e.activation(out=G_[:], in_=pg[:], func=AF.Sigmoid,
                                                                                     bias=bgate[:, ab * 16 + n:ab * 16 + n + 1], scale=1.0),
                             reads=[pgb, b_const], writes=[bG_])
                        pp, ppb = rot_b.get()
                        osrc = oAT if ab == 0 else oBT
                        for hh in range(8):
                            T.op("pe", lambda e, pp=pp, WP=WP, ab=ab, hh=hh, osrc=osrc: e.matmul(
                                pp[:], lhsT=WP[:, ab, hh, :], rhs=osrc[:, hh, :], start=(hh == 0), stop=(hh == 7)),
                                reads=[bW, b_o], writes=[ppb])
                        if ab == 0:
                            T.op("dve", lambda e, pp=pp, G_=G_: e.tensor_tensor(out=m1[:], in0=pp[:], in1=G_[:], op=ALU.mult),
                                 reads=[ppb, bG_], writes=[b_m1])
                        else:
                            T.op("dve", lambda e, pp=pp, G_=G_: e.tensor_tensor(out=G_[:], in0=pp[:], in1=G_[:], op=ALU.mult),
                                 reads=[ppb, bG_], writes=[bG_])
                            T.op("dve", lambda e, G_=G_, n=n: e.tensor_tensor(out=mT[:, n, :], in0=m1[:], in1=G_[:], op=ALU.add),
                                 reads=[b_m1, bG_], writes=[b_mT])
                for tt in range(4):
                    j = bi * 4 + tt
                    T.op("sp", lambda e, tt=tt, j=j: e.dma_start(out=x1blk[tt][:], in_=x[j * 128:(j + 1) * 128, :]),
                         writes=[b_x1blk[tt]], dma=True)
                for cg in range(4):
                    WO, bWO = wos[cg % 2], b_wos[cg % 2]
                    T.op("sp", lambda e, WO=WO, cg=cg: e.dma_start(
                        out=WO[:], in_=wo_s[cg]), reads=[b_wosc[cg]], writes=[bWO], dma=True)
                    for tt in range(4):
                        po, pob = rot_o.get()
                        for k in range(KC):
                            T.op("pe", lambda e, po=po, k=k, tt=tt, WO=WO: e.matmul(
                                po[:], lhsT=mT[:, k, tt * 128:(tt + 1) * 128], rhs=WO[:, k, :], start=(k == 0), stop=(k == KC - 1)),
                                reads=[b_mT, bWO], writes=[pob])
                        T.op("dve", lambda e, po=po, tt=tt, cg=cg: e.tensor_tensor(
                            out=x1blk[tt][:, cg * 512:(cg + 1) * 512], in0=po[:], in1=x1blk[tt][:, cg * 512:(cg + 1) * 512], op=ALU.add),
                            reads=[pob, b_x1blk[tt]], writes=[b_x1blk[tt]])
                for tt in range(4):
                    j = bi * 4 + tt
                    XB, bXB = x1blk[tt], b_x1blk[tt]
                    if j < NOWN:
                        T.op("pool", lambda e, XB=XB, j=j: e.dma_start(out=x1_s[j * 128:(j + 1) * 128, :], in_=XB[:]),
                             reads=[bXB], writes=[b_x1[j]], dma=True)
                    T.op("act", lambda e, XB=XB: e.activation(out=junk[:], in_=XB[:], func=AF.Square, accum_out=st[:, 0:1]),
                         reads=[bXB], writes=[b_junk, b_st])
                    rstd_from_ss(st[:, 0:1], st[:, 1:2], st[:, 2:3], 1, 1.0 / D, [b_st])
                    H2, bH2 = h2b[tt % 2], b_h2b[tt % 2]
                    T.op("dve", lambda e, H2=H2, XB=XB: e.scalar_tensor_tensor(out=H2[:], in0=XB[:], scalar=st[:, 2:3], in1=g_bc[:],
                                                                                op0=ALU.mult, op1=ALU.mult),
                         reads=[bXB, b_st, b_g], writes=[bH2])
                    if j < NOWN:
                        T.op("pool", lambda e, H2=H2, j=j: e.dma_start(out=h2_s[j * 128:(j + 1) * 128, :], in_=H2[:]),
                             reads=[bH2], writes=[b_h2[j]], dma=True)
                    for q4 in range(4):
                        pb, pbb = rot_t.get()
                        pv = pb[:].bitcast(BF16)
                        for kk in range(4):
                            k = q4 * 4 + kk
                            T.op("pe", lambda e, pv=pv, kk=kk, H2=H2, k=k: e.transpose(
                                out=pv[:, kk * 128:(kk + 1) * 128], in_=H2[:, k * 128:(k + 1) * 128], identity=ident[:]),
                                reads=[bH2, b_const], writes=[pbb])
                        T.op("act", lambda e, q4=q4, pv=pv: e.copy(out=h2T[:, q4 * 4:(q4 + 1) * 4, :],
                                                                   in_=pv[:, 0:512].rearrange("p (k t) -> p k t", t=128)),
                             reads=[pbb], writes=[b_h2T])
                    pl, plb = rot_t.get()
                    for k in range(KC):
                        T.op("pe", lambda e, pl=pl, k=k: e.matmul(pl[:, 0:NE], lhsT=h2T[:, k, :], rhs=wrs[:, k, :],
                                                                  start=(k == 0), stop=(k == KC - 1)),
                             reads=[b_h2T, b_wrs], writes=[plb])
                    T.op("dve", lambda e, pl=pl: e.tensor_reduce(out=st[:, 4:5], in_=pl[:, 0:NE], axis=AX.X, op=ALU.max, negate=True),
                         reads=[plb], writes=[b_st])
                    T.op("act", lambda e, pl=pl: e.activation(out=lg[:], in_=pl[:, 0:NE], func=AF.Exp, bias=st[:, 4:5], scale=1.0,
                                                              accum_out=st[:, 5:6]), reads=[plb, b_st], writes=[b_lg, b_st])
                    T.op("dve", lambda e: e.reciprocal(out=st[:, 6:7], in_=st[:, 5:6]), reads=[b_st], writes=[b_st])
                    T.op("dve", lambda e, j=j: e.tensor_scalar(out=aff[:, j, :], in0=lg[:], scalar1=st[:, 6:7], scalar2=None, op0=ALU.mult),
                         reads=[b_lg, b_st], writes=[b_aff[j]])
            T.sync_all()
            if "aff_dbg" in dbg:
                aff_dbg = nc.dram_tensor("aff_dbg", [128, NT * NE], F32, kind="ExternalOutput").ap()
                T.op("sp", lambda e: e.dma_start(out=aff_dbg, in_=aff[:].rearrange("p j e -> p (j e)")), reads=b_aff, writes=[B("affd")], dma=True)
                T.sync_all()
            if stop == 3:
                return nc

        b_Xg = [B("Xg%d" % i) for i in range(NE)]
        b_Yg = [B("Yg%d" % i) for i in range(NE)]
        with ExitStack() as p4:
            s4 = lambda n, s, d: p4.enter_context(nc.sbuf_tensor("sb_" + n, s, d))
            gt = s4("gt", [128, NOWN, NE], F32); b_gt = B("gt")
            idx_i = s4("idx_i", [128, NOWN * NE], I32); b_idx = B("idx")
            bc_reg = nc.gpsimd.to_reg(CAP - 1)
            with ExitStack() as p4a:
                sa = lambda n, s, d: p4a.enter_context(nc.sbuf_tensor("sb_" + n, s, d))
                cmp_ = sa("cmp", [128, NT, NE], F32); b_cmp = B("cmp")
                lo = sa("lo", [128, NE], F32); mid = sa("mid", [128, NE], F32); ge = sa("ge", [128, NE], F32)
                cntp = sa("cntp", [128, NE], BF16); b_bis = B("bis")
                msk = sa("msk", [128, NOWN, NE], F32); mskb = sa("mskb", [128, NOWN * NE], BF16); b_msk = B("msk")
                cc = sa("cc", [128, NOWN, NE], F32); offs = sa("offs", [128, NOWN, NE], F32); b_offs = B("offs")
                posf = sa("posf", [128, NOWN * NE], F32); tmpf = sa("tmpf", [128, NOWN * NE], F32); b_pos = B("pos")
                pc, pcb = banks[0], bankb[0]
                T.op("dve", lambda e: e.memset(lo[:], 0.0), writes=[b_bis])
                for it in range(NBISECT):
                    c_ = 2.0 ** (-(it + 1))
                    T.op("dve", lambda e, c_=c_: e.tensor_scalar(out=mid[:], in0=lo[:], scalar1=c_, scalar2=None, op0=ALU.add),
                         reads=[b_bis], writes=[b_bis])
                    T.op("dve", lambda e: e.tensor_tensor(out=cmp_[:], in0=aff[:], in1=mid[:].unsqueeze(1).to_broadcast([128, NT, NE]),
                                                          op=ALU.is_ge), reads=b_aff + [b_bis], writes=[b_cmp])
                    with nc.allow_low_precision(reason="integer counts <= 64 are exact in bf16"):
                        T.op("dve", lambda e: e.tensor_reduce(out=cntp[:], in_=cmp_[:].rearrange("p j e -> p e j"), axis=AX.X, op=ALU.add),
                             reads=[b_cmp], writes=[b_bis])
                    T.op("pe", lambda e: e.matmul(pc[:, 0:NE], lhsT=ones[:], rhs=cntp[:], start=True, stop=True),
                         reads=[b_bis, b_const], writes=[pcb])
                    T.op("dve", lambda e: e.tensor_scalar(out=ge[:], in0=pc[:, 0:NE], scalar1=float(CAP) - 0.5, scalar2=None, op0=ALU.is_ge),
                         reads=[pcb], writes=[b_bis])
                    T.op("dve", lambda e, c_=c_: e.scalar_tensor_tensor(out=lo[:], in0=ge[:], scalar=c_, in1=lo[:], op0=ALU.mult, op1=ALU.add),
                         reads=[b_bis], writes=[b_bis])
                T.op("dve", lambda e: e.tensor_tensor(out=msk[:], in0=aff[:, 0:NOWN, :], in1=lo[:].unsqueeze(1).to_broadcast([128, NOWN, NE]),
                                                      op=ALU.is_ge), reads=b_aff + [b_bis], writes=[b_msk])
                T.op("dve", lambda e: e.tensor_tensor(out=gt[:], in0=aff[:, 0:NOWN, :], in1=msk[:], op=ALU.mult), reads=b_aff + [b_msk], writes=[b_gt])
                T.op("dve", lambda e: e.tensor_copy(out=mskb[:], in_=msk[:].rearrange("p j e -> p (j e)")), reads=[b_msk], writes=[b_msk])
                pp_, ppb_ = banks[1], bankb[1]
                pc2, pc2b = banks[2], bankb[2]
                T.op("pe", lambda e: e.matmul(pp_[:], lhsT=triU[:], rhs=mskb[:], start=True, stop=True), reads=[b_msk, b_const], writes=[ppb_])
                T.op("pe", lambda e: e.matmul(pc2[:], lhsT=ones[:], rhs=mskb[:], start=True, stop=True), reads=[b_msk, b_const], writes=[pc2b])
                T.op("act", lambda e: e.copy(out=cc[:], in_=pc2[:].rearrange("p (j e) -> p j e", e=NE)), reads=[pc2b], writes=[b_offs])
                T.op("dve", lambda e: e.memset(offs[:, 0, :], 0.0), reads=[b_offs], writes=[b_offs])
                for j in range(1, NOWN):
                    T.op("dve", lambda e, j=j: e.tensor_tensor(out=offs[:, j, :], in0=offs[:, j - 1, :], in1=cc[:, j - 1, :], op=ALU.add),
                         reads=[b_offs], writes=[b_offs])
                T.op("dve", lambda e: e.tensor_tensor(out=posf[:], in0=pp_[:], in1=offs[:].rearrange("p j e -> p (j e)"), op=ALU.add),
                     reads=[ppb_, b_offs], writes=[b_pos])
                T.op("dve", lambda e: e.tensor_scalar(out=tmpf[:], in0=msk[:].rearrange("p j e -> p (j e)"), scalar1=-4096.0, scalar2=4096.0,
                                                      op0=ALU.mult, op1=ALU.add), reads=[b_msk], writes=[b_pos])
                T.op("dve", lambda e: e.tensor_tensor(out=posf[:], in0=posf[:], in1=tmpf[:], op=ALU.add), reads=[b_pos], writes=[b_pos])
                T.op("dve", lambda e: e.tensor_copy(out=idx_i[:], in_=posf[:]), reads=[b_pos], writes=[b_idx])
                T.sync_all()
            with ExitStack() as p4b:
                sbx = lambda n, s, d: p4b.enter_context(nc.sbuf_tensor("sb_" + n, s, d))
                Wg = sbx("Wg", [128, KC, FF], BF16); Wu = sbx("Wu", [128, KC, FF], BF16); Wd = sbx("Wd", [128, 8, D], BF16)
                b_Wg = B("Wg"); b_Wu = B("Wu"); b_Wd = B("Wd")
                XgT = sbx("XgT", [128, KC, CAP], BF16); b_XgT = [B("XgT%d" % i) for i in range(8)]
                HT = sbx("HT", [128, 8, CAP], BF16); b_HT = [B("HT0"), B("HT1")]
                xg = [sbx("xg%d" % i, [128, D], BF16) for i in range(2)]; b_xg = [B("xg0"), B("xg1")]
                ysb = [sbx("ysb%d" % i, [128, D], BF16) for i in range(2)]; b_ysb = [B("ysb0"), B("ysb1")]
                sg = [sbx("sg%d" % i, [128, 512], F32) for i in range(2)]; b_sg = [B("sg0"), B("sg1")]
                rot_t = Rot([0, 1]); rot_a = Rot([2, 3]); rot_u = Rot([4, 5]); rot_y = Rot([6, 7])
                hsc = [sbx("hsc%d" % i, [128, D], BF16) for i in range(4)]; b_hsc = [B("hsc%d" % i) for i in range(4)]
                sc_tok = [[] for _ in range(NE)]
                nsc = [0]

                def scatter_expert(ex):
                    for j in range(NOWN):
                        Hs, bHs = hsc[nsc[0] % 4], b_hsc[nsc[0] % 4]; nsc[0] += 1
                        T.op("sp", lambda e, Hs=Hs, j=j: e.dma_start(out=Hs[:], in_=h2_s[j * 128:(j + 1) * 128, :]),
                             reads=[b_h2[j]], writes=[bHs], dma=True)
                        sc_tok[ex].append(T.op("pool", lambda e, Hs=Hs, j=j: e.indirect_dma_start(
                            out=Xg[ex], out_offset=bass.IndirectOffsetOnAxis(ap=idx_i[:, j * NE + ex:j * NE + ex + 1], axis=0),
                            in_=Hs[:, :], in_offset=None, bounds_check=bc_reg, oob_is_err=False),
                            reads=[bHs, b_idx], writes=[], dma=True, nd=160))

                def load_w(ex, which):
                    if "g" in which:
                        T.op("sp", lambda e: e.dma_start(out=Wg[:], in_=wg_bf[ex].rearrange("(k p) f -> p k f", p=128)),
                             reads=[b_wg[ex]], writes=[b_Wg], dma=True)
                        T.op("sp", lambda e: e.dma_start(out=Wu[:], in_=wu_bf[ex].rearrange("(k p) f -> p k f", p=128)),
                             reads=[b_wu[ex]], writes=[b_Wu], dma=True)
                    if "d" in which:
                        T.op("sp", lambda e: e.dma_start(out=Wd[:], in_=wd_bf[ex].rearrange("(k p) f -> p k f", p=128)),
                             reads=[b_wd[ex]], writes=[b_Wd], dma=True)

                load_w(0, "gd")
                scatter_expert(0)
                for e_ in range(NE):
                    for tok in sc_tok[e_]:
                        T._wait("sp", tok[0], tok[1])
                    for st_ in range(8):
                        XGt, bXGt = xg[st_ % 2], b_xg[st_ % 2]
                        T.op("sp", lambda e, XGt=XGt, e_=e_, st_=st_: e.dma_start(out=XGt[:], in_=Xg[e_][st_ * 128:(st_ + 1) * 128, :]),
                             reads=[b_Xg[e_]], writes=[bXGt], dma=True)
                        for q4 in range(4):
                            pb, pbb = rot_t.get()
                            pv = pb[:].bitcast(BF16)
                            for kk in range(4):
                                k = q4 * 4 + kk
                                T.op("pe", lambda e, pv=pv, kk=kk, XGt=XGt, k=k: e.transpose(
                                    out=pv[:, kk * 128:(kk + 1) * 128], in_=XGt[:, k * 128:(k + 1) * 128], identity=ident[:]),
                                    reads=[bXGt, b_const], writes=[pbb])
                            if q4 % 2 == 0:
                                T.op("act", lambda e, pv=pv, q4=q4, st_=st_: e.copy(
                                    out=XgT[:, q4 * 4:(q4 + 1) * 4, st_ * 128:(st_ + 1) * 128],
                                    in_=pv[:, 0:512].rearrange("p (k t) -> p k t", t=128)), reads=[pbb], writes=[b_XgT[st_]])
                            else:
                                T.op("dve", lambda e, pv=pv, q4=q4, st_=st_: e.tensor_copy(
                                    out=XgT[:, q4 * 4:(q4 + 1) * 4, st_ * 128:(st_ + 1) * 128],
                                    in_=pv[:, 0:512].rearrange("p (k t) -> p k t", t=128)), reads=[pbb], writes=[b_XgT[st_]])
                    if e_ + 1 < NE:
                        scatter_expert(e_ + 1)
                    for sh in range(2):
                        for fc in range(8):
                            pa, pab = rot_a.get(); pu, pub = rot_u.get()
                            for k in range(KC):
                                T.op("pe", lambda e, pa=pa, k=k, fc=fc, sh=sh: e.matmul(
                                    pa[:], lhsT=Wg[:, k, fc * 128:(fc + 1) * 128], rhs=XgT[:, k, sh * 512:(sh + 1) * 512],
                                    start=(k == 0), stop=(k == KC - 1)), reads=[b_Wg] + b_XgT[sh * 4:sh * 4 + 4], writes=[pab])
                            for k in range(KC):
                                T.op("pe", lambda e, pu=pu, k=k, fc=fc, sh=sh: e.matmul(
                                    pu[:], lhsT=Wu[:, k, fc * 128:(fc + 1) * 128], rhs=XgT[:, k, sh * 512:(sh + 1) * 512],
                                    start=(k == 0), stop=(k == KC - 1)), reads=[b_Wu] + b_XgT[sh * 4:sh * 4 + 4], writes=[pub])
                            SG, bSG = sg[fc % 2], b_sg[fc % 2]
                            T.op("act", lambda e, SG=SG, pa=pa: e.activation(out=SG[:], in_=pa[:], func=AF.Silu), reads=[pab], writes=[bSG])
                            T.op("dve", lambda e, SG=SG, pu=pu, fc=fc, sh=sh: e.tensor_tensor(
                                out=HT[:, fc, sh * 512:(sh + 1) * 512], in0=pu[:], in1=SG[:], op=ALU.mult),
                                reads=[pub, bSG], writes=[b_HT[sh]])
                    if e_ + 1 < NE:
                        load_w(e_ + 1, "g")
                    for st_ in range(8):
                        Y, bY = ysb[st_ % 2], b_ysb[st_ % 2]
                        for dg in range(4):
                            py, pyb = rot_y.get()
                            for fc in range(8):
                                T.op("pe", lambda e, py=py, fc=fc, st_=st_, dg=dg: e.matmul(
                                    py[:], lhsT=HT[:, fc, st_ * 128:(st_ + 1) * 128], rhs=Wd[:, fc, dg * 512:(dg + 1) * 512],
                                    start=(fc == 0), stop=(fc == 7)), reads=[b_HT[st_ // 4], b_Wd], writes=[pyb])
                            if dg % 2 == 0:
                                T.op("act", lambda e, Y=Y, py=py, dg=dg: e.copy(out=Y[:, dg * 512:(dg + 1) * 512], in_=py[:]),
                                     reads=[pyb], writes=[bY])
                            else:
                                T.op("dve", lambda e, Y=Y, py=py, dg=dg: e.tensor_copy(out=Y[:, dg * 512:(dg + 1) * 512], in_=py[:]),
                                     reads=[pyb], writes=[bY])
                        T.op("sp", lambda e, Y=Y, e_=e_, st_=st_: e.dma_start(out=Yg[e_][st_ * 128:(st_ + 1) * 128, :], in_=Y[:]),
                             reads=[bY], writes=[b_Yg[e_]], dma=True)
                    if e_ + 1 < NE:
                        load_w(e_ + 1, "d")
                T.sync_all()
                if stop == 5:
                    return nc
            with ExitStack() as p4c:
                sc = lambda n, s, d: p4c.enter_context(nc.sbuf_tensor("sb_" + n, s, d))
                g_bc = sc("g_bc3", [128, D], F32); b_g = B("g_bc3")
                acc = [sc("acc%d" % i, [128, D], F32) for i in range(2)]; b_acc = [B("acc0"), B("acc1")]
                NG = 12
                G = [sc("G%d" % i, [128, D], BF16) for i in range(NG)]; b_G = [B("G%d" % i) for i in range(NG)]
                dgm = [sc("dgm%d" % i, [128, 128], BF16) for i in range(NG)]; b_dgm = [B("dgm%d" % i) for i in range(NG)]
                ob = [sc("ob%d" % i, [128, D], F32) for i in range(2)]; b_ob = [B("ob0"), B("ob1")]
                junk = sc("junk3", [128, D], BF16); b_junk = B("junk3")
                st = sc("st4", [128, 8], F32); b_st = B("st4")
                T.op("sp", lambda e: e.dma_start(out=g_bc[:], in_=g_fin.partition_broadcast(128)), writes=[b_g], dma=True)
                for i in range(NG):
                    T.op("dve", lambda e, i=i: e.memset(G[i][:], 0.0), writes=[b_G[i]])
                ng = 0
                b_out = B("out")
                for j in range(NOWN):
                    A, bA = acc[j % 2], b_acc[j % 2]
                    T.op("sp", lambda e, A=A, j=j: e.dma_start(out=A[:], in_=x1_s[j * 128:(j + 1) * 128, :]), reads=[b_x1[j]], writes=[bA], dma=True)
                    pbk = [(banks[(j % 2) * 4 + c], bankb[(j % 2) * 4 + c]) for c in range(4)]
                    for e_ in range(NE):
                        Gb, bGb = G[ng % NG], b_G[ng % NG]
                        Dg, bDg = dgm[ng % NG], b_dgm[ng % NG]; ng += 1
                        T.op("pool", lambda e, Gb=Gb, j=j, e_=e_: e.indirect_dma_start(
                            out=Gb[:, :], out_offset=None, in_=Yg[e_],
                            in_offset=bass.IndirectOffsetOnAxis(ap=idx_i[:, j * NE + e_:j * NE + e_ + 1], axis=0),
                            bounds_check=bc_reg, oob_is_err=False), reads=[b_Yg[e_], b_idx], writes=[bGb], dma=True, nd=160)
                        T.op("dve", lambda e, Dg=Dg, j=j, e_=e_: e.tensor_scalar(out=Dg[:], in0=ident[:], scalar1=gt[:, j, e_:e_ + 1], scalar2=None,
                                                                                op0=ALU.mult), reads=[b_const, b_gt], writes=[bDg])
                        for c in range(4):
                            T.op("pe", lambda e, c=c, Dg=Dg, Gb=Gb, e_=e_: e.matmul(
                                pbk[c][0][:], lhsT=Dg[:], rhs=Gb[:, c * 512:(c + 1) * 512], start=(e_ == 0), stop=(e_ == NE - 1)),
                                reads=[bDg, bGb], writes=[pbk[c][1]])
                    for c in range(4):
                        T.op("dve", lambda e, c=c, A=A: e.tensor_tensor(out=A[:, c * 512:(c + 1) * 512], in0=pbk[c][0][:],
                                                                         in1=A[:, c * 512:(c + 1) * 512], op=ALU.add),
                             reads=[pbk[c][1], bA], writes=[bA])
                    T.op("act", lambda e, A=A: e.activation(out=junk[:], in_=A[:], func=AF.Square, accum_out=st[:, 0:1]),
                         reads=[bA], writes=[b_junk, b_st])
                    rstd_from_ss(st[:, 0:1], st[:, 1:2], st[:, 2:3], 1, 1.0 / D, [b_st])
                    O_, bO_ = ob[j % 2], b_ob[j % 2]
                    T.op("dve", lambda e, O_=O_, A=A: e.scalar_tensor_tensor(out=O_[:], in0=A[:], scalar=st[:, 2:3], in1=g_bc[:],
                                                                            op0=ALU.mult, op1=ALU.mult), reads=[bA, b_st, b_g], writes=[bO_])
                    T.op("sp", lambda e, O_=O_, j=j: e.dma_start(out=out[j * 128:(j + 1) * 128, :], in_=O_[:]), reads=[bO_], writes=[b_out], dma=True)
                T.sync_all()
        print("ops:", T.cnt, "dmas:", T.dcnt, "waits:", T.nwait, "sems:", len(T.sems))
    return nc
```
